# Optimizing a Trainium2 kernel written in Bass

```python
import math
import jax
import jax.numpy as jnp
from jax import lax
import numpy as np

D_MODEL = 2048
BATCH = 4
SEQ = 2048
DEPTH = 1

CTX_LEN = 256
GRID_W = 64
HEAD_DIM = 128
A_HEADS = D_MODEL // HEAD_DIM
KV_HEADS = A_HEADS // 4
Q_BLOCK = 128
ROPE_THETA = 10000.0
ROPE_PAIRS_AXIS = HEAD_DIM // 4
M_HEADS = 4
MV_DIM = D_MODEL // M_HEADS
MQK_DIM = MV_DIM // 2
M_CHUNK = 64
N_EXPERTS = 32
TOP_K = 4
D_FF = D_MODEL
SWIGLU_LIMIT = 7.0
SWIGLU_ALPHA = 1.702
EXPERT_BLOCK = 128
EPS = 1e-6

IN_SPLITS = (
    M_HEADS * MQK_DIM,
    M_HEADS * MQK_DIM,
    M_HEADS * MV_DIM,
    M_HEADS * MV_DIM,
    4 * M_HEADS,
    A_HEADS * HEAD_DIM,
    KV_HEADS * HEAD_DIM,
    KV_HEADS * HEAD_DIM,
    2 * D_MODEL,
)
F_IN = sum(IN_SPLITS)

kernel_name = 'hybrid_mlstm_gqa_moe_diffusion_block'


def rmsnorm(x, g):
    xf = x.astype(jnp.float32)
    y = xf * lax.rsqrt(jnp.mean(xf * xf, axis=-1, keepdims=True) + EPS)
    return (y * g.astype(jnp.float32)).astype(x.dtype)


def split_in(p):
    parts, start = [], 0
    for size in IN_SPLITS:
        parts.append(p[..., start:start + size])
        start += size
    return parts


def axial_rope_tables(n_tokens):
    rows = n_tokens // GRID_W
    row_ids = jnp.repeat(jnp.arange(rows), GRID_W).astype(jnp.float32)
    col_ids = jnp.tile(jnp.arange(GRID_W), rows).astype(jnp.float32)
    freqs = jnp.exp(-math.log(ROPE_THETA) * jnp.arange(ROPE_PAIRS_AXIS, dtype=jnp.float32) / ROPE_PAIRS_AXIS)
    ang = jnp.concatenate([row_ids[:, None] * freqs, col_ids[:, None] * freqs], axis=-1)
    return jnp.cos(ang), jnp.sin(ang)


def apply_rope(x, cos, sin):
    xf = x.astype(jnp.float32).reshape(x.shape[:-1] + (HEAD_DIM // 2, 2))
    x0, x1 = xf[..., 0], xf[..., 1]
    co, si = cos[None, :, None, :], sin[None, :, None, :]
    out = jnp.stack([x0 * co - x1 * si, x0 * si + x1 * co], axis=-1)
    return out.reshape(x.shape).astype(x.dtype)


def blocked_attention(q, k, v):
    n_b, n_t = q.shape[:2]
    n_blk = n_t // Q_BLOCK
    qb = q.reshape(n_b, n_blk, Q_BLOCK, KV_HEADS, A_HEADS // KV_HEADS, HEAD_DIM).transpose(1, 0, 2, 3, 4, 5)
    scale = HEAD_DIM ** -0.5

    def one_block(qi):
        s = jnp.einsum('bqkgd,bnkd->bkgqn', qi, k, preferred_element_type=jnp.float32) * scale
        p = jax.nn.softmax(s, axis=-1).astype(v.dtype)
        return jnp.einsum('bkgqn,bnkd->bqkgd', p, v)

    o = lax.map(one_block, qb)
    return o.transpose(1, 0, 2, 3, 4, 5).reshape(n_b, n_t, A_HEADS * HEAD_DIM)


def mlstm_inputs(pp):
    n_b, n_t = pp[0].shape[:2]

    def heads(a, d):
        return a.astype(jnp.float32).reshape(n_b, n_t, M_HEADS, d).transpose(0, 2, 1, 3)

    q = heads(pp[0], MQK_DIM) * (MQK_DIM ** -0.5)
    k = heads(pp[1], MQK_DIM)
    v = heads(pp[2], MV_DIM)
    gates = pp[4].astype(jnp.float32).reshape(n_b, n_t, 4, M_HEADS).transpose(2, 0, 3, 1)
    i_f, f_f, i_b, f_b = gates[0], gates[1], gates[2], gates[3]
    return (q, k, v, i_f, jax.nn.log_sigmoid(f_f)), (q, k, v, i_b, jax.nn.log_sigmoid(f_b))


def mlstm_chunkwise(q, k, v, ig, lf, state):
    n_b, n_h, n_t, _ = q.shape
    n_chunk = n_t // M_CHUNK

    def chunks(a):
        return jnp.moveaxis(a.reshape(a.shape[:2] + (n_chunk, M_CHUNK) + a.shape[3:]), 2, 0)

    tril = jnp.tril(jnp.ones((M_CHUNK, M_CHUNK), dtype=bool))

    def step(carry, inp):
        c_mat, n_vec, m = carry
        qc, kc, vc, igc, lfc = inp
        b = jnp.cumsum(lfc, axis=-1)
        d_intra = jnp.where(tril, b[..., :, None] - b[..., None, :] + igc[..., None, :], -jnp.inf)
        d_inter = b + m[..., None]
        m_t = jnp.maximum(d_inter, jnp.max(d_intra, axis=-1))
        s = jnp.einsum('bhtd,bhsd->bhts', qc, kc) * jnp.exp(d_intra - m_t[..., None])
        w_inter = jnp.exp(d_inter - m_t)
        num = jnp.einsum('bhts,bhsv->bhtv', s, vc) + w_inter[..., None] * jnp.einsum('bhtd,bhvd->bhtv', qc, c_mat)
        den = jnp.sum(s, axis=-1) + w_inter * jnp.einsum('bhtd,bhd->bht', qc, n_vec)
        h = num / jnp.maximum(jnp.abs(den), jnp.exp(-m_t))[..., None]
        b_end = b[..., -1]
        g = b_end[..., None] - b + igc
        m_new = jnp.maximum(b_end + m, jnp.max(g, axis=-1))
        w_s = jnp.exp(g - m_new[..., None])
        w_c = jnp.exp(b_end + m - m_new)
        c_new = w_c[..., None, None] * c_mat + jnp.einsum('bhs,bhsv,bhsd->bhvd', w_s, vc, kc)
        n_new = w_c[..., None] * n_vec + jnp.einsum('bhs,bhsd->bhd', w_s, kc)
        return (c_new, n_new, m_new), h

    state, h = lax.scan(step, state, tuple(chunks(a) for a in (q, k, v, ig, lf)))
    h = jnp.moveaxis(h, 0, 2).reshape(n_b, n_h, n_t, MV_DIM)
    return h, state


def mlstm_final_state(k, v, ig, lf):
    b = jnp.cumsum(lf, axis=-1)
    b_end = b[..., -1]
    g = b_end[..., None] - b + ig
    m = jnp.maximum(b_end, jnp.max(g, axis=-1))
    w = jnp.exp(g - m[..., None])
    return (jnp.einsum('bhs,bhsv,bhsd->bhvd', w, v, k), jnp.einsum('bhs,bhsd->bhd', w, k), m)


def mlstm_direction(lat, ctx, need_ctx_out):
    if need_ctx_out:
        kc = ctx[1]
        zero = (jnp.zeros(kc.shape[:2] + (MV_DIM, MQK_DIM), jnp.float32),
                jnp.zeros(kc.shape[:2] + (MQK_DIM,), jnp.float32),
                jnp.zeros(kc.shape[:2], jnp.float32))
        h_ctx, state = mlstm_chunkwise(ctx[0], ctx[1], ctx[2], ctx[3], ctx[4], zero)
    else:
        h_ctx, state = None, mlstm_final_state(ctx[1], ctx[2], ctx[3], ctx[4])
    h_lat, _ = mlstm_chunkwise(lat[0], lat[1], lat[2], lat[3], lat[4], state)
    return h_lat, h_ctx


def flip_time(tup):
    return tuple(jnp.flip(a, axis=2) for a in tup)


def mlstm_readout(h, o_pre, g_mh):
    n_b, _, n_t, _ = h.shape
    y = rmsnorm(h, g_mh.reshape(M_HEADS, 1, MV_DIM))
    y = y.transpose(0, 2, 1, 3).reshape(n_b, n_t, M_HEADS * MV_DIM).astype(o_pre.dtype)
    return y * jax.nn.sigmoid(o_pre)


def merge_branches(gate_pre, m_out, a_out, w_br_m, w_br_a, w_out):
    g_m, g_a = jnp.split(jax.nn.sigmoid(gate_pre), 2, axis=-1)
    return (g_m * (m_out @ w_br_m) + g_a * (a_out @ w_br_a)) @ w_out


def moe_ffn(u, w_router, b_router, w_gu, b_gu, w_dn, b_dn):
    n_tok = u.shape[0]
    logits = (u @ w_router + b_router).astype(jnp.float32)
    top_logit, top_idx = lax.top_k(logits, TOP_K)
    top_w = jax.nn.softmax(top_logit, axis=-1)
    n_assign = n_tok * TOP_K
    exp_ids = top_idx.reshape(n_assign)
    tok_ids = jnp.repeat(jnp.arange(n_tok, dtype=jnp.int32), TOP_K)
    order = jnp.argsort(exp_ids)
    exp_sorted = exp_ids[order]
    counts = jnp.bincount(exp_ids, length=N_EXPERTS)
    padded = (counts + EXPERT_BLOCK - 1) // EXPERT_BLOCK * EXPERT_BLOCK
    start = jnp.cumsum(counts) - counts
    pad_end = jnp.cumsum(padded)
    pad_start = pad_end - padded
    slot = pad_start[exp_sorted] + jnp.arange(n_assign, dtype=jnp.int32) - start[exp_sorted]
    n_blocks = (n_assign + N_EXPERTS * (EXPERT_BLOCK - 1) + EXPERT_BLOCK - 1) // EXPERT_BLOCK
    n_slots = n_blocks * EXPERT_BLOCK
    slot_tok = jnp.zeros((n_slots,), jnp.int32).at[slot].set(tok_ids[order])
    slot_w = jnp.zeros((n_slots,), jnp.float32).at[slot].set(top_w.reshape(n_assign)[order])
    block_exp = jnp.minimum(jnp.searchsorted(pad_end, jnp.arange(n_blocks) * EXPERT_BLOCK, side='right'), N_EXPERTS - 1)
    xs = u[slot_tok].reshape(n_blocks, EXPERT_BLOCK, u.shape[1])

    def expert_block(args):
        xb, e = args
        gu = xb @ w_gu[e] + b_gu[e]
        gate = jnp.minimum(gu[:, :D_FF], SWIGLU_LIMIT)
        up = jnp.clip(gu[:, D_FF:], -SWIGLU_LIMIT, SWIGLU_LIMIT)
        hid = (up + 1) * gate * jax.nn.sigmoid(SWIGLU_ALPHA * gate)
        return hid @ w_dn[e] + b_dn[e]

    ys = lax.map(expert_block, (xs, block_exp)).reshape(n_slots, u.shape[1])
    return jnp.zeros_like(u).at[slot_tok].add(ys * slot_w[:, None].astype(ys.dtype))


def layer(hx, hc, c, c_ctx, need_ctx_out, cos, sin, w_mod, b_mod, g_norm1, g_norm2, w_in, b_in,
          g_q, g_k, g_mh, w_br_m, w_br_a, w_out, w_router, b_router, w_gu, b_gu, w_dn, b_dn):
    n_b, n_t, d = hx.shape
    n_c = hc.shape[1]
    mod_x = jax.nn.silu(c) @ w_mod + b_mod
    mod_c = jax.nn.silu(c_ctx) @ w_mod + b_mod
    sh1x, sc1x, gt1x, sh2x, sc2x, gt2x = [m[:, None, :] for m in jnp.split(mod_x, 6, axis=-1)]
    sh1c, sc1c, gt1c, sh2c, sc2c, gt2c = jnp.split(mod_c, 6, axis=-1)

    ux = rmsnorm(hx, g_norm1) * (1 + sc1x) + sh1x
    uc = rmsnorm(hc, g_norm1) * (1 + sc1c) + sh1c
    px = split_in(ux @ w_in + b_in)
    pc = split_in(uc @ w_in + b_in)

    qx = apply_rope(rmsnorm(px[5].reshape(n_b, n_t, A_HEADS, HEAD_DIM), g_q), cos, sin)
    kx = apply_rope(rmsnorm(px[6].reshape(n_b, n_t, KV_HEADS, HEAD_DIM), g_k), cos, sin)
    vx = px[7].reshape(n_b, n_t, KV_HEADS, HEAD_DIM)
    kc = rmsnorm(pc[6].reshape(n_b, n_c, KV_HEADS, HEAD_DIM), g_k)
    vc = pc[7].reshape(n_b, n_c, KV_HEADS, HEAD_DIM)
    ax = blocked_attention(qx, jnp.concatenate([kx, kc], axis=1), jnp.concatenate([vx, vc], axis=1))

    lat_f, lat_b = mlstm_inputs(px)
    ctx_f, ctx_b = mlstm_inputs(pc)
    hx_f, hc_f = mlstm_direction(lat_f, ctx_f, need_ctx_out)
    hx_b, hc_b = mlstm_direction(flip_time(lat_b), flip_time(ctx_b), need_ctx_out)
    mx = mlstm_readout(hx_f + jnp.flip(hx_b, axis=2), px[3], g_mh)

    hx_new = hx + gt1x * merge_branches(px[8], mx, ax, w_br_m, w_br_a, w_out)
    u2x = rmsnorm(hx_new, g_norm2) * (1 + sc2x) + sh2x
    hx_new = hx_new + gt2x * moe_ffn(u2x.reshape(-1, d), w_router, b_router, w_gu, b_gu, w_dn, b_dn).reshape(hx.shape)

    hc_new = hc
    if need_ctx_out:
        qc = rmsnorm(pc[5].reshape(n_b, n_c, A_HEADS, HEAD_DIM), g_q)
        ac = blocked_attention(qc, kc, vc)
        mc = mlstm_readout(hc_f + jnp.flip(hc_b, axis=2), pc[3], g_mh)
        hc_new = hc + gt1c * merge_branches(pc[8], mc, ac, w_br_m, w_br_a, w_out)
        u2c = rmsnorm(hc_new, g_norm2) * (1 + sc2c) + sh2c
        hc_new = hc_new + gt2c * moe_ffn(u2c.reshape(-1, d), w_router, b_router, w_gu, b_gu, w_dn, b_dn).reshape(hc.shape)
    return hx_new, hc_new


def setup_inputs(seed: int = 0) -> dict:
    key = jax.random.key(seed)
    ks = jax.random.split(key, 24)
    f32 = jnp.float32
    D = D_MODEL
    nrm = lambda k, shape, s: jax.random.normal(k, shape, f32) * s
    f_off = sum(IN_SPLITS[:4])
    forget_bias = 3.0 + 3.0 * jnp.linspace(0.0, 1.0, M_HEADS)
    b_in = nrm(ks[9], (DEPTH, F_IN), 0.01)
    b_in = b_in.at[:, f_off + M_HEADS:f_off + 2 * M_HEADS].add(forget_bias)
    b_in = b_in.at[:, f_off + 3 * M_HEADS:f_off + 4 * M_HEADS].add(forget_bias)
    return {
        'x': nrm(ks[0], (BATCH, SEQ, D), 1.0),
        'c': nrm(ks[1], (BATCH, D), 1.0),
        'ctx': nrm(ks[2], (BATCH, CTX_LEN, D), 1.0),
        'c_ctx': nrm(ks[3], (D,), 1.0),
        'w_mod': nrm(ks[4], (DEPTH, D, 6 * D), 0.5 * D ** -0.5),
        'b_mod': nrm(ks[5], (DEPTH, 6 * D), 0.01),
        'g_norm1': 1.0 + nrm(ks[6], (DEPTH, D), 0.01),
        'g_norm2': 1.0 + nrm(ks[7], (DEPTH, D), 0.01),
        'w_in': nrm(ks[8], (DEPTH, D, F_IN), D ** -0.5),
        'b_in': b_in,
        'g_q': 1.0 + nrm(ks[10], (DEPTH, HEAD_DIM), 0.01),
        'g_k': 1.0 + nrm(ks[11], (DEPTH, HEAD_DIM), 0.01),
        'g_mh': 1.0 + nrm(ks[12], (DEPTH, M_HEADS * MV_DIM), 0.01),
        'w_br_m': nrm(ks[13], (DEPTH, M_HEADS * MV_DIM, D), (M_HEADS * MV_DIM) ** -0.5),
        'w_br_a': nrm(ks[14], (DEPTH, A_HEADS * HEAD_DIM, D), (A_HEADS * HEAD_DIM) ** -0.5),
        'w_out': nrm(ks[15], (DEPTH, D, D), D ** -0.5),
        'w_router': nrm(ks[16], (DEPTH, D, N_EXPERTS), D ** -0.5),
        'b_router': nrm(ks[17], (DEPTH, N_EXPERTS), 0.01),
        'w_gu': nrm(ks[18], (DEPTH, N_EXPERTS, D, 2 * D_FF), D ** -0.5),
        'b_gu': nrm(ks[19], (DEPTH, N_EXPERTS, 2 * D_FF), 0.01),
        'w_dn': nrm(ks[20], (DEPTH, N_EXPERTS, D_FF, D), D_FF ** -0.5),
        'b_dn': nrm(ks[21], (DEPTH, N_EXPERTS, D), 0.01),
    }


def reference(x, c, ctx, c_ctx, w_mod, b_mod, g_norm1, g_norm2, w_in, b_in, g_q, g_k, g_mh,
              w_br_m, w_br_a, w_out, w_router, b_router, w_gu, b_gu, w_dn, b_dn):
    n_t = x.shape[1]
    cos, sin = axial_rope_tables(n_t)
    hx, hc = x, ctx
    for l in range(DEPTH):
        hx, hc = layer(hx, hc, c, c_ctx, l < DEPTH - 1, cos, sin, w_mod[l], b_mod[l], g_norm1[l], g_norm2[l],
                       w_in[l], b_in[l], g_q[l], g_k[l], g_mh[l], w_br_m[l], w_br_a[l], w_out[l],
                       w_router[l], b_router[l], w_gu[l], b_gu[l], w_dn[l], b_dn[l])
    return hx
```

```python
import contextlib
import math
import numpy as np
import concourse.bass as bass
import concourse.mybir as mybir
from concourse.bass_utils import run_bass_kernel_spmd

F32 = mybir.dt.float32
BF16 = mybir.dt.bfloat16
ALU = mybir.AluOpType
AF = mybir.ActivationFunctionType

COMPUTE = ("pe", "dve", "act", "pool")
NDMA = {"sp": 8, "pool": 4}


class Res:
    __slots__ = ("name", "w", "rd")

    def __init__(self, name=""):
        self.name = name
        self.w = None
        self.rd = []


class Prog:
    def __init__(self, nc, es):
        self.nc = nc
        self.es = es
        self.ops = {e: [] for e in ("pe", "dve", "act", "pool", "sp")}
        self.cnt = {e: 0 for e in COMPUTE}
        self.sem = {e: es.enter_context(nc.semaphore("s_" + e)) for e in COMPUTE}
        self.known = {e: {} for e in ("pe", "dve", "act", "pool", "sp")}
        self.snaps = {e: [dict()] for e in COMPUTE}
        self.dsem = {}
        self.duse = {}
        self.drot = {}
        for q, n in NDMA.items():
            for i in range(n):
                self.dsem[(q, i)] = es.enter_context(nc.semaphore(f"d_{q}{i}"))
                self.duse[(q, i)] = 0
            self.drot[q] = 0

    def semh(self, key):
        return self.sem[key] if key in self.sem else self.dsem[key]

    def _need(self, eng, ev, waits):
        if ev is None:
            return
        key, val = ev
        if self.known[eng].get(key, 0) >= val:
            return
        if waits.get(key, 0) < val:
            waits[key] = val

    def _learn(self, eng, key, val):
        kn = self.known[eng]
        if kn.get(key, 0) < val:
            kn[key] = val
        if key in self.snaps:
            for k2, v2 in self.snaps[key][val].items():
                if kn.get(k2, 0) < v2:
                    kn[k2] = v2

    def _deps(self, eng, reads, writes):
        waits = {}
        for r in reads:
            self._need(eng, r.w, waits)
        for r in writes:
            self._need(eng, r.w, waits)
            for ev in r.rd:
                self._need(eng, ev, waits)
        return waits

    def op(self, eng, fn, reads=(), writes=()):
        waits = self._deps(eng, reads, writes)
        if eng in waits:
            own_raw = 0
            for r in reads:
                if r.w is not None and r.w[0] == eng:
                    own_raw = max(own_raw, r.w[1])
            if eng != "pe" and own_raw > self.known[eng].get(eng, 0):
                waits[eng] = own_raw
            else:
                del waits[eng]
        for k, v in waits.items():
            self._learn(eng, k, v)
        self.cnt[eng] += 1
        n = self.cnt[eng]
        self.ops[eng].append((list(waits.items()), fn, (eng, 1)))
        self.snaps[eng].append(dict(self.known[eng]))
        ev = (eng, n)
        for r in reads:
            if len(r.rd) > 48:
                _compact([r])
            r.rd.append(ev)
        for r in writes:
            r.w = ev
            r.rd = []
        return ev

    def dma(self, q, fn, reads=(), writes=()):
        waits = self._deps(q, reads, writes)
        i = self.drot[q]
        self.drot[q] = (i + 1) % NDMA[q]
        key = (q, i)
        prev = self.duse[key]
        if prev > 0 and self.known[q].get(key, 0) < prev:
            if waits.get(key, 0) < prev:
                waits[key] = prev
        for k, v in waits.items():
            self._learn(q, k, v)
        self.duse[key] = prev + 16
        self.ops[q].append((list(waits.items()), fn, (key, 16)))
        ev = (key, prev + 16)
        for r in reads:
            r.rd.append(ev)
        for r in writes:
            r.w = ev
            r.rd = []
        return ev

    def barrier(self):
        targets = {e: self.cnt[e] for e in COMPUTE if self.cnt[e] > 0}
        targets.update({k: v for k, v in self.duse.items() if v > 0})
        for e in ("pe", "dve", "act", "pool", "sp"):
            waits = {k: v for k, v in targets.items() if k != e and self.known[e].get(k, 0) < v}
            for k, v in waits.items():
                self._learn(e, k, v)
            if waits:
                self.ops[e].append((list(waits.items()), None, None))

    def finish(self):
        nc = self.nc
        waits = {}
        for key, v in self.duse.items():
            if v > 0:
                waits[key] = v
        for e in COMPUTE:
            if self.cnt[e] > 0:
                waits[e] = self.cnt[e]
        self.ops["sp"].append((list(waits.items()), None, None))
        hmap = {"pe": "tensor", "dve": "vector", "act": "scalar", "pool": "gpsimd", "sp": "sync"}
        with nc.Block() as block:
            for e, attr in hmap.items():
                ops = self.ops[e]
                if not ops:
                    continue

                def section(engh, ops=ops):
                    for waits, fn, inc in ops:
                        for k, v in waits:
                            engh.wait_ge(self.semh(k), v)
                        if fn is None:
                            continue
                        inst = fn(engh)
                        if inc is not None:
                            inst.then_inc(self.semh(inc[0]), inc[1])

                getattr(block, attr)(section)


def _compact(res_list):
    for r in res_list:
        d = {}
        for k, v in r.rd:
            if d.get(k, 0) < v:
                d[k] = v
        r.rd = list(d.items())


class Cfg:
    D = 2048
    NCTX = 2
    NOTH = 8
    NOWN = 8
    NE = 32
    DFF = 2048
    SEQ = 2048
    GRID_W = 64
    TOPK = 4
    DEBUG = False


HD = 128
MH = 4
MQK = 256
MV = 512
EPS = 1e-6
F_IN = 13328
OFF_MQ, OFF_MK, OFF_MV, OFF_OG, OFF_GT, OFF_AQ, OFF_AK, OFF_AV, OFF_MG = (
    0, 1024, 2048, 4096, 6144, 6160, 8208, 8720, 9232)


def build(cfg):
    D = cfg.D
    DC = D // 128
    NT = cfg.NCTX + cfg.NOTH + cfg.NOWN
    NOWN = cfg.NOWN
    C0 = cfg.NCTX + cfg.NOTH
    T = NOWN * 128
    TA = NT * 128
    TB = min(512, T)
    NB = T // TB
    NE = cfg.NE
    DFF = cfg.DFF
    FC = DFF // 128

    nc = bass.Bass("TRN2", target_bir_lowering=False)

    def din(name, shape):
        return nc.dram_tensor(name, list(shape), F32, kind="ExternalInput").ap()

    xf = din("xf", [TA, D])
    cc = din("cc", [2, D])
    w_mod = din("w_mod", [D, 6 * D])
    b_mod = din("b_mod", [1, 6 * D])
    g1 = din("g1", [1, D])
    g2 = din("g2", [1, D])
    w_in = din("w_in", [D, F_IN])
    b_in = din("b_in", [1, F_IN])
    wg16 = din("wg16", [D, 16])
    bg16 = din("bg16", [1, 16])
    g_q = din("g_q", [128, 1])
    g_k = din("g_k", [128, 1])
    g_mh = din("g_mh", [1, MH * MV])
    w_br_m = din("w_br_m", [D, D])
    w_br_a = din("w_br_a", [D, D])
    w_out = din("w_out", [D, D])
    w_router = din("w_router", [D, NE])
    b_router = din("b_router", [1, NE])
    w_gu = din("w_gu", [NE, D, 2 * DFF])
    b_gu = din("b_gu", [NE, 2 * DFF])
    w_dn = din("w_dn", [NE, DFF, D])
    b_dn = din("b_dn", [NE, D])
    c_ident = din("c_ident", [128, 128])
    c_triU = din("c_triU", [128, 128])
    c_triL = din("c_triL", [128, 128])
    c_rot = din("c_rot", [128, 128])
    c_triSU = din("c_triSU", [128, 128])
    c_iota = din("c_iota", [128, 512])
    c_cos = din("c_cos", [128, TA])
    c_sin = din("c_sin", [128, TA])
    out_d = nc.dram_tensor("out", [T, D], F32, kind="ExternalOutput").ap()

    def scratch(name, shape, dt=BF16):
        if cfg.DEBUG:
            return nc.dram_tensor(name, list(shape), dt, kind="ExternalOutput").ap()
        return nc.dram_tensor(name, list(shape), dt).ap()

    def dbg_out(name, shape, dt):
        return nc.dram_tensor(name, list(shape), dt, kind="ExternalOutput").ap()

    mod_d = scratch("mod_d", [2, 6 * D], F32)
    MQT = scratch("MQT", [8, 128, T])
    MKT = scratch("MKT", [8, 128, T])
    AQT = scratch("AQT", [16, 128, T])
    GMT = scratch("GMT", [32, 128, T])
    MKd = scratch("MKd", [NT, 128, MH * MQK])
    MVd = scratch("MVd", [NT, 128, MH * MV])
    OGd = scratch("OGd", [NOWN, 128, MH * MV])
    AVd = scratch("AVd", [NT, 128, 4 * HD])

    with contextlib.ExitStack() as es:
        P = Prog(nc, es)

        KB = 1024
        ARENA = 171 * KB
        arena = es.enter_context(nc.sbuf_tensor("arena", [128, ARENA // 4], F32))

        class Bump:
            def __init__(self, off, limit):
                self.off = off
                self.limit = limit

        def sb(stack, name, shape, dt):
            if isinstance(stack, Bump):
                esz = 4 if dt == F32 else 2
                n = int(np.prod(shape[1:]))
                nbytes = (n * esz + 31) // 32 * 32
                assert stack.off + nbytes <= stack.limit, (name, stack.off, nbytes, stack.limit)
                a = arena[0:shape[0], stack.off // 4:(stack.off + nbytes) // 4]
                stack.off += nbytes
                v = a if dt == F32 else a.bitcast(BF16)
                v = v[:, 0:n]
                if len(shape) == 3:
                    v = v.rearrange("p (a b) -> p a b", a=shape[1])
                return v
            return stack.enter_context(nc.sbuf_tensor(name, list(shape), dt))

        def at(off, shape, dt):
            return sb(Bump(off, ARENA), "x", shape, dt)

        def V(fn, r=(), w=()):
            return P.op("dve", fn, r, w)

        def A(fn, r=(), w=()):
            return P.op("act", fn, r, w)

        def G(fn, r=(), w=()):
            return P.op("pool", fn, r, w)

        def M(fn, r=(), w=()):
            return P.op("pe", fn, r, w)

        def LD(out, in_, r=(), w=(), slow=False):
            if slow:
                return P.dma("sp", lambda e: e.dma_start(out=out, in_=in_, allow_slow_non_contiguous=True), r, w)
            return P.dma("sp", lambda e: e.dma_start(out=out, in_=in_), r, w)

        def LDC(out, in_, r=(), w=(), slow=False):
            if slow:
                return P.dma("pool", lambda e: e.dma_start(out=out, in_=in_, allow_slow_non_contiguous=True), r, w)
            return P.dma("pool", lambda e: e.dma_start(out=out, in_=in_), r, w)

        pbank = [es.enter_context(nc.psum_tensor(f"pb{i}", [128, 512], F32)) for i in range(7)]
        rbank = [Res(f"pb{i}") for i in range(7)]
        ptr = es.enter_context(nc.psum_tensor("ptr", [128, 1024], BF16))
        r_ptr = Res("ptr")

        ident_f = sb(es, "ident_f", [128, 128], F32); r_identf = Res()
        ident_b = sb(es, "ident_b", [128, 128], BF16); r_identb = Res()
        triU = sb(es, "triU", [128, 128], F32); r_triU = Res()
        triL = sb(es, "triL", [128, 128], F32); r_triL = Res()
        ones_f = sb(es, "ones_f", [128, 128], F32); r_onesf = Res()
        ones_b = sb(es, "ones_b", [128, 128], BF16); r_onesb = Res()
        rot_b = sb(es, "rot_b", [128, 128], BF16); r_rot = Res()
        epsb = sb(es, "epsb", [128, 1], F32); r_eps = Res()
        CONSTS = [r_identf, r_identb, r_triU, r_triL, r_onesf, r_onesb, r_rot, r_eps]
        LD(ident_f[:], c_ident, w=[r_identf])
        LDC(ident_b[:], c_ident, w=[r_identb])
        LD(triU[:], c_triU, w=[r_triU])
        LD(triL[:], c_triL, w=[r_triL])
        LDC(rot_b[:], c_rot, w=[r_rot])
        G(lambda e: e.memset(ones_f[:], 1.0), w=[r_onesf])
        G(lambda e: e.memset(ones_b[:], 1.0), w=[r_onesb])
        G(lambda e: e.memset(epsb[:], EPS), w=[r_eps])

        r_gt1, r_gt2, r_A2, r_sh2 = Res(), Res(), Res(), Res()

        NR = 2
        wring = [sb(es, f"wring{i}", [128, DC, 512], BF16) for i in range(NR)]
        rring = [Res(f"wring{i}") for i in range(NR)]
        ring_i = [0]

        def load_w(src2d, ncols):
            i = ring_i[0]
            ring_i[0] = (i + 1) % NR
            t, r = wring[i], rring[i]
            half = DC // 2
            v = src2d.rearrange("(dc p) n -> p dc n", p=128)
            LDC(t[:, 0:half, 0:ncols], v[:, 0:half, :], w=[r])
            P.dma("pool", lambda e: e.dma_start(out=t[:, half:DC, 0:ncols], in_=v[:, half:DC, :]), [r], [r])
            return t, r

        bank_i = [0]

        def next_bank():
            i = bank_i[0]
            bank_i[0] = (i + 1) % 7
            return pbank[i], rbank[i]

        NS = 4
        stg = []
        rstg = [Res(f"stg{i}") for i in range(NS)]
        stg_i = [0]

        def next_stg():
            i = stg_i[0]
            stg_i[0] = (i + 1) % NS
            return stg[i], rstg[i]

        r_mod = Res("mod_d")
        r_MQT, r_MKT, r_AQT, r_GMT = Res(), Res(), Res(), Res()
        r_MKd, r_MVd, r_OGd, r_AVd = Res(), Res(), Res(), Res()

        with contextlib.ExitStack() as s0:
            s0 = Bump(90 * KB, ARENA)
            cT_f = sb(s0, "cT_f", [128, 2, DC], F32); r_cTf = Res()
            cT_b = sb(s0, "cT_b", [128, DC, 2], BF16); r_cTb = Res()
            bm2 = sb(s0, "bm2", [2, 512], F32); r_bm2 = Res()
            mrow = sb(s0, "mrow", [2, 512], F32); r_mrow = Res()
            for r_ in range(2):
                P.dma("sp", lambda e, r_=r_: e.dma_start(out=cT_f[:, r_, :], in_=cc[r_, :].rearrange("(dc p) -> p dc", p=128),
                                                         allow_slow_non_contiguous=True), [r_cTf], [r_cTf])
            for r_ in range(2):
                A(lambda e, r_=r_: e.activation(out=cT_b[:, :, r_], in_=cT_f[:, r_, :], func=AF.Silu), [r_cTf, r_cTb], [r_cTb])
            for blk in range(6 * D // 512):
                wt, wr = load_w(w_mod[:, blk * 512:(blk + 1) * 512], 512)
                pb, rb = next_bank()
                for dc in range(DC):
                    M(lambda e, dc=dc, pb=pb, wt=wt: e.matmul(pb[0:2, :], cT_b[:, dc, :], wt[:, dc, :],
                                                                start=(dc == 0), stop=(dc == DC - 1)),
                      [r_cTb, wr], [rb])
                LD(bm2[0:1, :], b_mod[0:1, blk * 512:(blk + 1) * 512], w=[r_bm2])
                P.dma("sp", lambda e, blk=blk: e.dma_start(out=bm2[1:2, :], in_=b_mod[0:1, blk * 512:(blk + 1) * 512]),
                      [r_bm2], [r_bm2])
                V(lambda e, pb=pb: e.tensor_tensor(out=mrow[:], in0=pb[0:2, :], in1=bm2[:], op=ALU.add),
                  [rb, r_bm2], [r_mrow])
                LD(mod_d[:, blk * 512:(blk + 1) * 512], mrow[:], [r_mrow], [r_mod])

        def bc_row(ap_row, n):
            return ap_row.broadcast_to([128, n])

        P.barrier()

        uT = at(0, [128, DC, TA], BF16)
        r_uT = [Res(f"uT{c}") for c in range(NT)]
        with contextlib.ExitStack() as s1:
            s1 = Bump(72 * KB, ARENA)
            A1x = sb(s1, "A1x", [128, D], F32); r_A1x = Res()
            S1x = sb(s1, "S1x", [128, D], F32); r_S1x = Res()
            A1c = sb(s1, "A1c", [128, D], F32); r_A1c = Res()
            S1c = sb(s1, "S1c", [128, D], F32); r_S1c = Res()
            g1b = sb(s1, "g1b", [128, D], F32); r_g1b = Res()
            LD(g1b[:], bc_row(g1[0:1, :], D), w=[r_g1b])
            LD(A1x[:], bc_row(mod_d[0:1, D:2 * D], D), [r_mod], [r_A1x])
            LD(A1c[:], bc_row(mod_d[1:2, D:2 * D], D), [r_mod], [r_A1c])
            LD(S1x[:], bc_row(mod_d[0:1, 0:D], D), [r_mod], [r_S1x])
            LD(S1c[:], bc_row(mod_d[1:2, 0:D], D), [r_mod], [r_S1c])
            V(lambda e: e.scalar_tensor_tensor(out=A1x[:], in0=A1x[:], scalar=1.0, in1=g1b[:], op0=ALU.add, op1=ALU.mult),
              [r_A1x, r_g1b], [r_A1x])
            V(lambda e: e.scalar_tensor_tensor(out=A1c[:], in0=A1c[:], scalar=1.0, in1=g1b[:], op0=ALU.add, op1=ALU.mult),
              [r_A1c, r_g1b], [r_A1c])
            xt = [sb(s1, f"xt{i}", [128, D], F32) for i in range(2)]
            rxt = [Res(), Res()]
            xn = [sb(s1, f"xn{i}", [128, D], BF16) for i in range(2)]
            rxn = [Res(), Res()]
            ss = sb(s1, "ss", [128, 2], F32); r_ss = [Res(), Res()]
            for c in range(NT):
                i = c % 2
                Aw, rA, Sw, rS = (A1c, r_A1c, S1c, r_S1c) if c < cfg.NCTX else (A1x, r_A1x, S1x, r_S1x)
                LD(xt[i][:], xf[c * 128:(c + 1) * 128, :], w=[rxt[i]])
                G(lambda e, i=i: e.memset(ss[:, i:i + 1], 0.0), w=[r_ss[i]])
                A(lambda e, i=i: e.activation(out=xn[i][:], in_=xt[i][:], func=AF.Square, accum_out=ss[:, i:i + 1]),
                  [rxt[i], r_ss[i]], [rxn[i], r_ss[i]])
                A(lambda e, i=i: e.activation(out=ss[:, i:i + 1], in_=ss[:, i:i + 1], func=AF.Sqrt, bias=epsb[:], scale=1.0 / D),
                  [r_ss[i], r_eps], [r_ss[i]])
                V(lambda e, i=i: e.reciprocal(out=ss[:, i:i + 1], in_=ss[:, i:i + 1]), [r_ss[i]], [r_ss[i]])
                V(lambda e, i=i, Aw=Aw: e.scalar_tensor_tensor(out=xt[i][:], in0=xt[i][:], scalar=ss[:, i:i + 1], in1=Aw[:],
                                                              op0=ALU.mult, op1=ALU.mult),
                  [rxt[i], r_ss[i], rA], [rxt[i]])
                G(lambda e, i=i, Sw=Sw: e.tensor_tensor(out=xn[i][:], in0=xt[i][:], in1=Sw[:], op=ALU.add),
                  [rxt[i], rS], [rxn[i]])
                for half in range(DC // 8):
                    for j in range(8):
                        dc = half * 8 + j
                        M(lambda e, i=i, dc=dc, j=j: e.transpose(ptr[:, j * 128:(j + 1) * 128], xn[i][:, dc * 128:(dc + 1) * 128], ident_b[:]),
                          [rxn[i], r_identb], [r_ptr])
                    if half % 2 == 0:
                        V(lambda e, c=c, half=half: e.tensor_copy(
                            out=uT[:, half * 8:(half + 1) * 8, c * 128:(c + 1) * 128],
                            in_=ptr[:].rearrange("p (j t) -> p j t", j=8)), [r_ptr], [r_uT[c]])
                    else:
                        A(lambda e, c=c, half=half: e.activation(
                            out=uT[:, half * 8:(half + 1) * 8, c * 128:(c + 1) * 128],
                            in_=ptr[:].rearrange("p (j t) -> p j t", j=8), func=AF.Copy), [r_ptr], [r_uT[c]])
                _compact(CONSTS)

        own_blocks = [(C0 * 128 + b * TB, TB) for b in range(NB)]
        all_blocks = []
        o = 0
        while o < TA:
            n = min(512, TA - o)
            all_blocks.append((o, n))
            o += n

        def uT_res(t0, n):
            return r_uT[t0 // 128:(t0 + n + 127) // 128]

        if cfg.DEBUG:
            P.barrier()
            LD(dbg_out("dbg_uT", [128, DC, TA], BF16), uT[:], r_uT, [Res()])
        P.barrier()
        gates = at(160 * KB, [128, NT, 16], F32); r_gates = Res()
        AKT = at(72 * KB, [128, 4, TA], BF16); r_AKT = Res()

        with contextlib.ExitStack() as s2:
            s2 = Bump(90 * KB, 160 * KB)
            stg.extend(sb(s2, f"stg{i}", [128, 512], BF16) for i in range(NS))
            bT = sb(s2, "bT", [128, 72], F32); r_bT = Res()
            for (off, n, col) in ((OFF_MQ, 8, 0), (OFF_MK, 8, 8), (OFF_AQ, 16, 16), (OFF_AK, 4, 32), (OFF_MG, 32, 36)):
                P.dma("sp", lambda e, off=off, n=n, col=col: e.dma_start(
                    out=bT[:, col:col + n], in_=b_in[0, off:off + n * 128].rearrange("(j p) -> p j", p=128),
                    allow_slow_non_contiguous=True), [r_bT], [r_bT])
            brows = [sb(s2, f"brow{i}", [1, 512], BF16) for i in range(2)]
            rbrows = [Res(), Res()]
            brow_i = [0]
            bg_bc = sb(s2, "bg_bc", [128, 16], F32); r_bg = Res()
            LD(bg_bc[:], bc_row(bg16[0:1, :], 16), w=[r_bg])
            wg_b = sb(s2, "wg_b", [128, DC, 16], BF16); r_wg = Res()
            LDC(wg_b[:], wg16.rearrange("(dc p) n -> p dc n", p=128), w=[r_wg])
            gq_s = sb(s2, "gq_s", [128, 1], F32); r_gq = Res()
            gk_s = sb(s2, "gk_s", [128, 1], F32); r_gk = Res()
            LD(gq_s[:], g_q, w=[r_gq])
            LD(gk_s[:], g_k, w=[r_gk])
            cos_s = sb(s2, "cos_s", [128, TA], BF16); r_cos = Res()
            sin_s = sb(s2, "sin_s", [128, TA], BF16); r_sin = Res()
            LDC(cos_s[:], c_cos, w=[r_cos])
            LDC(sin_s[:], c_sin, w=[r_sin])
            sq = sb(s2, "sq", [128, 512], BF16); r_sq = Res()
            rstd = sb(s2, "rstd", [128, 512], F32); r_rstd = Res()
            xnb = sb(s2, "xnb", [128, 512], BF16); r_xnb = Res()
            t1 = sb(s2, "t1", [128, 512], F32); r_t1 = Res()
            t2 = sb(s2, "t2", [128, 512], F32); r_t2 = Res()

            def fm_group(off, nchunks, blocks, evac):
                for s in range(0, nchunks, 4):
                    ncol = min(4, nchunks - s) * 128
                    wt, wr = load_w(w_in[:, off + s * 128: off + s * 128 + ncol], ncol)
                    for jj in range(ncol // 128):
                        j = s + jj
                        for bi, (t0, n) in enumerate(blocks):
                            pb, rb = next_bank()
                            for dc in range(DC):
                                M(lambda e, dc=dc, pb=pb, wt=wt, jj=jj, t0=t0, n=n: e.matmul(
                                    pb[:, 0:n], wt[:, dc, jj * 128:(jj + 1) * 128], uT[:, dc, t0:t0 + n],
                                    start=(dc == 0), stop=(dc == DC - 1)),
                                  [wr] + uT_res(t0, n), [rb])
                            evac(j, bi, t0, n, pb, rb)
                    _compact(CONSTS + r_uT)

            def simple_evac(dst, rdst, bcol, scale):
                def f(j, bi, t0, n, pb, rb):
                    st, rs = next_stg()
                    V(lambda e: e.tensor_scalar(out=st[:, 0:n], in0=pb[:, 0:n], scalar1=bT[:, bcol + j:bcol + j + 1],
                                                scalar2=scale, op0=ALU.add, op1=ALU.mult), [rb, r_bT], [rs])
                    tl = t0 - C0 * 128
                    LD(dst[j, :, tl:tl + n], st[:, 0:n], [rs], [rdst])
                return f

            def sig_evac(dst, rdst, bcol):
                def f(j, bi, t0, n, pb, rb):
                    st, rs = next_stg()
                    A(lambda e: e.activation(out=st[:, 0:n], in_=pb[:, 0:n], func=AF.Sigmoid,
                                             bias=bT[:, bcol + j:bcol + j + 1], scale=1.0), [rb, r_bT], [rs])
                    tl = t0 - C0 * 128
                    LD(dst[j, :, tl:tl + n], st[:, 0:n], [rs], [rdst])
                return f

            def qknorm_evac(is_q):
                bcol = 16 if is_q else 32
                gs, rg = (gq_s, r_gq) if is_q else (gk_s, r_gk)

                def f(j, bi, t0, n, pb, rb):
                    V(lambda e: e.tensor_scalar(out=t1[:, 0:n], in0=pb[:, 0:n], scalar1=bT[:, bcol + j:bcol + j + 1],
                                                scalar2=None, op0=ALU.add), [rb, r_bT], [r_t1])
                    A(lambda e: e.activation(out=sq[:, 0:n], in_=t1[:, 0:n], func=AF.Square), [r_t1], [r_sq])
                    p2, r2 = next_bank()
                    M(lambda e: e.matmul(p2[:, 0:n], ones_b[:], sq[:, 0:n], start=True, stop=True), [r_onesb, r_sq], [r2])
                    A(lambda e: e.activation(out=rstd[:, 0:n], in_=p2[:, 0:n], func=AF.Sqrt, bias=epsb[:], scale=1.0 / HD),
                      [r2, r_eps], [r_rstd])
                    V(lambda e: e.reciprocal(out=rstd[:, 0:n], in_=rstd[:, 0:n]), [r_rstd], [r_rstd])
                    V(lambda e: e.scalar_tensor_tensor(out=xnb[:, 0:n], in0=t1[:, 0:n], scalar=gs[:, 0:1], in1=rstd[:, 0:n],
                                                       op0=ALU.mult, op1=ALU.mult), [r_t1, rg, r_rstd], [r_xnb])
                    p3, r3 = next_bank()
                    M(lambda e: e.matmul(p3[:, 0:n], rot_b[:], xnb[:, 0:n], start=True, stop=True), [r_rot, r_xnb], [r3])
                    V(lambda e: e.tensor_tensor(out=t2[:, 0:n], in0=p3[:, 0:n], in1=sin_s[:, t0:t0 + n], op=ALU.mult),
                      [r3, r_sin], [r_t2])
                    G(lambda e: e.tensor_tensor(out=t1[:, 0:n], in0=xnb[:, 0:n], in1=cos_s[:, t0:t0 + n], op=ALU.mult),
                      [r_xnb, r_cos], [r_t1])
                    if is_q:
                        st, rs = next_stg()
                        G(lambda e: e.tensor_tensor(out=st[:, 0:n], in0=t1[:, 0:n], in1=t2[:, 0:n], op=ALU.add),
                          [r_t1, r_t2], [rs])
                        tl = t0 - C0 * 128
                        LD(AQT[j, :, tl:tl + n], st[:, 0:n], [rs], [r_AQT])
                    else:
                        G(lambda e: e.tensor_tensor(out=AKT[:, j, t0:t0 + n], in0=t1[:, 0:n], in1=t2[:, 0:n], op=ALU.add),
                          [r_t1, r_t2], [r_AKT])
                return f

            fm_group(OFF_MQ, 8, own_blocks, simple_evac(MQT, r_MQT, 0, 1.0 / 16.0))
            fm_group(OFF_MK, 8, own_blocks, simple_evac(MKT, r_MKT, 8, 1.0))
            fm_group(OFF_AQ, 16, own_blocks, qknorm_evac(True))
            fm_group(OFF_AK, 4, all_blocks, qknorm_evac(False))
            fm_group(OFF_MG, 32, own_blocks, sig_evac(GMT, r_GMT, 36))

            def tm_group(off, ncols, chunks, dst, rdst, sig, own_only):
                for s in range(0, ncols, 512):
                    wt, wr = load_w(w_in[:, off + s: off + s + 512], 512)
                    bi_ = brow_i[0]
                    brow_i[0] = 1 - bi_
                    brow, r_brow = brows[bi_], rbrows[bi_]
                    LDC(brow[:], b_in[0:1, off + s: off + s + 512], w=[r_brow])
                    for c in chunks:
                        pb, rb = next_bank()
                        for dc in range(DC):
                            M(lambda e, dc=dc, pb=pb, wt=wt, c=c: e.matmul(
                                pb[:, :], uT[:, dc, c * 128:(c + 1) * 128], wt[:, dc, :], start=(dc == 0), stop=False),
                              [wr, r_uT[c]], [rb])
                        M(lambda e, pb=pb, brow=brow: e.matmul(pb[:, :], ones_b[0:1, :], brow[0:1, :],
                                                               start=False, stop=True), [r_onesb, r_brow], [rb])
                        st, rs = next_stg()
                        if sig:
                            A(lambda e, pb=pb, st=st: e.activation(out=st[:], in_=pb[:], func=AF.Sigmoid), [rb], [rs])
                        else:
                            V(lambda e, pb=pb, st=st: e.tensor_copy(out=st[:], in_=pb[:]), [rb], [rs])
                        cl = c - C0 if own_only else c
                        LD(dst[cl, :, s:s + 512], st[:], [rs], [rdst])
                    _compact(CONSTS + r_uT)

            allc = list(range(NT))
            ownc = list(range(C0, NT))
            tm_group(OFF_MK, MH * MQK, allc, MKd, r_MKd, False, False)
            tm_group(OFF_MV, MH * MV, allc, MVd, r_MVd, False, False)
            tm_group(OFF_OG, MH * MV, ownc, OGd, r_OGd, True, True)
            tm_group(OFF_AV, 4 * HD, allc, AVd, r_AVd, False, False)
            for c in allc:
                pb, rb = next_bank()
                for dc in range(DC):
                    M(lambda e, dc=dc, pb=pb, c=c: e.matmul(pb[:, 0:16], uT[:, dc, c * 128:(c + 1) * 128], wg_b[:, dc, :],
                                                            start=(dc == 0), stop=(dc == DC - 1)), [r_wg, r_uT[c]], [rb])
                V(lambda e, pb=pb, c=c: e.tensor_tensor(out=gates[:, c, :], in0=pb[:, 0:16], in1=bg_bc[:], op=ALU.add),
                  [rb, r_bg], [r_gates])
        _compact(CONSTS + r_uT)

        if cfg.DEBUG:
            P.barrier()
            LD(dbg_out("dbg_gates", [128, NT, 16], F32), gates[:], [r_gates], [Res()])
            LD(dbg_out("dbg_AKT", [128, 4, TA], BF16), AKT[:], [r_AKT], [Res()])
        P.barrier()
        MOT = at(0, [128, 16, T], BF16); r_MOT = Res()
        AOT = at(32 * KB, [128, 16, T], BF16); r_AOT = Res()

        with contextlib.ExitStack() as s3:
            s3 = Bump(90 * KB, 160 * KB)
            s3b = Bump(32 * KB, 72 * KB)
            lf = sb(s3, "lf", [128, NT, 8], F32); r_lf = Res()
            A(lambda e: e.activation(out=lf[:], in_=gates[:, :, 8:16], func=AF.Exp, scale=-1.0), [r_gates], [r_lf])
            A(lambda e: e.activation(out=lf[:], in_=lf[:], func=AF.Ln, bias=1.0, scale=1.0), [r_lf], [r_lf])
            V(lambda e: e.tensor_scalar(out=lf[:], in0=lf[:], scalar1=-1.0, scalar2=None, op0=ALU.mult), [r_lf], [r_lf])
            rr = sb(s3, "rr", [128, NT, 8], F32); r_rr = Res()
            einv = sb(s3, "einv", [128, NT, 8], F32); r_einv = Res()
            etot = sb(s3, "etot", [128, NT, 8], F32); r_etot = Res()
            for c in range(NT):
                pb, rb = next_bank()
                M(lambda e, pb=pb, c=c: e.matmul(pb[:, 0:4], triU[:], lf[:, c, 0:4], start=True, stop=True), [r_triU, r_lf], [rb])
                M(lambda e, pb=pb, c=c: e.matmul(pb[:, 4:8], triL[:], lf[:, c, 4:8], start=True, stop=True), [r_triL, r_lf], [rb])
                M(lambda e, pb=pb, c=c: e.matmul(pb[:, 8:16], ones_f[:], lf[:, c, 0:8], start=True, stop=True), [r_onesf, r_lf], [rb])
                V(lambda e, pb=pb, c=c: e.tensor_tensor(out=rr[:, c, :], in0=gates[:, c, 0:8], in1=pb[:, 0:8], op=ALU.subtract),
                  [rb, r_gates], [r_rr])
                A(lambda e, pb=pb, c=c: e.activation(out=einv[:, c, :], in_=pb[:, 0:8], func=AF.Exp, scale=-1.0), [rb], [r_einv])
                A(lambda e, pb=pb, c=c: e.activation(out=etot[:, c, :], in_=pb[:, 8:16], func=AF.Exp), [rb], [r_etot])
            A(lambda e: e.activation(out=rr[:], in_=rr[:], func=AF.Exp), [r_rr], [r_rr])
            _compact(CONSTS)

            gmh_bc = sb(s3, "gmh_bc", [128, MV], F32); r_gmh = Res()
            qT = sb(s3, "qT", [128, 2, T], BF16); r_qT = Res()
            kT = sb(s3, "kT", [128, 2, T], BF16); r_kT = Res()
            ktm = sb(s3, "ktm", [128, NT, MQK], BF16); r_ktm = Res()
            vtm = sb(s3b, "vtm", [128, NT, MV], BF16); r_vtm = Res()
            ogt = sb(s3, "ogt", [128, NOWN, MV], BF16); r_ogt = Res()
            hA = sb(s3b, "hA", [128, NOWN, MV], F32); r_hA = Res()
            Cst = sb(s3, "Cst", [128, 2, MV + 1], F32); r_Cst = Res()
            Cb = sb(s3, "Cb", [128, 2, MV + 1], BF16); r_Cb = Res()
            kr = sb(s3, "kr", [128, MQK], BF16); r_kr = Res()
            PT = sb(s3, "PT", [128, 128], BF16); r_PT = Res()
            dtmp = sb(s3, "dtmp", [128, 2, MV + 1], F32); r_dtmp = Res()
            den = sb(s3, "den", [128, 2], F32); r_den = Res()
            hs = sb(s3, "hs", [128, MV], F32); r_hs = Res()
            hjunk = sb(s3, "hjunk", [128, MV], BF16); r_hjunk = Res()
            hss = sb(s3, "hss", [128, 1], F32); r_hss = Res()
            mo = sb(s3, "mo", [128, MV], BF16); r_mo = Res()

            for h in range(MH):
                for j in range(2):
                    LD(qT[:, j, :], MQT[2 * h + j], [r_MQT], [r_qT])
                    LD(kT[:, j, :], MKT[2 * h + j], [r_MKT], [r_kT])
                LD(ktm[:], MKd[:, :, h * MQK:(h + 1) * MQK].rearrange("c p n -> p c n"), [r_MKd], [r_ktm])
                LD(vtm[:], MVd[:, :, h * MV:(h + 1) * MV].rearrange("c p n -> p c n"), [r_MVd], [r_vtm])
                LD(ogt[:], OGd[:, :, h * MV:(h + 1) * MV].rearrange("c p n -> p c n"), [r_OGd], [r_ogt])
                LD(gmh_bc[:], bc_row(g_mh[0:1, h * MV:(h + 1) * MV], MV), w=[r_gmh])
                for dirn in range(2):
                    gi = h + 4 * dirn
                    mask, rmask = (triU, r_triU) if dirn == 0 else (triL, r_triL)
                    if dirn == 0:
                        order = list(range(NT))
                    else:
                        order = [1, 0] if cfg.NCTX == 2 else list(range(cfg.NCTX - 1, -1, -1))
                        order = order + list(range(NT - 1, C0 - 1, -1))
                    G(lambda e: e.memset(Cst[:], 0.0), w=[r_Cst])
                    G(lambda e: e.memset(Cb[:], 0.0), w=[r_Cb])
                    for idx, c in enumerate(order):
                        own = c >= C0
                        last = idx == len(order) - 1
                        co = c - C0
                        if own:
                            pS, rS = next_bank()
                            for j in range(2):
                                M(lambda e, j=j, pS=pS, co=co: e.matmul(pS[:, 0:128], kT[:, j, co * 128:(co + 1) * 128],
                                                                         qT[:, j, co * 128:(co + 1) * 128], start=(j == 0), stop=(j == 1)),
                                  [r_kT, r_qT], [rS])
                            V(lambda e, pS=pS, c=c, gi=gi, mask=mask: e.scalar_tensor_tensor(
                                out=PT[:], in0=pS[:, 0:128], scalar=rr[:, c, gi:gi + 1], in1=mask[:], op0=ALU.mult, op1=ALU.mult),
                              [rS, r_rr, rmask], [r_PT])
                            pN, rN = next_bank()
                            pD, rD = next_bank()
                            M(lambda e, pN=pN, c=c: e.matmul(pN[:, :], PT[:], vtm[:, c, :], start=True, stop=False), [r_PT, r_vtm], [rN])
                            for j in range(2):
                                M(lambda e, pN=pN, j=j, co=co: e.matmul(pN[:, :], qT[:, j, co * 128:(co + 1) * 128], Cb[:, j, 0:MV],
                                                                         start=False, stop=(j == 1)), [r_qT, r_Cb], [rN])
                            M(lambda e, pD=pD: e.matmul(pD[:, 0:1], PT[:], ones_b[:, 0:1], start=True, stop=False), [r_PT, r_onesb], [rD])
                            for j in range(2):
                                M(lambda e, pD=pD, j=j, co=co: e.matmul(pD[:, 0:1], qT[:, j, co * 128:(co + 1) * 128], Cb[:, j, MV:MV + 1],
                                                                         start=False, stop=(j == 1)), [r_qT, r_Cb], [rD])
                            A(lambda e, pD=pD: e.activation(out=den[:, 0:1], in_=pD[:, 0:1], func=AF.Abs), [rD], [r_den])
                            V(lambda e, c=c, gi=gi: e.tensor_tensor(out=den[:, 0:1], in0=den[:, 0:1], in1=einv[:, c, gi:gi + 1], op=ALU.max),
                              [r_den, r_einv], [r_den])
                            V(lambda e: e.reciprocal(out=den[:, 1:2], in_=den[:, 0:1]), [r_den], [r_den])
                            if dirn == 0:
                                V(lambda e, pN=pN, co=co: e.tensor_scalar(out=hA[:, co, :], in0=pN[:, :], scalar1=den[:, 1:2], scalar2=None,
                                                                           op0=ALU.mult), [rN, r_den], [r_hA])
                            else:
                                V(lambda e, pN=pN, co=co: e.scalar_tensor_tensor(out=hs[:], in0=pN[:, :], scalar=den[:, 1:2], in1=hA[:, co, :],
                                                                                  op0=ALU.mult, op1=ALU.add), [rN, r_den, r_hA], [r_hs])
                                G(lambda e: e.memset(hss[:], 0.0), w=[r_hss])
                                A(lambda e: e.activation(out=hjunk[:], in_=hs[:], func=AF.Square, accum_out=hss[:]), [r_hs, r_hss], [r_hjunk, r_hss])
                                A(lambda e: e.activation(out=hss[:], in_=hss[:], func=AF.Sqrt, bias=epsb[:], scale=1.0 / MV), [r_hss, r_eps], [r_hss])
                                V(lambda e: e.reciprocal(out=hss[:], in_=hss[:]), [r_hss], [r_hss])
                                V(lambda e, h=h: e.scalar_tensor_tensor(out=hs[:], in0=hs[:], scalar=hss[:, 0:1], in1=gmh_bc[:],
                                                                       op0=ALU.mult, op1=ALU.mult), [r_hs, r_hss, r_gmh], [r_hs])
                                G(lambda e, co=co: e.tensor_tensor(out=mo[:], in0=hs[:], in1=ogt[:, co, :], op=ALU.mult), [r_hs, r_ogt], [r_mo])
                                for j in range(4):
                                    M(lambda e, j=j: e.transpose(ptr[:, j * 128:(j + 1) * 128], mo[:, j * 128:(j + 1) * 128], ident_b[:]),
                                      [r_mo, r_identb], [r_ptr])
                                V(lambda e, h=h, co=co: e.tensor_copy(out=MOT[:, 4 * h:4 * h + 4, co * 128:(co + 1) * 128],
                                                                     in_=ptr[:, 0:512].rearrange("p (j t) -> p j t", j=4)), [r_ptr], [r_MOT])
                        if not last:
                            V(lambda e, c=c, gi=gi: e.tensor_scalar(out=kr[:], in0=ktm[:, c, :], scalar1=rr[:, c, gi:gi + 1], scalar2=None,
                                                                    op0=ALU.mult), [r_ktm, r_rr], [r_kr])
                            for j in range(2):
                                pC, rC = next_bank()
                                pn, rn = next_bank()
                                M(lambda e, pC=pC, j=j, c=c: e.matmul(pC[:, :], kr[:, j * 128:(j + 1) * 128], vtm[:, c, :], start=True, stop=True),
                                  [r_kr, r_vtm], [rC])
                                M(lambda e, pn=pn, j=j: e.matmul(pn[:, 0:1], kr[:, j * 128:(j + 1) * 128], ones_b[:, 0:1], start=True, stop=True),
                                  [r_kr, r_onesb], [rn])
                                V(lambda e, pC=pC, j=j: e.tensor_tensor(out=dtmp[:, j, 0:MV], in0=pC[:, :], in1=Cst[:, j, 0:MV], op=ALU.add),
                                  [rC, r_Cst], [r_dtmp])
                                V(lambda e, pn=pn, j=j: e.tensor_tensor(out=dtmp[:, j, MV:MV + 1], in0=pn[:, 0:1], in1=Cst[:, j, MV:MV + 1], op=ALU.add),
                                  [rn, r_Cst], [r_dtmp])
                            V(lambda e, c=c, gi=gi: e.tensor_scalar(out=Cst[:], in0=dtmp[:], scalar1=etot[:, c, gi:gi + 1], scalar2=None, op0=ALU.mult),
                              [r_dtmp, r_etot], [r_Cst])
                            A(lambda e: e.activation(out=Cb[:], in_=Cst[:], func=AF.Copy), [r_Cst], [r_Cb])
                    _compact(CONSTS + [r_rr, r_einv, r_etot, r_vtm, r_ktm, r_qT, r_kT])

        P.barrier()
        with contextlib.ExitStack() as s4:
            s4 = Bump(90 * KB, ARENA)
            vat = sb(s4, "vat", [128, NT, HD], BF16); r_vat = Res()
            qh = sb(s4, "qh", [128, T], BF16); r_qh = Res()
            PTa = [sb(s4, f"PTa{i}", [128, 512], BF16) for i in range(2)]
            rPTa = [Res(), Res()]
            rsum = sb(s4, "rsum", [128, 512], F32); r_rsum = Res()
            sc = 1.0 / math.sqrt(HD)
            for kh in range(4):
                LD(vat[:], AVd[:, :, kh * HD:(kh + 1) * HD].rearrange("c p n -> p c n"), [r_AVd], [r_vat])
                for g in range(4):
                    head = kh * 4 + g
                    LD(qh[:], AQT[head], [r_AQT], [r_qh])
                    for b in range(NB):
                        oz = 2 * ((head * NB + b) % 2)
                        pO, rO = pbank[oz], rbank[oz]
                        pZ, rZ = pbank[oz + 1], rbank[oz + 1]
                        for c in range(NT):
                            si = 4 + (c % 3)
                            pS, rS = pbank[si], rbank[si]
                            M(lambda e, pS=pS, c=c, b=b, kh=kh: e.matmul(pS[:, 0:TB], AKT[:, kh, c * 128:(c + 1) * 128], qh[:, b * TB:(b + 1) * TB],
                                                                        start=True, stop=True), [r_AKT, r_qh], [rS])
                            i = c % 2
                            A(lambda e, pS=pS, i=i: e.activation(out=PTa[i][:, 0:TB], in_=pS[:, 0:TB], func=AF.Exp, scale=sc), [rS], [rPTa[i]])
                            M(lambda e, pO=pO, c=c, i=i: e.matmul(pO[:, 0:TB], vat[:, c, :], PTa[i][:, 0:TB], start=(c == 0), stop=(c == NT - 1)),
                              [r_vat, rPTa[i]], [rO])
                            M(lambda e, pZ=pZ, c=c, i=i: e.matmul(pZ[:, 0:TB], ones_b[:], PTa[i][:, 0:TB], start=(c == 0), stop=(c == NT - 1)),
                              [r_onesb, rPTa[i]], [rZ])
                        V(lambda e, pZ=pZ: e.reciprocal(out=rsum[:, 0:TB], in_=pZ[:, 0:TB]), [rZ], [r_rsum])
                        V(lambda e, pO=pO, head=head, b=b: e.tensor_tensor(out=AOT[:, head, b * TB:(b + 1) * TB], in0=pO[:, 0:TB], in1=rsum[:, 0:TB],
                                                                          op=ALU.mult), [rO, r_rsum], [r_AOT])
                    _compact(CONSTS + [r_AKT, r_vat])

        P.barrier()

        acc = at(0, [128, NOWN, D], F32); r_acc = [Res(f"acc{c}") for c in range(NOWN)]
        u2tm = at(64 * KB, [128, NOWN, D], BF16); r_u2T = Res()
        Gd = at(96 * KB, [128, NOWN, NE], F32); r_Gd = Res()
        posm = at(97 * KB, [128, NOWN, NE], F32); r_posm = Res()
        gt1_bc = at(64 * KB, [128, D], F32)
        gt2_bc = at(98 * KB, [128, D], F32)

        with contextlib.ExitStack() as s5:
            s5 = Bump(122 * KB, ARENA)
            zT = at(90 * KB, [128, 16, T], BF16); r_zT = Res()
            gmt = sb(s5, "gmt", [128, 2, T], BF16); r_gmt = Res()
            za = sb(s5, "za", [128, 512], F32); r_za = Res()
            zb = sb(s5, "zb", [128, 512], F32); r_zb = Res()
            for s in range(4):
                wm, rwm = load_w(w_br_m[:, s * 512:(s + 1) * 512], 512)
                wa, rwa = load_w(w_br_a[:, s * 512:(s + 1) * 512], 512)
                for jj in range(4):
                    j = s * 4 + jj
                    LD(gmt[:, 0, :], GMT[j], [r_GMT], [r_gmt])
                    LD(gmt[:, 1, :], GMT[16 + j], [r_GMT], [r_gmt])
                    for b in range(NB):
                        pm, rm = next_bank()
                        pa, ra = next_bank()
                        for k in range(16):
                            M(lambda e, pm=pm, wm=wm, jj=jj, k=k, b=b: e.matmul(pm[:, 0:TB], wm[:, k, jj * 128:(jj + 1) * 128], MOT[:, k, b * TB:(b + 1) * TB],
                                                                               start=(k == 0), stop=(k == 15)), [rwm, r_MOT], [rm])
                        for k in range(16):
                            M(lambda e, pa=pa, wa=wa, jj=jj, k=k, b=b: e.matmul(pa[:, 0:TB], wa[:, k, jj * 128:(jj + 1) * 128], AOT[:, k, b * TB:(b + 1) * TB],
                                                                               start=(k == 0), stop=(k == 15)), [rwa, r_AOT], [ra])
                        V(lambda e, pm=pm, b=b: e.tensor_tensor(out=za[:, 0:TB], in0=pm[:, 0:TB], in1=gmt[:, 0, b * TB:(b + 1) * TB], op=ALU.mult),
                          [rm, r_gmt], [r_za])
                        V(lambda e, pa=pa, b=b: e.tensor_tensor(out=zb[:, 0:TB], in0=pa[:, 0:TB], in1=gmt[:, 1, b * TB:(b + 1) * TB], op=ALU.mult),
                          [ra, r_gmt], [r_zb])
                        G(lambda e, j=j, b=b: e.tensor_tensor(out=zT[:, j, b * TB:(b + 1) * TB], in0=za[:, 0:TB], in1=zb[:, 0:TB], op=ALU.add),
                          [r_za, r_zb], [r_zT])
                _compact(CONSTS + [r_MOT, r_AOT])
            if cfg.DEBUG:
                P.barrier()
                LD(dbg_out("dbg_MOT", [128, 16, T], BF16), MOT[:], [r_MOT], [Res()])
                LD(dbg_out("dbg_AOT", [128, 16, T], BF16), AOT[:], [r_AOT], [Res()])
                LD(dbg_out("dbg_zT", [128, 16, T], BF16), zT[:], [r_zT], [Res()])
            P.barrier()
            for c in range(NOWN):
                LD(acc[:, c, :], xf[(C0 + c) * 128:(C0 + c + 1) * 128, :], w=[r_acc[c]])
            LD(gt1_bc[:], bc_row(mod_d[0:1, 2 * D:3 * D], D), [r_mod], [r_gt1])
            for db in range(4):
                wo, rwo = load_w(w_out[:, db * 512:(db + 1) * 512], 512)
                for c in range(NOWN):
                    pb, rb = next_bank()
                    for k in range(16):
                        M(lambda e, pb=pb, wo=wo, k=k, c=c: e.matmul(pb[:, :], zT[:, k, c * 128:(c + 1) * 128], wo[:, k, :], start=(k == 0), stop=(k == 15)),
                          [rwo, r_zT], [rb])
                    V(lambda e, pb=pb, db=db: e.tensor_tensor(out=za[:], in0=pb[:], in1=gt1_bc[:, db * 512:(db + 1) * 512], op=ALU.mult),
                      [rb, r_gt1], [r_za])
                    G(lambda e, c=c, db=db: e.tensor_tensor(out=acc[:, c, db * 512:(db + 1) * 512], in0=acc[:, c, db * 512:(db + 1) * 512], in1=za[:], op=ALU.add),
                      [r_za, r_acc[c]], [r_acc[c]])
                _compact(CONSTS + [r_zT, r_gt1])
        P.barrier()

        if cfg.DEBUG:
            LD(dbg_out("dbg_hx", [128, NOWN, D], F32), acc[:], r_acc, [Res()])
            P.barrier()
        with contextlib.ExitStack() as s5b:
            s5b = Bump(98 * KB, ARENA)
            Mk_f = sb(s5b, "Mk_f", [128, NOWN, NE], F32); r_Mkf = Res()
            Mk_b = sb(s5b, "Mk_b", [128, NOWN, NE], BF16); r_Mkb = Res()
            triSU_b = sb(s5b, "triSU_b", [128, 128], BF16); r_triSU = Res()
            LDC(triSU_b[:], c_triSU, w=[r_triSU])
            A2_bc = sb(s5b, "A2_bc", [128, D], F32)
            sh2_bc = sb(s5b, "sh2_bc", [128, D], F32)
            g2b = sb(s5b, "g2b", [128, D], F32); r_g2b = Res()
            LD(g2b[:], bc_row(g2[0:1, :], D), w=[r_g2b])
            LD(sh2_bc[:], bc_row(mod_d[0:1, 3 * D:4 * D], D), [r_mod], [r_sh2])
            LD(A2_bc[:], bc_row(mod_d[0:1, 4 * D:5 * D], D), [r_mod], [r_A2])
            V(lambda e: e.scalar_tensor_tensor(out=A2_bc[:], in0=A2_bc[:], scalar=1.0, in1=g2b[:], op0=ALU.add, op1=ALU.mult),
              [r_A2, r_g2b], [r_A2])
            u2f = sb(s5b, "u2f", [128, D], F32); r_u2f = Res()
            u2b = sb(s5b, "u2b", [128, D], BF16); r_u2b = Res()
            u2Tf = sb(s5b, "u2Tf", [128, 4, 128], F32); r_u2Tf = Res()
            wr_f = sb(s5b, "wr_f", [128, DC, NE], F32); r_wr = Res()
            br_bc = sb(s5b, "br_bc", [128, NE], F32); r_br = Res()
            lg = sb(s5b, "lg", [128, NE], F32); r_lg = Res()
            mx8 = sb(s5b, "mx8", [128, 8], F32); r_mx8 = Res()
            nmx = sb(s5b, "nmx", [128, 1], F32); r_nmx = Res()
            ex = sb(s5b, "ex", [128, NE], F32); r_ex = Res()
            msk = sb(s5b, "msk", [128, NE], F32); r_msk = Res()
            esum = sb(s5b, "esum", [128, 1], F32); r_esum = Res()
            ss2 = sb(s5b, "ss2", [128, 1], F32); r_ss2 = Res()
            LD(wr_f[:], w_router.rearrange("(dc p) n -> p dc n", p=128), w=[r_wr])
            LD(br_bc[:], bc_row(b_router[0:1, :], NE), w=[r_br])
            for c in range(NOWN):
                G(lambda e: e.memset(ss2[:], 0.0), w=[r_ss2])
                A(lambda e, c=c: e.activation(out=u2b[:], in_=acc[:, c, :], func=AF.Square, accum_out=ss2[:]), [r_acc[c], r_ss2], [r_u2b, r_ss2])
                A(lambda e: e.activation(out=ss2[:], in_=ss2[:], func=AF.Sqrt, bias=epsb[:], scale=1.0 / D), [r_ss2, r_eps], [r_ss2])
                V(lambda e: e.reciprocal(out=ss2[:], in_=ss2[:]), [r_ss2], [r_ss2])
                V(lambda e, c=c: e.scalar_tensor_tensor(out=u2f[:], in0=acc[:, c, :], scalar=ss2[:, 0:1], in1=A2_bc[:], op0=ALU.mult, op1=ALU.mult),
                  [r_acc[c], r_ss2, r_A2], [r_u2f])
                V(lambda e: e.tensor_tensor(out=u2f[:], in0=u2f[:], in1=sh2_bc[:], op=ALU.add), [r_u2f, r_sh2], [r_u2f])
                G(lambda e, c=c: e.tensor_copy(out=u2tm[:, c, :], in_=u2f[:]), [r_u2f], [r_u2T])
                pl, rl = next_bank()
                for q4 in range(DC // 4):
                    pb, rb = next_bank()
                    if pb is pl:
                        pb, rb = next_bank()
                    for j in range(4):
                        dc = q4 * 4 + j
                        M(lambda e, pb=pb, dc=dc, j=j: e.transpose(pb[:, j * 128:(j + 1) * 128], u2f[:, dc * 128:(dc + 1) * 128], ident_f[:]),
                          [r_u2f, r_identf], [rb])
                    A(lambda e, pb=pb: e.activation(out=u2Tf[:], in_=pb[:].rearrange("p (j t) -> p j t", j=4), func=AF.Copy),
                      [rb], [r_u2Tf])
                    for j in range(4):
                        dc = q4 * 4 + j
                        M(lambda e, pl=pl, dc=dc, j=j: e.matmul(pl[:, 0:NE], u2Tf[:, j, :], wr_f[:, dc, :], start=(dc == 0), stop=(dc == DC - 1)),
                          [r_u2Tf, r_wr], [rl])
                V(lambda e, pl=pl: e.tensor_tensor(out=lg[:], in0=pl[:, 0:NE], in1=br_bc[:], op=ALU.add), [rl, r_br], [r_lg])
                V(lambda e: e.max(out=mx8[:], in_=lg[:]), [r_lg], [r_mx8])
                V(lambda e: e.tensor_scalar(out=nmx[:], in0=mx8[:, 0:1], scalar1=-1.0, scalar2=None, op0=ALU.mult), [r_mx8], [r_nmx])
                A(lambda e: e.activation(out=ex[:], in_=lg[:], func=AF.Exp, bias=nmx[:], scale=1.0), [r_lg, r_nmx], [r_ex])
                V(lambda e: e.tensor_scalar(out=msk[:], in0=lg[:], scalar1=mx8[:, cfg.TOPK - 1:cfg.TOPK], scalar2=None, op0=ALU.is_ge),
                  [r_lg, r_mx8], [r_msk])
                V(lambda e: e.tensor_tensor(out=ex[:], in0=ex[:], in1=msk[:], op=ALU.mult), [r_ex, r_msk], [r_ex])
                G(lambda e, c=c: e.tensor_copy(out=Mk_f[:, c, :], in_=msk[:]), [r_msk], [r_Mkf])
                G(lambda e, c=c: e.tensor_copy(out=Mk_b[:, c, :], in_=msk[:]), [r_msk], [r_Mkb])
                V(lambda e: e.tensor_reduce(out=esum[:], in_=ex[:], axis=mybir.AxisListType.X, op=ALU.add), [r_ex], [r_esum])
                V(lambda e: e.reciprocal(out=esum[:], in_=esum[:]), [r_esum], [r_esum])
                V(lambda e, c=c: e.tensor_scalar(out=Gd[:, c, :], in0=ex[:], scalar1=esum[:, 0:1], scalar2=None, op0=ALU.mult),
                  [r_ex, r_esum], [r_Gd])
                _compact(CONSTS + [r_wr, r_br, r_A2, r_sh2])
            for c in range(NOWN):
                pp, rp = next_bank()
                for c2 in range(c):
                    M(lambda e, pp=pp, c2=c2: e.matmul(pp[:, 0:NE], ones_b[:], Mk_b[:, c2, :], start=(c2 == 0), stop=False),
                      [r_onesb, r_Mkb], [rp])
                M(lambda e, pp=pp, c=c: e.matmul(pp[:, 0:NE], triSU_b[:], Mk_b[:, c, :], start=(c == 0), stop=True),
                  [r_triSU, r_Mkb], [rp])
                V(lambda e, pp=pp, c=c: e.scalar_tensor_tensor(out=posm[:, c, :], in0=pp[:, 0:NE], scalar=1.0, in1=Mk_f[:, c, :],
                                                               op0=ALU.add, op1=ALU.mult), [rp, r_Mkf], [r_posm])
            V(lambda e: e.tensor_scalar(out=posm[:], in0=posm[:], scalar1=-1.0, scalar2=None, op0=ALU.add), [r_posm], [r_posm])

        if cfg.DEBUG:
            LD(dbg_out("dbg_Gd", [128, NOWN, NE], F32), Gd[:], [r_Gd], [Res()])
            LD(dbg_out("dbg_u2tm", [128, NOWN, D], BF16), u2tm[:], [r_u2T], [Res()])
            LD(dbg_out("dbg_posm", [128, NOWN, NE], F32), posm[:], [r_posm], [Res()])
        P.barrier()
        if True:
            CAP = 512
            s6 = Bump(106 * KB, ARENA)
            LD(gt2_bc[:], bc_row(mod_d[0:1, 5 * D:6 * D], D), [r_mod], [r_gt2])
            iota_f = sb(s6, "iota_f", [128, CAP], F32); r_iota = Res()
            LD(iota_f[:], c_iota, w=[r_iota])
            STs = sb(s6, "STs", [128, CAP // 128, T], BF16); r_ST = Res()
            xT = sb(s6, "xT", [128, DC, CAP], BF16); r_xT = Res()
            hidT = sb(s6, "hidT", [128, FC, CAP], BF16); r_hid = Res()
            ys_off = s6.off
            Ys = sb(s6, "Ys", [128, CAP // 128, D], BF16); r_Ys = Res()
            Ssel = at(ys_off, [128, NOWN, CAP], BF16); r_S = r_Ys
            bgu = [sb(s6, f"bgu{i}", [128, 2 * FC], F32) for i in range(2)]
            rbgu = [Res(), Res()]
            bdn = [sb(s6, f"bdn{i}", [1, 512], BF16) for i in range(2)]
            rbdn = [Res(), Res()]
            bdn_i = [0]
            gg = sb(s6, "gg", [128, CAP], F32); r_gg = Res()
            sg = sb(s6, "sg", [128, CAP], BF16); r_sg = Res()
            uu = sb(s6, "uu", [128, CAP], BF16); r_uu = Res()
            jobs = []
            for ex_i in range(NE):
                for s in range(0, FC, 2):
                    jobs.append(("gu", ex_i, s))
                for db in range(4):
                    jobs.append(("dn", ex_i, db))

            def issue(job):
                kind, ex_i, k = job
                i0 = ring_i[0]
                ring_i[0] = (i0 + 1) % NR
                wt, wr = wring[i0], rring[i0]
                if kind == "gu":
                    vg = w_gu[ex_i][:, k * 128:(k + 2) * 128].rearrange("(dc p) n -> p dc n", p=128)
                    vu = w_gu[ex_i][:, DFF + k * 128:DFF + (k + 2) * 128].rearrange("(dc p) n -> p dc n", p=128)
                    LDC(wt[:, :, 0:256], vg, w=[wr])
                    P.dma("pool", lambda e: e.dma_start(out=wt[:, :, 256:512], in_=vu), [wr], [wr])
                else:
                    vd = w_dn[ex_i][:, k * 512:(k + 1) * 512].rearrange("(dc p) n -> p dc n", p=128)
                    LDC(wt[:, 0:FC, :], vd, w=[wr])
                return wt, wr

            def prologue(ex_i):
                pi = ex_i % 2
                P.dma("sp", lambda e: e.dma_start(out=bgu[pi][:], in_=b_gu[ex_i, :].rearrange("(j p) -> p j", p=128),
                                                  allow_slow_non_contiguous=True), [], [rbgu[pi]])
                for c in range(NOWN):
                    V(lambda e, c=c: e.tensor_scalar(out=Ssel[:, c, :], in0=iota_f[:], scalar1=posm[:, c, ex_i:ex_i + 1], scalar2=None,
                                                     op0=ALU.is_equal), [r_iota, r_posm], [r_S])
                for sbk in range(CAP // 128):
                    for c in range(NOWN):
                        M(lambda e, c=c, sbk=sbk: e.transpose(ptr[:, c * 128:(c + 1) * 128], Ssel[:, c, sbk * 128:(sbk + 1) * 128], ident_b[:]),
                          [r_S, r_identb], [r_ptr])
                    A(lambda e, sbk=sbk: e.activation(out=STs[:, sbk, :], in_=ptr[:, 0:T], func=AF.Copy), [r_ptr], [r_ST])
                for dc in range(DC):
                    pb, rb = next_bank()
                    for c in range(NOWN):
                        M(lambda e, pb=pb, dc=dc, c=c: e.matmul(pb[:, 0:CAP], u2tm[:, c, dc * 128:(dc + 1) * 128], Ssel[:, c, :],
                                                                start=(c == 0), stop=(c == NOWN - 1)), [r_u2T, r_S], [rb])
                    if dc % 2 == 0:
                        V(lambda e, pb=pb, dc=dc: e.tensor_copy(out=xT[:, dc, :], in_=pb[:, 0:CAP]), [rb], [r_xT])
                    else:
                        A(lambda e, pb=pb, dc=dc: e.activation(out=xT[:, dc, :], in_=pb[:, 0:CAP], func=AF.Copy), [rb], [r_xT])

            cur = issue(jobs[0])
            for ji, job in enumerate(jobs):
                nxt = issue(jobs[ji + 1]) if ji + 1 < len(jobs) else None
                kind, ex_i, k = job
                wt, wr = cur
                pi = ex_i % 2
                if kind == "gu" and k == 0:
                    prologue(ex_i)
                if kind == "gu":
                    for ii in range(2):
                        i = k + ii
                        pg, rg = next_bank()
                        pu, ru = next_bank()
                        for dc in range(DC):
                            M(lambda e, pg=pg, wt=wt, ii=ii, dc=dc: e.matmul(pg[:, 0:CAP], wt[:, dc, ii * 128:(ii + 1) * 128], xT[:, dc, :],
                                                                          start=(dc == 0), stop=(dc == DC - 1)), [wr, r_xT], [rg])
                        for dc in range(DC):
                            M(lambda e, pu=pu, wt=wt, ii=ii, dc=dc: e.matmul(pu[:, 0:CAP], wt[:, dc, 256 + ii * 128:256 + (ii + 1) * 128], xT[:, dc, :],
                                                                          start=(dc == 0), stop=(dc == DC - 1)), [wr, r_xT], [ru])
                        V(lambda e, pg=pg, i=i, pi=pi: e.tensor_scalar(out=gg[:], in0=pg[:, 0:CAP], scalar1=bgu[pi][:, i:i + 1], scalar2=7.0,
                                                                      op0=ALU.add, op1=ALU.min), [rg, rbgu[pi]], [r_gg])
                        A(lambda e: e.activation(out=sg[:], in_=gg[:], func=AF.Sigmoid, scale=1.702), [r_gg], [r_sg])
                        V(lambda e, pu=pu, i=i, pi=pi: e.tensor_scalar(out=uu[:], in0=pu[:, 0:CAP], scalar1=bgu[pi][:, FC + i:FC + i + 1], scalar2=7.0,
                                                                      op0=ALU.add, op1=ALU.min), [ru, rbgu[pi]], [r_uu])
                        V(lambda e: e.tensor_scalar(out=uu[:], in0=uu[:], scalar1=-7.0, scalar2=1.0, op0=ALU.max, op1=ALU.add),
                          [r_uu], [r_uu])
                        G(lambda e: e.tensor_tensor(out=gg[:], in0=gg[:], in1=sg[:], op=ALU.mult), [r_gg, r_sg], [r_gg])
                        G(lambda e, i=i: e.tensor_tensor(out=hidT[:, i, :], in0=gg[:], in1=uu[:], op=ALU.mult), [r_gg, r_uu], [r_hid])
                else:
                    db = k
                    bi_ = bdn_i[0]
                    bdn_i[0] = 1 - bi_
                    bd, rbd = bdn[bi_], rbdn[bi_]
                    LDC(bd[:], b_dn[ex_i:ex_i + 1, db * 512:(db + 1) * 512], w=[rbd])
                    for sbk in range(CAP // 128):
                        pb, rb = next_bank()
                        for i in range(FC):
                            M(lambda e, pb=pb, wt=wt, i=i, sbk=sbk: e.matmul(pb[:, :], hidT[:, i, sbk * 128:(sbk + 1) * 128], wt[:, i, :], start=(i == 0), stop=False),
                              [wr, r_hid], [rb])
                        M(lambda e, pb=pb, bd=bd: e.matmul(pb[:, :], ones_b[0:1, :], bd[0:1, :], start=False, stop=True),
                          [r_onesb, rbd], [rb])
                        V(lambda e, pb=pb, sbk=sbk, db=db: e.tensor_tensor(out=Ys[:, sbk, db * 512:(db + 1) * 512], in0=pb[:], in1=gt2_bc[:, db * 512:(db + 1) * 512],
                                                                          op=ALU.mult), [rb, r_gt2], [r_Ys])
                    for c in range(NOWN):
                        pb, rb = next_bank()
                        for sbk in range(CAP // 128):
                            M(lambda e, pb=pb, sbk=sbk, c=c, db=db: e.matmul(pb[:, :], STs[:, sbk, c * 128:(c + 1) * 128], Ys[:, sbk, db * 512:(db + 1) * 512],
                                                                            start=(sbk == 0), stop=(sbk == CAP // 128 - 1)), [r_ST, r_Ys], [rb])
                        V(lambda e, pb=pb, c=c, ex_i=ex_i, db=db: e.scalar_tensor_tensor(out=acc[:, c, db * 512:(db + 1) * 512], in0=pb[:], scalar=Gd[:, c, ex_i:ex_i + 1],
                                                                                       in1=acc[:, c, db * 512:(db + 1) * 512], op0=ALU.mult, op1=ALU.add),
                          [rb, r_Gd, r_acc[c]], [r_acc[c]])
                _compact(CONSTS + [r_u2T, r_Gd, r_gt2, r_posm, r_iota])
                cur = nxt

        r_out = Res("out")
        for c in range(NOWN):
            LD(out_d[c * 128:(c + 1) * 128, :], acc[:, c, :], [r_acc[c]], [r_out])
        P.finish()
    return nc


def _consts(cfg, h):
    NT = cfg.NCTX + cfg.NOTH + cfg.NOWN
    TA = NT * 128
    ident = np.eye(128, dtype=np.float32)
    jj, tt = np.meshgrid(np.arange(128), np.arange(128), indexing="ij")
    triU = (jj <= tt).astype(np.float32)
    triL = (jj >= tt).astype(np.float32)
    rot = np.zeros((128, 128), np.float32)
    for i in range(64):
        rot[2 * i + 1, 2 * i] = -1.0
        rot[2 * i, 2 * i + 1] = 1.0
    nctx = cfg.NCTX * 128
    seq = cfg.SEQ
    pos = np.arange(seq)
    if h == 0:
        pos = pos[::-1]
    rows = (pos // cfg.GRID_W).astype(np.float32)
    cols = (pos % cfg.GRID_W).astype(np.float32)
    freqs = np.exp(-math.log(10000.0) * np.arange(32, dtype=np.float32) / 32).astype(np.float32)
    ang = np.concatenate([rows[:, None] * freqs, cols[:, None] * freqs], axis=-1).astype(np.float32)
    cos = np.repeat(np.cos(ang), 2, axis=1).T
    sin = np.repeat(np.sin(ang), 2, axis=1).T
    cosT = np.concatenate([np.ones((128, nctx), np.float32), cos.astype(np.float32)], axis=1)
    sinT = np.concatenate([np.zeros((128, nctx), np.float32), sin.astype(np.float32)], axis=1)
    assert cosT.shape[1] == TA
    triSU = (jj < tt).astype(np.float32)
    iota = np.ascontiguousarray(np.broadcast_to(np.arange(512, dtype=np.float32)[None, :], (128, 512)))
    return dict(c_ident=ident, c_triU=triU, c_triL=triL, c_rot=rot, c_triSU=triSU, c_iota=iota,
                c_cos=np.ascontiguousarray(cosT), c_sin=np.ascontiguousarray(sinT))


def make_in_maps(cfg, inp):
    f = lambda a: np.ascontiguousarray(np.asarray(a, dtype=np.float32))
    x, c, ctx, c_ctx = f(inp["x"]), f(inp["c"]), f(inp["ctx"]), f(inp["c_ctx"])
    B = x.shape[0]
    shared = dict(
        w_mod=f(inp["w_mod"][0]), b_mod=f(inp["b_mod"][0])[None, :], g1=f(inp["g_norm1"][0])[None, :],
        g2=f(inp["g_norm2"][0])[None, :], w_in=f(inp["w_in"][0]), b_in=f(inp["b_in"][0])[None, :],
        g_q=f(inp["g_q"][0])[:, None], g_k=f(inp["g_k"][0])[:, None], g_mh=f(inp["g_mh"][0])[None, :],
        w_br_m=f(inp["w_br_m"][0]), w_br_a=f(inp["w_br_a"][0]), w_out=f(inp["w_out"][0]),
        w_router=f(inp["w_router"][0]), b_router=f(inp["b_router"][0])[None, :],
        w_gu=f(inp["w_gu"][0]), b_gu=f(inp["b_gu"][0]), w_dn=f(inp["w_dn"][0]), b_dn=f(inp["b_dn"][0]),
    )
    wgt = shared["w_in"][:, OFF_GT:OFF_GT + 16]
    bgt = shared["b_in"][0, OFF_GT:OFF_GT + 16]
    perm = {1: list(range(0, 4)) + list(range(8, 12)) + list(range(4, 8)) + list(range(12, 16)),
            0: list(range(8, 12)) + list(range(0, 4)) + list(range(12, 16)) + list(range(4, 8))}
    half = cfg.SEQ // 2
    maps = []
    for core in range(2 * B):
        b, h = core // 2, core % 2
        if h == 1:
            xfm = np.concatenate([ctx[b], x[b]], axis=0)
        else:
            xfm = np.concatenate([ctx[b, ::-1], x[b, ::-1]], axis=0)
        m = dict(shared)
        m["xf"] = np.ascontiguousarray(xfm)
        m["cc"] = np.ascontiguousarray(np.stack([c[b], c_ctx], axis=0))
        m["wg16"] = np.ascontiguousarray(wgt[:, perm[h]])
        m["bg16"] = np.ascontiguousarray(bgt[perm[h]])[None, :]
        m.update(_consts(cfg, h))
        maps.append(m)
    return maps


def assemble(cfg, results, B):
    half = cfg.SEQ // 2
    out = np.zeros((B, cfg.SEQ, cfg.D), np.float32)
    for core in range(2 * B):
        b, h = core // 2, core % 2
        o = np.asarray(results[core]["out"], dtype=np.float32)
        if h == 1:
            out[b, half:] = o
        else:
            out[b, :half] = o[::-1]
    return out


def kernel(**inputs):
    cfg = Cfg()
    nc = build(cfg)
    maps = make_in_maps(cfg, inputs)
    res = run_bass_kernel_spmd(nc, maps, core_ids=list(range(8)))
    return assemble(cfg, res.results, 4)
```

```python
import contextlib
import math
import numpy as np
import concourse.bass as bass
import concourse.mybir as mybir
from concourse.bass_utils import run_bass_kernel_spmd

F32 = mybir.dt.float32
BF16 = mybir.dt.bfloat16
ALU = mybir.AluOpType
AF = mybir.ActivationFunctionType

COMPUTE = ("pe", "dve", "act", "pool")
NDMA = {"sp": 8, "pool": 4}


class Res:
    __slots__ = ("name", "w", "rd")

    def __init__(self, name=""):
        self.name = name
        self.w = None
        self.rd = []


class Prog:
    def __init__(self, nc, es):
        self.nc = nc
        self.es = es
        self.ops = {e: [] for e in ("pe", "dve", "act", "pool", "sp")}
        self.cnt = {e: 0 for e in COMPUTE}
        self.sem = {e: es.enter_context(nc.semaphore("s_" + e)) for e in COMPUTE}
        self.known = {e: {} for e in ("pe", "dve", "act", "pool", "sp")}
        self.snaps = {e: [dict()] for e in COMPUTE}
        self.dsem = {}
        self.duse = {}
        self.drot = {}
        for q, n in NDMA.items():
            for i in range(n):
                self.dsem[(q, i)] = es.enter_context(nc.semaphore(f"d_{q}{i}"))
                self.duse[(q, i)] = 0
            self.drot[q] = 0

    def semh(self, key):
        return self.sem[key] if key in self.sem else self.dsem[key]

    def _need(self, eng, ev, waits):
        if ev is None:
            return
        key, val = ev
        if self.known[eng].get(key, 0) >= val:
            return
        if waits.get(key, 0) < val:
            waits[key] = val

    def _learn(self, eng, key, val):
        kn = self.known[eng]
        if kn.get(key, 0) < val:
            kn[key] = val
        if key in self.snaps:
            for k2, v2 in self.snaps[key][val].items():
                if kn.get(k2, 0) < v2:
                    kn[k2] = v2

    def _deps(self, eng, reads, writes):
        waits = {}
        for r in reads:
            self._need(eng, r.w, waits)
        for r in writes:
            self._need(eng, r.w, waits)
            for ev in r.rd:
                self._need(eng, ev, waits)
        return waits

    def op(self, eng, fn, reads=(), writes=()):
        waits = self._deps(eng, reads, writes)
        if eng in waits:
            own_raw = 0
            for r in reads:
                if r.w is not None and r.w[0] == eng:
                    own_raw = max(own_raw, r.w[1])
            if eng != "pe" and own_raw > self.known[eng].get(eng, 0):
                waits[eng] = own_raw
            else:
                del waits[eng]
        for k, v in waits.items():
            self._learn(eng, k, v)
        self.cnt[eng] += 1
        n = self.cnt[eng]
        self.ops[eng].append((list(waits.items()), fn, (eng, 1)))
        self.snaps[eng].append(dict(self.known[eng]))
        ev = (eng, n)
        for r in reads:
            if len(r.rd) > 48:
                _compact([r])
            r.rd.append(ev)
        for r in writes:
            r.w = ev
            r.rd = []
        return ev

    def dma(self, q, fn, reads=(), writes=()):
        waits = self._deps(q, reads, writes)
        i = self.drot[q]
        self.drot[q] = (i + 1) % NDMA[q]
        key = (q, i)
        prev = self.duse[key]
        if prev > 0 and self.known[q].get(key, 0) < prev:
            if waits.get(key, 0) < prev:
                waits[key] = prev
        for k, v in waits.items():
            self._learn(q, k, v)
        self.duse[key] = prev + 16
        self.ops[q].append((list(waits.items()), fn, (key, 16)))
        ev = (key, prev + 16)
        for r in reads:
            r.rd.append(ev)
        for r in writes:
            r.w = ev
            r.rd = []
        return ev

    def barrier(self):
        targets = {e: self.cnt[e] for e in COMPUTE if self.cnt[e] > 0}
        targets.update({k: v for k, v in self.duse.items() if v > 0})
        for e in ("pe", "dve", "act", "pool", "sp"):
            waits = {k: v for k, v in targets.items() if k != e and self.known[e].get(k, 0) < v}
            for k, v in waits.items():
                self._learn(e, k, v)
            if waits:
                self.ops[e].append((list(waits.items()), None, None))

    def finish(self):
        nc = self.nc
        waits = {}
        for key, v in self.duse.items():
            if v > 0:
                waits[key] = v
        for e in COMPUTE:
            if self.cnt[e] > 0:
                waits[e] = self.cnt[e]
        self.ops["sp"].append((list(waits.items()), None, None))
        hmap = {"pe": "tensor", "dve": "vector", "act": "scalar", "pool": "gpsimd", "sp": "sync"}
        with nc.Block() as block:
            for e, attr in hmap.items():
                ops = self.ops[e]
                if not ops:
                    continue

                def section(engh, ops=ops):
                    for waits, fn, inc in ops:
                        for k, v in waits:
                            engh.wait_ge(self.semh(k), v)
                        if fn is None:
                            continue
                        inst = fn(engh)
                        if inc is not None:
                            inst.then_inc(self.semh(inc[0]), inc[1])

                getattr(block, attr)(section)


def _compact(res_list):
    for r in res_list:
        d = {}
        for k, v in r.rd:
            if d.get(k, 0) < v:
                d[k] = v
        r.rd = list(d.items())


class Cfg:
    D = 2048
    NCTX = 2
    NOTH = 8
    NOWN = 8
    NE = 32
    DFF = 2048
    SEQ = 2048
    GRID_W = 64
    TOPK = 4
    DEBUG = False


HD = 128
MH = 4
MQK = 256
MV = 512
EPS = 1e-6
F_IN = 13328
OFF_MQ, OFF_MK, OFF_MV, OFF_OG, OFF_GT, OFF_AQ, OFF_AK, OFF_AV, OFF_MG = (
    0, 1024, 2048, 4096, 6144, 6160, 8208, 8720, 9232)


def build(cfg):
    D = cfg.D
    DC = D // 128
    NT = cfg.NCTX + cfg.NOTH + cfg.NOWN
    NOWN = cfg.NOWN
    C0 = cfg.NCTX + cfg.NOTH
    T = NOWN * 128
    TA = NT * 128
    TB = min(512, T)
    NB = T // TB
    NE = cfg.NE
    DFF = cfg.DFF
    FC = DFF // 128

    nc = bass.Bass("TRN2", target_bir_lowering=False)

    def din(name, shape):
        return nc.dram_tensor(name, list(shape), F32, kind="ExternalInput").ap()

    xf = din("xf", [TA, D])
    cc = din("cc", [2, D])
    w_mod = din("w_mod", [D, 6 * D])
    b_mod = din("b_mod", [1, 6 * D])
    g1 = din("g1", [1, D])
    g2 = din("g2", [1, D])
    w_in = din("w_in", [D, F_IN])
    b_in = din("b_in", [1, F_IN])
    wg16 = din("wg16", [D, 16])
    bg16 = din("bg16", [1, 16])
    g_q = din("g_q", [128, 1])
    g_k = din("g_k", [128, 1])
    g_mh = din("g_mh", [1, MH * MV])
    w_br_m = din("w_br_m", [D, D])
    w_br_a = din("w_br_a", [D, D])
    w_out = din("w_out", [D, D])
    w_router = din("w_router", [D, NE])
    b_router = din("b_router", [1, NE])
    w_gu = din("w_gu", [NE, D, 2 * DFF])
    b_gu = din("b_gu", [NE, 2 * DFF])
    w_dn = din("w_dn", [NE, DFF, D])
    b_dn = din("b_dn", [NE, D])
    c_ident = din("c_ident", [128, 128])
    c_triU = din("c_triU", [128, 128])
    c_triL = din("c_triL", [128, 128])
    c_rot = din("c_rot", [128, 128])
    c_triSU = din("c_triSU", [128, 128])
    c_iota = din("c_iota", [128, 512])
    c_cos = din("c_cos", [128, TA])
    c_sin = din("c_sin", [128, TA])
    out_d = nc.dram_tensor("out", [T, D], F32, kind="ExternalOutput").ap()

    def scratch(name, shape, dt=BF16):
        if cfg.DEBUG:
            return nc.dram_tensor(name, list(shape), dt, kind="ExternalOutput").ap()
        return nc.dram_tensor(name, list(shape), dt).ap()

    def dbg_out(name, shape, dt):
        return nc.dram_tensor(name, list(shape), dt, kind="ExternalOutput").ap()

    mod_d = scratch("mod_d", [2, 6 * D], F32)
    MQT = scratch("MQT", [8, 128, T])
    MKT = scratch("MKT", [8, 128, T])
    AQT = scratch("AQT", [16, 128, T])
    GMT = scratch("GMT", [32, 128, T])
    MKd = scratch("MKd", [NT, 128, MH * MQK])
    MVd = scratch("MVd", [NT, 128, MH * MV])
    OGd = scratch("OGd", [NOWN, 128, MH * MV])
    AVd = scratch("AVd", [NT, 128, 4 * HD])

    with contextlib.ExitStack() as es:
        P = Prog(nc, es)

        KB = 1024
        ARENA = 171 * KB
        arena = es.enter_context(nc.sbuf_tensor("arena", [128, ARENA // 4], F32))

        class Bump:
            def __init__(self, off, limit):
                self.off = off
                self.limit = limit

        def sb(stack, name, shape, dt):
            if isinstance(stack, Bump):
                esz = 4 if dt == F32 else 2
                n = int(np.prod(shape[1:]))
                nbytes = (n * esz + 31) // 32 * 32
                assert stack.off + nbytes <= stack.limit, (name, stack.off, nbytes, stack.limit)
                a = arena[0:shape[0], stack.off // 4:(stack.off + nbytes) // 4]
                stack.off += nbytes
                v = a if dt == F32 else a.bitcast(BF16)
                v = v[:, 0:n]
                if len(shape) == 3:
                    v = v.rearrange("p (a b) -> p a b", a=shape[1])
                return v
            return stack.enter_context(nc.sbuf_tensor(name, list(shape), dt))

        def at(off, shape, dt):
            return sb(Bump(off, ARENA), "x", shape, dt)

        def V(fn, r=(), w=()):
            return P.op("dve", fn, r, w)

        def A(fn, r=(), w=()):
            return P.op("act", fn, r, w)

        def G(fn, r=(), w=()):
            return P.op("pool", fn, r, w)

        def M(fn, r=(), w=()):
            return P.op("pe", fn, r, w)

        def LD(out, in_, r=(), w=(), slow=False):
            if slow:
                return P.dma("sp", lambda e: e.dma_start(out=out, in_=in_, allow_slow_non_contiguous=True), r, w)
            return P.dma("sp", lambda e: e.dma_start(out=out, in_=in_), r, w)

        def LDC(out, in_, r=(), w=(), slow=False):
            if slow:
                return P.dma("pool", lambda e: e.dma_start(out=out, in_=in_, allow_slow_non_contiguous=True), r, w)
            return P.dma("pool", lambda e: e.dma_start(out=out, in_=in_), r, w)

        pbank = [es.enter_context(nc.psum_tensor(f"pb{i}", [128, 512], F32)) for i in range(7)]
        rbank = [Res(f"pb{i}") for i in range(7)]
        ptr = es.enter_context(nc.psum_tensor("ptr", [128, 1024], BF16))
        r_ptr = Res("ptr")

        ident_f = sb(es, "ident_f", [128, 128], F32); r_identf = Res()
        ident_b = sb(es, "ident_b", [128, 128], BF16); r_identb = Res()
        triU = sb(es, "triU", [128, 128], F32); r_triU = Res()
        triL = sb(es, "triL", [128, 128], F32); r_triL = Res()
        ones_f = sb(es, "ones_f", [128, 128], F32); r_onesf = Res()
        ones_b = sb(es, "ones_b", [128, 128], BF16); r_onesb = Res()
        rot_b = sb(es, "rot_b", [128, 128], BF16); r_rot = Res()
        epsb = sb(es, "epsb", [128, 1], F32); r_eps = Res()
        CONSTS = [r_identf, r_identb, r_triU, r_triL, r_onesf, r_onesb, r_rot, r_eps]
        LD(ident_f[:], c_ident, w=[r_identf])
        LDC(ident_b[:], c_ident, w=[r_identb])
        LD(triU[:], c_triU, w=[r_triU])
        LD(triL[:], c_triL, w=[r_triL])
        LDC(rot_b[:], c_rot, w=[r_rot])
        G(lambda e: e.memset(ones_f[:], 1.0), w=[r_onesf])
        G(lambda e: e.memset(ones_b[:], 1.0), w=[r_onesb])
        G(lambda e: e.memset(epsb[:], EPS), w=[r_eps])

        r_gt1, r_gt2, r_A2, r_sh2 = Res(), Res(), Res(), Res()

        NR = 2
        wring = [sb(es, f"wring{i}", [128, DC, 512], BF16) for i in range(NR)]
        rring = [Res(f"wring{i}") for i in range(NR)]
        ring_i = [0]

        def load_w(src2d, ncols):
            i = ring_i[0]
            ring_i[0] = (i + 1) % NR
            t, r = wring[i], rring[i]
            half = DC // 2
            v = src2d.rearrange("(dc p) n -> p dc n", p=128)
            LDC(t[:, 0:half, 0:ncols], v[:, 0:half, :], w=[r])
            P.dma("pool", lambda e: e.dma_start(out=t[:, half:DC, 0:ncols], in_=v[:, half:DC, :]), [r], [r])
            return t, r

        bank_i = [0]

        def next_bank():
            i = bank_i[0]
            bank_i[0] = (i + 1) % 7
            return pbank[i], rbank[i]

        NS = 4
        stg = []
        rstg = [Res(f"stg{i}") for i in range(NS)]
        stg_i = [0]

        def next_stg():
            i = stg_i[0]
            stg_i[0] = (i + 1) % NS
            return stg[i], rstg[i]

        r_mod = Res("mod_d")
        r_MQT, r_MKT, r_AQT, r_GMT = Res(), Res(), Res(), Res()
        r_MKd, r_MVd, r_OGd, r_AVd = Res(), Res(), Res(), Res()

        with contextlib.ExitStack() as s0:
            s0 = Bump(90 * KB, ARENA)
            cT_f = sb(s0, "cT_f", [128, 2, DC], F32); r_cTf = Res()
            cT_b = sb(s0, "cT_b", [128, DC, 2], BF16); r_cTb = Res()
            bm2 = sb(s0, "bm2", [2, 512], F32); r_bm2 = Res()
            mrow = sb(s0, "mrow", [2, 512], F32); r_mrow = Res()
            for r_ in range(2):
                P.dma("sp", lambda e, r_=r_: e.dma_start(out=cT_f[:, r_, :], in_=cc[r_, :].rearrange("(dc p) -> p dc", p=128),
                                                         allow_slow_non_contiguous=True), [r_cTf], [r_cTf])
            for r_ in range(2):
                A(lambda e, r_=r_: e.activation(out=cT_b[:, :, r_], in_=cT_f[:, r_, :], func=AF.Silu), [r_cTf, r_cTb], [r_cTb])
            for blk in range(6 * D // 512):
                wt, wr = load_w(w_mod[:, blk * 512:(blk + 1) * 512], 512)
                pb, rb = next_bank()
                for dc in range(DC):
                    M(lambda e, dc=dc, pb=pb, wt=wt: e.matmul(pb[0:2, :], cT_b[:, dc, :], wt[:, dc, :],
                                                                start=(dc == 0), stop=(dc == DC - 1)),
                      [r_cTb, wr], [rb])
                LD(bm2[0:1, :], b_mod[0:1, blk * 512:(blk + 1) * 512], w=[r_bm2])
                P.dma("sp", lambda e, blk=blk: e.dma_start(out=bm2[1:2, :], in_=b_mod[0:1, blk * 512:(blk + 1) * 512]),
                      [r_bm2], [r_bm2])
                V(lambda e, pb=pb: e.tensor_tensor(out=mrow[:], in0=pb[0:2, :], in1=bm2[:], op=ALU.add),
                  [rb, r_bm2], [r_mrow])
                LD(mod_d[:, blk * 512:(blk + 1) * 512], mrow[:], [r_mrow], [r_mod])

        def bc_row(ap_row, n):
            return ap_row.broadcast_to([128, n])

        P.barrier()

        uT = at(0, [128, DC, TA], BF16)
        r_uT = [Res(f"uT{c}") for c in range(NT)]
        with contextlib.ExitStack() as s1:
            s1 = Bump(72 * KB, ARENA)
            A1x = sb(s1, "A1x", [128, D], F32); r_A1x = Res()
            S1x = sb(s1, "S1x", [128, D], F32); r_S1x = Res()
            A1c = sb(s1, "A1c", [128, D], F32); r_A1c = Res()
            S1c = sb(s1, "S1c", [128, D], F32); r_S1c = Res()
            g1b = sb(s1, "g1b", [128, D], F32); r_g1b = Res()
            LD(g1b[:], bc_row(g1[0:1, :], D), w=[r_g1b])
            LD(A1x[:], bc_row(mod_d[0:1, D:2 * D], D), [r_mod], [r_A1x])
            LD(A1c[:], bc_row(mod_d[1:2, D:2 * D], D), [r_mod], [r_A1c])
            LD(S1x[:], bc_row(mod_d[0:1, 0:D], D), [r_mod], [r_S1x])
            LD(S1c[:], bc_row(mod_d[1:2, 0:D], D), [r_mod], [r_S1c])
            V(lambda e: e.scalar_tensor_tensor(out=A1x[:], in0=A1x[:], scalar=1.0, in1=g1b[:], op0=ALU.add, op1=ALU.mult),
              [r_A1x, r_g1b], [r_A1x])
            V(lambda e: e.scalar_tensor_tensor(out=A1c[:], in0=A1c[:], scalar=1.0, in1=g1b[:], op0=ALU.add, op1=ALU.mult),
              [r_A1c, r_g1b], [r_A1c])
            xt = [sb(s1, f"xt{i}", [128, D], F32) for i in range(2)]
            rxt = [Res(), Res()]
            xn = [sb(s1, f"xn{i}", [128, D], BF16) for i in range(2)]
            rxn = [Res(), Res()]
            ss = sb(s1, "ss", [128, 2], F32); r_ss = [Res(), Res()]
            for c in range(NT):
                i = c % 2
                Aw, rA, Sw, rS = (A1c, r_A1c, S1c, r_S1c) if c < cfg.NCTX else (A1x, r_A1x, S1x, r_S1x)
                LD(xt[i][:], xf[c * 128:(c + 1) * 128, :], w=[rxt[i]])
                G(lambda e, i=i: e.memset(ss[:, i:i + 1], 0.0), w=[r_ss[i]])
                A(lambda e, i=i: e.activation(out=xn[i][:], in_=xt[i][:], func=AF.Square, accum_out=ss[:, i:i + 1]),
                  [rxt[i], r_ss[i]], [rxn[i], r_ss[i]])
                A(lambda e, i=i: e.activation(out=ss[:, i:i + 1], in_=ss[:, i:i + 1], func=AF.Sqrt, bias=epsb[:], scale=1.0 / D),
                  [r_ss[i], r_eps], [r_ss[i]])
                V(lambda e, i=i: e.reciprocal(out=ss[:, i:i + 1], in_=ss[:, i:i + 1]), [r_ss[i]], [r_ss[i]])
                V(lambda e, i=i, Aw=Aw: e.scalar_tensor_tensor(out=xt[i][:], in0=xt[i][:], scalar=ss[:, i:i + 1], in1=Aw[:],
                                                              op0=ALU.mult, op1=ALU.mult),
                  [rxt[i], r_ss[i], rA], [rxt[i]])
                G(lambda e, i=i, Sw=Sw: e.tensor_tensor(out=xn[i][:], in0=xt[i][:], in1=Sw[:], op=ALU.add),
                  [rxt[i], rS], [rxn[i]])
                for half in range(DC // 8):
                    for j in range(8):
                        dc = half * 8 + j
                        M(lambda e, i=i, dc=dc, j=j: e.transpose(ptr[:, j * 128:(j + 1) * 128], xn[i][:, dc * 128:(dc + 1) * 128], ident_b[:]),
                          [rxn[i], r_identb], [r_ptr])
                    if half % 2 == 0:
                        V(lambda e, c=c, half=half: e.tensor_copy(
                            out=uT[:, half * 8:(half + 1) * 8, c * 128:(c + 1) * 128],
                            in_=ptr[:].rearrange("p (j t) -> p j t", j=8)), [r_ptr], [r_uT[c]])
                    else:
                        A(lambda e, c=c, half=half: e.activation(
                            out=uT[:, half * 8:(half + 1) * 8, c * 128:(c + 1) * 128],
                            in_=ptr[:].rearrange("p (j t) -> p j t", j=8), func=AF.Copy), [r_ptr], [r_uT[c]])
                _compact(CONSTS)

        own_blocks = [(C0 * 128 + b * TB, TB) for b in range(NB)]
        all_blocks = []
        o = 0
        while o < TA:
            n = min(512, TA - o)
            all_blocks.append((o, n))
            o += n

        def uT_res(t0, n):
            return r_uT[t0 // 128:(t0 + n + 127) // 128]

        if cfg.DEBUG:
            P.barrier()
            LD(dbg_out("dbg_uT", [128, DC, TA], BF16), uT[:], r_uT, [Res()])
        P.barrier()
        gates = at(160 * KB, [128, NT, 16], F32); r_gates = Res()
        AKT = at(72 * KB, [128, 4, TA], BF16); r_AKT = Res()

        with contextlib.ExitStack() as s2:
            s2 = Bump(90 * KB, 160 * KB)
            stg.extend(sb(s2, f"stg{i}", [128, 512], BF16) for i in range(NS))
            bT = sb(s2, "bT", [128, 72], F32); r_bT = Res()
            for (off, n, col) in ((OFF_MQ, 8, 0), (OFF_MK, 8, 8), (OFF_AQ, 16, 16), (OFF_AK, 4, 32), (OFF_MG, 32, 36)):
                P.dma("sp", lambda e, off=off, n=n, col=col: e.dma_start(
                    out=bT[:, col:col + n], in_=b_in[0, off:off + n * 128].rearrange("(j p) -> p j", p=128),
                    allow_slow_non_contiguous=True), [r_bT], [r_bT])
            brows = [sb(s2, f"brow{i}", [1, 512], BF16) for i in range(2)]
            rbrows = [Res(), Res()]
            brow_i = [0]
            bg_bc = sb(s2, "bg_bc", [128, 16], F32); r_bg = Res()
            LD(bg_bc[:], bc_row(bg16[0:1, :], 16), w=[r_bg])
            wg_b = sb(s2, "wg_b", [128, DC, 16], BF16); r_wg = Res()
            LDC(wg_b[:], wg16.rearrange("(dc p) n -> p dc n", p=128), w=[r_wg])
            gq_s = sb(s2, "gq_s", [128, 1], F32); r_gq = Res()
            gk_s = sb(s2, "gk_s", [128, 1], F32); r_gk = Res()
            LD(gq_s[:], g_q, w=[r_gq])
            LD(gk_s[:], g_k, w=[r_gk])
            cos_s = sb(s2, "cos_s", [128, TA], BF16); r_cos = Res()
            sin_s = sb(s2, "sin_s", [128, TA], BF16); r_sin = Res()
            LDC(cos_s[:], c_cos, w=[r_cos])
            LDC(sin_s[:], c_sin, w=[r_sin])
            sq = sb(s2, "sq", [128, 512], BF16); r_sq = Res()
            rstd = sb(s2, "rstd", [128, 512], F32); r_rstd = Res()
            xnb = sb(s2, "xnb", [128, 512], BF16); r_xnb = Res()
            t1 = sb(s2, "t1", [128, 512], F32); r_t1 = Res()
            t2 = sb(s2, "t2", [128, 512], F32); r_t2 = Res()

            def fm_group(off, nchunks, blocks, evac):
                for s in range(0, nchunks, 4):
                    ncol = min(4, nchunks - s) * 128
                    wt, wr = load_w(w_in[:, off + s * 128: off + s * 128 + ncol], ncol)
                    for jj in range(ncol // 128):
                        j = s + jj
                        for bi, (t0, n) in enumerate(blocks):
                            pb, rb = next_bank()
                            for dc in range(DC):
                                M(lambda e, dc=dc, pb=pb, wt=wt, jj=jj, t0=t0, n=n: e.matmul(
                                    pb[:, 0:n], wt[:, dc, jj * 128:(jj + 1) * 128], uT[:, dc, t0:t0 + n],
                                    start=(dc == 0), stop=(dc == DC - 1)),
                                  [wr] + uT_res(t0, n), [rb])
                            evac(j, bi, t0, n, pb, rb)
                    _compact(CONSTS + r_uT)

            def simple_evac(dst, rdst, bcol, scale):
                def f(j, bi, t0, n, pb, rb):
                    st, rs = next_stg()
                    V(lambda e: e.tensor_scalar(out=st[:, 0:n], in0=pb[:, 0:n], scalar1=bT[:, bcol + j:bcol + j + 1],
                                                scalar2=scale, op0=ALU.add, op1=ALU.mult), [rb, r_bT], [rs])
                    tl = t0 - C0 * 128
                    LD(dst[j, :, tl:tl + n], st[:, 0:n], [rs], [rdst])
                return f

            def sig_evac(dst, rdst, bcol):
                def f(j, bi, t0, n, pb, rb):
                    st, rs = next_stg()
                    A(lambda e: e.activation(out=st[:, 0:n], in_=pb[:, 0:n], func=AF.Sigmoid,
                                             bias=bT[:, bcol + j:bcol + j + 1], scale=1.0), [rb, r_bT], [rs])
                    tl = t0 - C0 * 128
                    LD(dst[j, :, tl:tl + n], st[:, 0:n], [rs], [rdst])
                return f

            def qknorm_evac(is_q):
                bcol = 16 if is_q else 32
                gs, rg = (gq_s, r_gq) if is_q else (gk_s, r_gk)

                def f(j, bi, t0, n, pb, rb):
                    V(lambda e: e.tensor_scalar(out=t1[:, 0:n], in0=pb[:, 0:n], scalar1=bT[:, bcol + j:bcol + j + 1],
                                                scalar2=None, op0=ALU.add), [rb, r_bT], [r_t1])
                    A(lambda e: e.activation(out=sq[:, 0:n], in_=t1[:, 0:n], func=AF.Square), [r_t1], [r_sq])
                    p2, r2 = next_bank()
                    M(lambda e: e.matmul(p2[:, 0:n], ones_b[:], sq[:, 0:n], start=True, stop=True), [r_onesb, r_sq], [r2])
                    A(lambda e: e.activation(out=rstd[:, 0:n], in_=p2[:, 0:n], func=AF.Sqrt, bias=epsb[:], scale=1.0 / HD),
                      [r2, r_eps], [r_rstd])
                    V(lambda e: e.reciprocal(out=rstd[:, 0:n], in_=rstd[:, 0:n]), [r_rstd], [r_rstd])
                    V(lambda e: e.scalar_tensor_tensor(out=xnb[:, 0:n], in0=t1[:, 0:n], scalar=gs[:, 0:1], in1=rstd[:, 0:n],
                                                       op0=ALU.mult, op1=ALU.mult), [r_t1, rg, r_rstd], [r_xnb])
                    p3, r3 = next_bank()
                    M(lambda e: e.matmul(p3[:, 0:n], rot_b[:], xnb[:, 0:n], start=True, stop=True), [r_rot, r_xnb], [r3])
                    V(lambda e: e.tensor_tensor(out=t2[:, 0:n], in0=p3[:, 0:n], in1=sin_s[:, t0:t0 + n], op=ALU.mult),
                      [r3, r_sin], [r_t2])
                    G(lambda e: e.tensor_tensor(out=t1[:, 0:n], in0=xnb[:, 0:n], in1=cos_s[:, t0:t0 + n], op=ALU.mult),
                      [r_xnb, r_cos], [r_t1])
                    if is_q:
                        st, rs = next_stg()
                        G(lambda e: e.tensor_tensor(out=st[:, 0:n], in0=t1[:, 0:n], in1=t2[:, 0:n], op=ALU.add),
                          [r_t1, r_t2], [rs])
                        tl = t0 - C0 * 128
                        LD(AQT[j, :, tl:tl + n], st[:, 0:n], [rs], [r_AQT])
                    else:
                        G(lambda e: e.tensor_tensor(out=AKT[:, j, t0:t0 + n], in0=t1[:, 0:n], in1=t2[:, 0:n], op=ALU.add),
                          [r_t1, r_t2], [r_AKT])
                return f

            fm_group(OFF_MQ, 8, own_blocks, simple_evac(MQT, r_MQT, 0, 1.0 / 16.0))
            fm_group(OFF_MK, 8, own_blocks, simple_evac(MKT, r_MKT, 8, 1.0))
            fm_group(OFF_AQ, 16, own_blocks, qknorm_evac(True))
            fm_group(OFF_AK, 4, all_blocks, qknorm_evac(False))
            fm_group(OFF_MG, 32, own_blocks, sig_evac(GMT, r_GMT, 36))

            def tm_group(off, ncols, chunks, dst, rdst, sig, own_only):
                for s in range(0, ncols, 512):
                    wt, wr = load_w(w_in[:, off + s: off + s + 512], 512)
                    bi_ = brow_i[0]
                    brow_i[0] = 1 - bi_
                    brow, r_brow = brows[bi_], rbrows[bi_]
                    LDC(brow[:], b_in[0:1, off + s: off + s + 512], w=[r_brow])
                    for c in chunks:
                        pb, rb = next_bank()
                        for dc in range(DC):
                            M(lambda e, dc=dc, pb=pb, wt=wt, c=c: e.matmul(
                                pb[:, :], uT[:, dc, c * 128:(c + 1) * 128], wt[:, dc, :], start=(dc == 0), stop=False),
                              [wr, r_uT[c]], [rb])
                        M(lambda e, pb=pb, brow=brow: e.matmul(pb[:, :], ones_b[0:1, :], brow[0:1, :],
                                                               start=False, stop=True), [r_onesb, r_brow], [rb])
                        st, rs = next_stg()
                        if sig:
                            A(lambda e, pb=pb, st=st: e.activation(out=st[:], in_=pb[:], func=AF.Sigmoid), [rb], [rs])
                        else:
                            V(lambda e, pb=pb, st=st: e.tensor_copy(out=st[:], in_=pb[:]), [rb], [rs])
                        cl = c - C0 if own_only else c
                        LD(dst[cl, :, s:s + 512], st[:], [rs], [rdst])
                    _compact(CONSTS + r_uT)

            allc = list(range(NT))
            ownc = list(range(C0, NT))
            tm_group(OFF_MK, MH * MQK, allc, MKd, r_MKd, False, False)
            tm_group(OFF_MV, MH * MV, allc, MVd, r_MVd, False, False)
            tm_group(OFF_OG, MH * MV, ownc, OGd, r_OGd, True, True)
            tm_group(OFF_AV, 4 * HD, allc, AVd, r_AVd, False, False)
            for c in allc:
                pb, rb = next_bank()
                for dc in range(DC):
                    M(lambda e, dc=dc, pb=pb, c=c: e.matmul(pb[:, 0:16], uT[:, dc, c * 128:(c + 1) * 128], wg_b[:, dc, :],
                                                            start=(dc == 0), stop=(dc == DC - 1)), [r_wg, r_uT[c]], [rb])
                V(lambda e, pb=pb, c=c: e.tensor_tensor(out=gates[:, c, :], in0=pb[:, 0:16], in1=bg_bc[:], op=ALU.add),
                  [rb, r_bg], [r_gates])
        _compact(CONSTS + r_uT)

        if cfg.DEBUG:
            P.barrier()
            LD(dbg_out("dbg_gates", [128, NT, 16], F32), gates[:], [r_gates], [Res()])
            LD(dbg_out("dbg_AKT", [128, 4, TA], BF16), AKT[:], [r_AKT], [Res()])
        P.barrier()
        MOT = at(0, [128, 16, T], BF16); r_MOT = Res()
        AOT = at(32 * KB, [128, 16, T], BF16); r_AOT = Res()

        with contextlib.ExitStack() as s3:
            s3 = Bump(90 * KB, 160 * KB)
            s3b = Bump(32 * KB, 72 * KB)
            lf = sb(s3, "lf", [128, NT, 8], F32); r_lf = Res()
            A(lambda e: e.activation(out=lf[:], in_=gates[:, :, 8:16], func=AF.Exp, scale=-1.0), [r_gates], [r_lf])
            A(lambda e: e.activation(out=lf[:], in_=lf[:], func=AF.Ln, bias=1.0, scale=1.0), [r_lf], [r_lf])
            V(lambda e: e.tensor_scalar(out=lf[:], in0=lf[:], scalar1=-1.0, scalar2=None, op0=ALU.mult), [r_lf], [r_lf])
            rr = sb(s3, "rr", [128, NT, 8], F32); r_rr = Res()
            einv = sb(s3, "einv", [128, NT, 8], F32); r_einv = Res()
            etot = sb(s3, "etot", [128, NT, 8], F32); r_etot = Res()
            for c in range(NT):
                pb, rb = next_bank()
                M(lambda e, pb=pb, c=c: e.matmul(pb[:, 0:4], triU[:], lf[:, c, 0:4], start=True, stop=True), [r_triU, r_lf], [rb])
                M(lambda e, pb=pb, c=c: e.matmul(pb[:, 4:8], triL[:], lf[:, c, 4:8], start=True, stop=True), [r_triL, r_lf], [rb])
                M(lambda e, pb=pb, c=c: e.matmul(pb[:, 8:16], ones_f[:], lf[:, c, 0:8], start=True, stop=True), [r_onesf, r_lf], [rb])
                V(lambda e, pb=pb, c=c: e.tensor_tensor(out=rr[:, c, :], in0=gates[:, c, 0:8], in1=pb[:, 0:8], op=ALU.subtract),
                  [rb, r_gates], [r_rr])
                A(lambda e, pb=pb, c=c: e.activation(out=einv[:, c, :], in_=pb[:, 0:8], func=AF.Exp, scale=-1.0), [rb], [r_einv])
                A(lambda e, pb=pb, c=c: e.activation(out=etot[:, c, :], in_=pb[:, 8:16], func=AF.Exp), [rb], [r_etot])
            A(lambda e: e.activation(out=rr[:], in_=rr[:], func=AF.Exp), [r_rr], [r_rr])
            _compact(CONSTS)

            gmh_bc = sb(s3, "gmh_bc", [128, MV], F32); r_gmh = Res()
            qT = sb(s3, "qT", [128, 2, T], BF16); r_qT = Res()
            kT = sb(s3, "kT", [128, 2, T], BF16); r_kT = Res()
            ktm = sb(s3, "ktm", [128, NT, MQK], BF16); r_ktm = Res()
            vtm = sb(s3b, "vtm", [128, NT, MV], BF16); r_vtm = Res()
            ogt = sb(s3, "ogt", [128, NOWN, MV], BF16); r_ogt = Res()
            hA = sb(s3b, "hA", [128, NOWN, MV], F32); r_hA = Res()
            Cst = sb(s3, "Cst", [128, 2, MV + 1], F32); r_Cst = Res()
            Cb = sb(s3, "Cb", [128, 2, MV + 1], BF16); r_Cb = Res()
            kr = sb(s3, "kr", [128, MQK], BF16); r_kr = Res()
            PT = sb(s3, "PT", [128, 128], BF16); r_PT = Res()
            dtmp = sb(s3, "dtmp", [128, 2, MV + 1], F32); r_dtmp = Res()
            den = sb(s3, "den", [128, 2], F32); r_den = Res()
            hs = sb(s3, "hs", [128, MV], F32); r_hs = Res()
            hjunk = sb(s3, "hjunk", [128, MV], BF16); r_hjunk = Res()
            hss = sb(s3, "hss", [128, 1], F32); r_hss = Res()
            mo = sb(s3, "mo", [128, MV], BF16); r_mo = Res()

            for h in range(MH):
                for j in range(2):
                    LD(qT[:, j, :], MQT[2 * h + j], [r_MQT], [r_qT])
                    LD(kT[:, j, :], MKT[2 * h + j], [r_MKT], [r_kT])
                LD(ktm[:], MKd[:, :, h * MQK:(h + 1) * MQK].rearrange("c p n -> p c n"), [r_MKd], [r_ktm])
                LD(vtm[:], MVd[:, :, h * MV:(h + 1) * MV].rearrange("c p n -> p c n"), [r_MVd], [r_vtm])
                LD(ogt[:], OGd[:, :, h * MV:(h + 1) * MV].rearrange("c p n -> p c n"), [r_OGd], [r_ogt])
                LD(gmh_bc[:], bc_row(g_mh[0:1, h * MV:(h + 1) * MV], MV), w=[r_gmh])
                for dirn in range(2):
                    gi = h + 4 * dirn
                    mask, rmask = (triU, r_triU) if dirn == 0 else (triL, r_triL)
                    if dirn == 0:
                        order = list(range(NT))
                    else:
                        order = [1, 0] if cfg.NCTX == 2 else list(range(cfg.NCTX - 1, -1, -1))
                        order = order + list(range(NT - 1, C0 - 1, -1))
                    G(lambda e: e.memset(Cst[:], 0.0), w=[r_Cst])
                    G(lambda e: e.memset(Cb[:], 0.0), w=[r_Cb])
                    for idx, c in enumerate(order):
                        own = c >= C0
                        last = idx == len(order) - 1
                        co = c - C0
                        if own:
                            pS, rS = next_bank()
                            for j in range(2):
                                M(lambda e, j=j, pS=pS, co=co: e.matmul(pS[:, 0:128], kT[:, j, co * 128:(co + 1) * 128],
                                                                         qT[:, j, co * 128:(co + 1) * 128], start=(j == 0), stop=(j == 1)),
                                  [r_kT, r_qT], [rS])
                            V(lambda e, pS=pS, c=c, gi=gi, mask=mask: e.scalar_tensor_tensor(
                                out=PT[:], in0=pS[:, 0:128], scalar=rr[:, c, gi:gi + 1], in1=mask[:], op0=ALU.mult, op1=ALU.mult),
                              [rS, r_rr, rmask], [r_PT])
                            pN, rN = next_bank()
                            pD, rD = next_bank()
                            M(lambda e, pN=pN, c=c: e.matmul(pN[:, :], PT[:], vtm[:, c, :], start=True, stop=False), [r_PT, r_vtm], [rN])
                            for j in range(2):
                                M(lambda e, pN=pN, j=j, co=co: e.matmul(pN[:, :], qT[:, j, co * 128:(co + 1) * 128], Cb[:, j, 0:MV],
                                                                         start=False, stop=(j == 1)), [r_qT, r_Cb], [rN])
                            M(lambda e, pD=pD: e.matmul(pD[:, 0:1], PT[:], ones_b[:, 0:1], start=True, stop=False), [r_PT, r_onesb], [rD])
                            for j in range(2):
                                M(lambda e, pD=pD, j=j, co=co: e.matmul(pD[:, 0:1], qT[:, j, co * 128:(co + 1) * 128], Cb[:, j, MV:MV + 1],
                                                                         start=False, stop=(j == 1)), [r_qT, r_Cb], [rD])
                            A(lambda e, pD=pD: e.activation(out=den[:, 0:1], in_=pD[:, 0:1], func=AF.Abs), [rD], [r_den])
                            V(lambda e, c=c, gi=gi: e.tensor_tensor(out=den[:, 0:1], in0=den[:, 0:1], in1=einv[:, c, gi:gi + 1], op=ALU.max),
                              [r_den, r_einv], [r_den])
                            V(lambda e: e.reciprocal(out=den[:, 1:2], in_=den[:, 0:1]), [r_den], [r_den])
                            if dirn == 0:
                                V(lambda e, pN=pN, co=co: e.tensor_scalar(out=hA[:, co, :], in0=pN[:, :], scalar1=den[:, 1:2], scalar2=None,
                                                                           op0=ALU.mult), [rN, r_den], [r_hA])
                            else:
                                V(lambda e, pN=pN, co=co: e.scalar_tensor_tensor(out=hs[:], in0=pN[:, :], scalar=den[:, 1:2], in1=hA[:, co, :],
                                                                                  op0=ALU.mult, op1=ALU.add), [rN, r_den, r_hA], [r_hs])
                                G(lambda e: e.memset(hss[:], 0.0), w=[r_hss])
                                A(lambda e: e.activation(out=hjunk[:], in_=hs[:], func=AF.Square, accum_out=hss[:]), [r_hs, r_hss], [r_hjunk, r_hss])
                                A(lambda e: e.activation(out=hss[:], in_=hss[:], func=AF.Sqrt, bias=epsb[:], scale=1.0 / MV), [r_hss, r_eps], [r_hss])
                                V(lambda e: e.reciprocal(out=hss[:], in_=hss[:]), [r_hss], [r_hss])
                                V(lambda e, h=h: e.scalar_tensor_tensor(out=hs[:], in0=hs[:], scalar=hss[:, 0:1], in1=gmh_bc[:],
                                                                       op0=ALU.mult, op1=ALU.mult), [r_hs, r_hss, r_gmh], [r_hs])
                                G(lambda e, co=co: e.tensor_tensor(out=mo[:], in0=hs[:], in1=ogt[:, co, :], op=ALU.mult), [r_hs, r_ogt], [r_mo])
                                for j in range(4):
                                    M(lambda e, j=j: e.transpose(ptr[:, j * 128:(j + 1) * 128], mo[:, j * 128:(j + 1) * 128], ident_b[:]),
                                      [r_mo, r_identb], [r_ptr])
                                V(lambda e, h=h, co=co: e.tensor_copy(out=MOT[:, 4 * h:4 * h + 4, co * 128:(co + 1) * 128],
                                                                     in_=ptr[:, 0:512].rearrange("p (j t) -> p j t", j=4)), [r_ptr], [r_MOT])
                        if not last:
                            V(lambda e, c=c, gi=gi: e.tensor_scalar(out=kr[:], in0=ktm[:, c, :], scalar1=rr[:, c, gi:gi + 1], scalar2=None,
                                                                    op0=ALU.mult), [r_ktm, r_rr], [r_kr])
                            for j in range(2):
                                pC, rC = next_bank()
                                pn, rn = next_bank()
                                M(lambda e, pC=pC, j=j, c=c: e.matmul(pC[:, :], kr[:, j * 128:(j + 1) * 128], vtm[:, c, :], start=True, stop=True),
                                  [r_kr, r_vtm], [rC])
                                M(lambda e, pn=pn, j=j: e.matmul(pn[:, 0:1], kr[:, j * 128:(j + 1) * 128], ones_b[:, 0:1], start=True, stop=True),
                                  [r_kr, r_onesb], [rn])
                                V(lambda e, pC=pC, j=j: e.tensor_tensor(out=dtmp[:, j, 0:MV], in0=pC[:, :], in1=Cst[:, j, 0:MV], op=ALU.add),
                                  [rC, r_Cst], [r_dtmp])
                                V(lambda e, pn=pn, j=j: e.tensor_tensor(out=dtmp[:, j, MV:MV + 1], in0=pn[:, 0:1], in1=Cst[:, j, MV:MV + 1], op=ALU.add),
                                  [rn, r_Cst], [r_dtmp])
                            V(lambda e, c=c, gi=gi: e.tensor_scalar(out=Cst[:], in0=dtmp[:], scalar1=etot[:, c, gi:gi + 1], scalar2=None, op0=ALU.mult),
                              [r_dtmp, r_etot], [r_Cst])
                            A(lambda e: e.activation(out=Cb[:], in_=Cst[:], func=AF.Copy), [r_Cst], [r_Cb])
                    _compact(CONSTS + [r_rr, r_einv, r_etot, r_vtm, r_ktm, r_qT, r_kT])

        P.barrier()
        with contextlib.ExitStack() as s4:
            s4 = Bump(90 * KB, ARENA)
            vat = sb(s4, "vat", [128, NT, HD], BF16); r_vat = Res()
            qh = sb(s4, "qh", [128, T], BF16); r_qh = Res()
            PTa = [sb(s4, f"PTa{i}", [128, 512], BF16) for i in range(2)]
            rPTa = [Res(), Res()]
            rsum = sb(s4, "rsum", [128, 512], F32); r_rsum = Res()
            sc = 1.0 / math.sqrt(HD)
            for kh in range(4):
                LD(vat[:], AVd[:, :, kh * HD:(kh + 1) * HD].rearrange("c p n -> p c n"), [r_AVd], [r_vat])
                for g in range(4):
                    head = kh * 4 + g
                    LD(qh[:], AQT[head], [r_AQT], [r_qh])
                    for b in range(NB):
                        oz = 2 * ((head * NB + b) % 2)
                        pO, rO = pbank[oz], rbank[oz]
                        pZ, rZ = pbank[oz + 1], rbank[oz + 1]
                        for c in range(NT):
                            si = 4 + (c % 3)
                            pS, rS = pbank[si], rbank[si]
                            M(lambda e, pS=pS, c=c, b=b, kh=kh: e.matmul(pS[:, 0:TB], AKT[:, kh, c * 128:(c + 1) * 128], qh[:, b * TB:(b + 1) * TB],
                                                                        start=True, stop=True), [r_AKT, r_qh], [rS])
                            i = c % 2
                            A(lambda e, pS=pS, i=i: e.activation(out=PTa[i][:, 0:TB], in_=pS[:, 0:TB], func=AF.Exp, scale=sc), [rS], [rPTa[i]])
                            M(lambda e, pO=pO, c=c, i=i: e.matmul(pO[:, 0:TB], vat[:, c, :], PTa[i][:, 0:TB], start=(c == 0), stop=(c == NT - 1)),
                              [r_vat, rPTa[i]], [rO])
                            M(lambda e, pZ=pZ, c=c, i=i: e.matmul(pZ[:, 0:TB], ones_b[:], PTa[i][:, 0:TB], start=(c == 0), stop=(c == NT - 1)),
                              [r_onesb, rPTa[i]], [rZ])
                        V(lambda e, pZ=pZ: e.reciprocal(out=rsum[:, 0:TB], in_=pZ[:, 0:TB]), [rZ], [r_rsum])
                        V(lambda e, pO=pO, head=head, b=b: e.tensor_tensor(out=AOT[:, head, b * TB:(b + 1) * TB], in0=pO[:, 0:TB], in1=rsum[:, 0:TB],
                                                                          op=ALU.mult), [rO, r_rsum], [r_AOT])
                    _compact(CONSTS + [r_AKT, r_vat])

        P.barrier()

        acc = at(0, [128, NOWN, D], F32); r_acc = [Res(f"acc{c}") for c in range(NOWN)]
        u2tm = at(64 * KB, [128, NOWN, D], BF16); r_u2T = Res()
        Gd = at(96 * KB, [128, NOWN, NE], F32); r_Gd = Res()
        posm = at(97 * KB, [128, NOWN, NE], F32); r_posm = Res()
        gt1_bc = at(64 * KB, [128, D], F32)
        gt2_bc = at(98 * KB, [128, D], F32)

        with contextlib.ExitStack() as s5:
            s5 = Bump(122 * KB, ARENA)
            zT = at(90 * KB, [128, 16, T], BF16); r_zT = Res()
            gmt = sb(s5, "gmt", [128, 2, T], BF16); r_gmt = Res()
            za = sb(s5, "za", [128, 512], F32); r_za = Res()
            zb = sb(s5, "zb", [128, 512], F32); r_zb = Res()
            for s in range(4):
                wm, rwm = load_w(w_br_m[:, s * 512:(s + 1) * 512], 512)
                wa, rwa = load_w(w_br_a[:, s * 512:(s + 1) * 512], 512)
                for jj in range(4):
                    j = s * 4 + jj
                    LD(gmt[:, 0, :], GMT[j], [r_GMT], [r_gmt])
                    LD(gmt[:, 1, :], GMT[16 + j], [r_GMT], [r_gmt])
                    for b in range(NB):
                        pm, rm = next_bank()
                        pa, ra = next_bank()
                        for k in range(16):
                            M(lambda e, pm=pm, wm=wm, jj=jj, k=k, b=b: e.matmul(pm[:, 0:TB], wm[:, k, jj * 128:(jj + 1) * 128], MOT[:, k, b * TB:(b + 1) * TB],
                                                                               start=(k == 0), stop=(k == 15)), [rwm, r_MOT], [rm])
                        for k in range(16):
                            M(lambda e, pa=pa, wa=wa, jj=jj, k=k, b=b: e.matmul(pa[:, 0:TB], wa[:, k, jj * 128:(jj + 1) * 128], AOT[:, k, b * TB:(b + 1) * TB],
                                                                               start=(k == 0), stop=(k == 15)), [rwa, r_AOT], [ra])
                        V(lambda e, pm=pm, b=b: e.tensor_tensor(out=za[:, 0:TB], in0=pm[:, 0:TB], in1=gmt[:, 0, b * TB:(b + 1) * TB], op=ALU.mult),
                          [rm, r_gmt], [r_za])
                        V(lambda e, pa=pa, b=b: e.tensor_tensor(out=zb[:, 0:TB], in0=pa[:, 0:TB], in1=gmt[:, 1, b * TB:(b + 1) * TB], op=ALU.mult),
                          [ra, r_gmt], [r_zb])
                        G(lambda e, j=j, b=b: e.tensor_tensor(out=zT[:, j, b * TB:(b + 1) * TB], in0=za[:, 0:TB], in1=zb[:, 0:TB], op=ALU.add),
                          [r_za, r_zb], [r_zT])
                _compact(CONSTS + [r_MOT, r_AOT])
            if cfg.DEBUG:
                P.barrier()
                LD(dbg_out("dbg_MOT", [128, 16, T], BF16), MOT[:], [r_MOT], [Res()])
                LD(dbg_out("dbg_AOT", [128, 16, T], BF16), AOT[:], [r_AOT], [Res()])
                LD(dbg_out("dbg_zT", [128, 16, T], BF16), zT[:], [r_zT], [Res()])
            P.barrier()
            for c in range(NOWN):
                LD(acc[:, c, :], xf[(C0 + c) * 128:(C0 + c + 1) * 128, :], w=[r_acc[c]])
            LD(gt1_bc[:], bc_row(mod_d[0:1, 2 * D:3 * D], D), [r_mod], [r_gt1])
            for db in range(4):
                wo, rwo = load_w(w_out[:, db * 512:(db + 1) * 512], 512)
                for c in range(NOWN):
                    pb, rb = next_bank()
                    for k in range(16):
                        M(lambda e, pb=pb, wo=wo, k=k, c=c: e.matmul(pb[:, :], zT[:, k, c * 128:(c + 1) * 128], wo[:, k, :], start=(k == 0), stop=(k == 15)),
                          [rwo, r_zT], [rb])
                    V(lambda e, pb=pb, db=db: e.tensor_tensor(out=za[:], in0=pb[:], in1=gt1_bc[:, db * 512:(db + 1) * 512], op=ALU.mult),
                      [rb, r_gt1], [r_za])
                    G(lambda e, c=c, db=db: e.tensor_tensor(out=acc[:, c, db * 512:(db + 1) * 512], in0=acc[:, c, db * 512:(db + 1) * 512], in1=za[:], op=ALU.add),
                      [r_za, r_acc[c]], [r_acc[c]])
                _compact(CONSTS + [r_zT, r_gt1])
        P.barrier()

        if cfg.DEBUG:
            LD(dbg_out("dbg_hx", [128, NOWN, D], F32), acc[:], r_acc, [Res()])
            P.barrier()
        with contextlib.ExitStack() as s5b:
            s5b = Bump(98 * KB, ARENA)
            Mk_f = sb(s5b, "Mk_f", [128, NOWN, NE], F32); r_Mkf = Res()
            Mk_b = sb(s5b, "Mk_b", [128, NOWN, NE], BF16); r_Mkb = Res()
            triSU_b = sb(s5b, "triSU_b", [128, 128], BF16); r_triSU = Res()
            LDC(triSU_b[:], c_triSU, w=[r_triSU])
            A2_bc = sb(s5b, "A2_bc", [128, D], F32)
            sh2_bc = sb(s5b, "sh2_bc", [128, D], F32)
            g2b = sb(s5b, "g2b", [128, D], F32); r_g2b = Res()
            LD(g2b[:], bc_row(g2[0:1, :], D), w=[r_g2b])
            LD(sh2_bc[:], bc_row(mod_d[0:1, 3 * D:4 * D], D), [r_mod], [r_sh2])
            LD(A2_bc[:], bc_row(mod_d[0:1, 4 * D:5 * D], D), [r_mod], [r_A2])
            V(lambda e: e.scalar_tensor_tensor(out=A2_bc[:], in0=A2_bc[:], scalar=1.0, in1=g2b[:], op0=ALU.add, op1=ALU.mult),
              [r_A2, r_g2b], [r_A2])
            u2f = sb(s5b, "u2f", [128, D], F32); r_u2f = Res()
            u2b = sb(s5b, "u2b", [128, D], BF16); r_u2b = Res()
            u2Tf = sb(s5b, "u2Tf", [128, 4, 128], F32); r_u2Tf = Res()
            wr_f = sb(s5b, "wr_f", [128, DC, NE], F32); r_wr = Res()
            br_bc = sb(s5b, "br_bc", [128, NE], F32); r_br = Res()
            lg = sb(s5b, "lg", [128, NE], F32); r_lg = Res()
            mx8 = sb(s5b, "mx8", [128, 8], F32); r_mx8 = Res()
            nmx = sb(s5b, "nmx", [128, 1], F32); r_nmx = Res()
            ex = sb(s5b, "ex", [128, NE], F32); r_ex = Res()
            msk = sb(s5b, "msk", [128, NE], F32); r_msk = Res()
            esum = sb(s5b, "esum", [128, 1], F32); r_esum = Res()
            ss2 = sb(s5b, "ss2", [128, 1], F32); r_ss2 = Res()
            LD(wr_f[:], w_router.rearrange("(dc p) n -> p dc n", p=128), w=[r_wr])
            LD(br_bc[:], bc_row(b_router[0:1, :], NE), w=[r_br])
            for c in range(NOWN):
                G(lambda e: e.memset(ss2[:], 0.0), w=[r_ss2])
                A(lambda e, c=c: e.activation(out=u2b[:], in_=acc[:, c, :], func=AF.Square, accum_out=ss2[:]), [r_acc[c], r_ss2], [r_u2b, r_ss2])
                A(lambda e: e.activation(out=ss2[:], in_=ss2[:], func=AF.Sqrt, bias=epsb[:], scale=1.0 / D), [r_ss2, r_eps], [r_ss2])
                V(lambda e: e.reciprocal(out=ss2[:], in_=ss2[:]), [r_ss2], [r_ss2])
                V(lambda e, c=c: e.scalar_tensor_tensor(out=u2f[:], in0=acc[:, c, :], scalar=ss2[:, 0:1], in1=A2_bc[:], op0=ALU.mult, op1=ALU.mult),
                  [r_acc[c], r_ss2, r_A2], [r_u2f])
                V(lambda e: e.tensor_tensor(out=u2f[:], in0=u2f[:], in1=sh2_bc[:], op=ALU.add), [r_u2f, r_sh2], [r_u2f])
                G(lambda e, c=c: e.tensor_copy(out=u2tm[:, c, :], in_=u2f[:]), [r_u2f], [r_u2T])
                pl, rl = next_bank()
                for q4 in range(DC // 4):
                    pb, rb = next_bank()
                    if pb is pl:
                        pb, rb = next_bank()
                    for j in range(4):
                        dc = q4 * 4 + j
                        M(lambda e, pb=pb, dc=dc, j=j: e.transpose(pb[:, j * 128:(j + 1) * 128], u2f[:, dc * 128:(dc + 1) * 128], ident_f[:]),
                          [r_u2f, r_identf], [rb])
                    A(lambda e, pb=pb: e.activation(out=u2Tf[:], in_=pb[:].rearrange("p (j t) -> p j t", j=4), func=AF.Copy),
                      [rb], [r_u2Tf])
                    for j in range(4):
                        dc = q4 * 4 + j
                        M(lambda e, pl=pl, dc=dc, j=j: e.matmul(pl[:, 0:NE], u2Tf[:, j, :], wr_f[:, dc, :], start=(dc == 0), stop=(dc == DC - 1)),
                          [r_u2Tf, r_wr], [rl])
                V(lambda e, pl=pl: e.tensor_tensor(out=lg[:], in0=pl[:, 0:NE], in1=br_bc[:], op=ALU.add), [rl, r_br], [r_lg])
                V(lambda e: e.max(out=mx8[:], in_=lg[:]), [r_lg], [r_mx8])
                V(lambda e: e.tensor_scalar(out=nmx[:], in0=mx8[:, 0:1], scalar1=-1.0, scalar2=None, op0=ALU.mult), [r_mx8], [r_nmx])
                A(lambda e: e.activation(out=ex[:], in_=lg[:], func=AF.Exp, bias=nmx[:], scale=1.0), [r_lg, r_nmx], [r_ex])
                V(lambda e: e.tensor_scalar(out=msk[:], in0=lg[:], scalar1=mx8[:, cfg.TOPK - 1:cfg.TOPK], scalar2=None, op0=ALU.is_ge),
                  [r_lg, r_mx8], [r_msk])
                V(lambda e: e.tensor_tensor(out=ex[:], in0=ex[:], in1=msk[:], op=ALU.mult), [r_ex, r_msk], [r_ex])
                G(lambda e, c=c: e.tensor_copy(out=Mk_f[:, c, :], in_=msk[:]), [r_msk], [r_Mkf])
                G(lambda e, c=c: e.tensor_copy(out=Mk_b[:, c, :], in_=msk[:]), [r_msk], [r_Mkb])
                V(lambda e: e.tensor_reduce(out=esum[:], in_=ex[:], axis=mybir.AxisListType.X, op=ALU.add), [r_ex], [r_esum])
                V(lambda e: e.reciprocal(out=esum[:], in_=esum[:]), [r_esum], [r_esum])
                V(lambda e, c=c: e.tensor_scalar(out=Gd[:, c, :], in0=ex[:], scalar1=esum[:, 0:1], scalar2=None, op0=ALU.mult),
                  [r_ex, r_esum], [r_Gd])
                _compact(CONSTS + [r_wr, r_br, r_A2, r_sh2])
            for c in range(NOWN):
                pp, rp = next_bank()
                for c2 in range(c):
                    M(lambda e, pp=pp, c2=c2: e.matmul(pp[:, 0:NE], ones_b[:], Mk_b[:, c2, :], start=(c2 == 0), stop=False),
                      [r_onesb, r_Mkb], [rp])
                M(lambda e, pp=pp, c=c: e.matmul(pp[:, 0:NE], triSU_b[:], Mk_b[:, c, :], start=(c == 0), stop=True),
                  [r_triSU, r_Mkb], [rp])
                V(lambda e, pp=pp, c=c: e.scalar_tensor_tensor(out=posm[:, c, :], in0=pp[:, 0:NE], scalar=1.0, in1=Mk_f[:, c, :],
                                                               op0=ALU.add, op1=ALU.mult), [rp, r_Mkf], [r_posm])
            V(lambda e: e.tensor_scalar(out=posm[:], in0=posm[:], scalar1=-1.0, scalar2=None, op0=ALU.add), [r_posm], [r_posm])

        if cfg.DEBUG:
            LD(dbg_out("dbg_Gd", [128, NOWN, NE], F32), Gd[:], [r_Gd], [Res()])
            LD(dbg_out("dbg_u2tm", [128, NOWN, D], BF16), u2tm[:], [r_u2T], [Res()])
            LD(dbg_out("dbg_posm", [128, NOWN, NE], F32), posm[:], [r_posm], [Res()])
        P.barrier()
        if True:
            CAP = 512
            s6 = Bump(106 * KB, ARENA)
            LD(gt2_bc[:], bc_row(mod_d[0:1, 5 * D:6 * D], D), [r_mod], [r_gt2])
            iota_f = sb(s6, "iota_f", [128, CAP], F32); r_iota = Res()
            LD(iota_f[:], c_iota, w=[r_iota])
            STs = sb(s6, "STs", [128, CAP // 128, T], BF16); r_ST = Res()
            xT = sb(s6, "xT", [128, DC, CAP], BF16); r_xT = Res()
            hid_off = s6.off
            hidT = sb(s6, "hidT", [128, FC, CAP], BF16); r_hid = Res()
            Ys = sb(s6, "Ys", [128, CAP // 128, D], BF16); r_Ys = Res()
            assert NOWN <= FC
            Ssel = at(hid_off, [128, NOWN, CAP], BF16); r_S = r_hid
            bgu = [sb(s6, f"bgu{i}", [128, 2 * FC], F32) for i in range(2)]
            rbgu = [Res(), Res()]
            bdn = [sb(s6, f"bdn{i}", [1, 512], BF16) for i in range(2)]
            rbdn = [Res(), Res()]
            bdn_i = [0]
            gg = sb(s6, "gg", [128, CAP], F32); r_gg = Res()
            sg = sb(s6, "sg", [128, CAP], BF16); r_sg = Res()
            uu = sb(s6, "uu", [128, CAP], BF16); r_uu = Res()
            jobs = []
            for ex_i in range(NE):
                for s in range(0, FC, 2):
                    jobs.append(("gu", ex_i, s))
                for db in range(4):
                    jobs.append(("dn", ex_i, db))

            def issue(job):
                kind, ex_i, k = job
                i0 = ring_i[0]
                ring_i[0] = (i0 + 1) % NR
                wt, wr = wring[i0], rring[i0]
                if kind == "gu":
                    vg = w_gu[ex_i][:, k * 128:(k + 2) * 128].rearrange("(dc p) n -> p dc n", p=128)
                    vu = w_gu[ex_i][:, DFF + k * 128:DFF + (k + 2) * 128].rearrange("(dc p) n -> p dc n", p=128)
                    LDC(wt[:, :, 0:256], vg, w=[wr])
                    P.dma("pool", lambda e: e.dma_start(out=wt[:, :, 256:512], in_=vu), [wr], [wr])
                else:
                    vd = w_dn[ex_i][:, k * 512:(k + 1) * 512].rearrange("(dc p) n -> p dc n", p=128)
                    LDC(wt[:, 0:FC, :], vd, w=[wr])
                    bi_ = bdn_i[0]
                    bdn_i[0] = 1 - bi_
                    LDC(bdn[bi_][:], b_dn[ex_i:ex_i + 1, k * 512:(k + 1) * 512], w=[rbdn[bi_]])
                    bd_q.append((bdn[bi_], rbdn[bi_]))
                return wt, wr

            bd_q = []

            def prologue(ex_i):
                pi = ex_i % 2
                P.dma("sp", lambda e: e.dma_start(out=bgu[pi][:], in_=b_gu[ex_i, :].rearrange("(j p) -> p j", p=128),
                                                  allow_slow_non_contiguous=True), [], [rbgu[pi]])
                for c in range(NOWN):
                    V(lambda e, c=c: e.tensor_scalar(out=Ssel[:, c, :], in0=iota_f[:], scalar1=posm[:, c, ex_i:ex_i + 1], scalar2=None,
                                                     op0=ALU.is_equal), [r_iota, r_posm], [r_S])
                for sbk in range(CAP // 128):
                    for c in range(NOWN):
                        M(lambda e, c=c, sbk=sbk: e.transpose(ptr[:, c * 128:(c + 1) * 128], Ssel[:, c, sbk * 128:(sbk + 1) * 128], ident_b[:]),
                          [r_S, r_identb], [r_ptr])
                    A(lambda e, sbk=sbk: e.activation(out=STs[:, sbk, :], in_=ptr[:, 0:T], func=AF.Copy), [r_ptr], [r_ST])
                for dc in range(DC):
                    pb, rb = next_bank()
                    for c in range(NOWN):
                        M(lambda e, pb=pb, dc=dc, c=c: e.matmul(pb[:, 0:CAP], u2tm[:, c, dc * 128:(dc + 1) * 128], Ssel[:, c, :],
                                                                start=(c == 0), stop=(c == NOWN - 1)), [r_u2T, r_S], [rb])
                    if dc % 2 == 0:
                        V(lambda e, pb=pb, dc=dc: e.tensor_copy(out=xT[:, dc, :], in_=pb[:, 0:CAP]), [rb], [r_xT])
                    else:
                        A(lambda e, pb=pb, dc=dc: e.activation(out=xT[:, dc, :], in_=pb[:, 0:CAP], func=AF.Copy), [rb], [r_xT])

            cur = issue(jobs[0])
            for ji, job in enumerate(jobs):
                nxt = issue(jobs[ji + 1]) if ji + 1 < len(jobs) else None
                kind, ex_i, k = job
                wt, wr = cur
                pi = ex_i % 2
                if kind == "dn":
                    cur_bd = bd_q.pop(0)
                if kind == "gu" and k == 0:
                    prologue(ex_i)
                if kind == "gu":
                    for ii in range(2):
                        i = k + ii
                        pg, rg = next_bank()
                        pu, ru = next_bank()
                        for dc in range(DC):
                            M(lambda e, pg=pg, wt=wt, ii=ii, dc=dc: e.matmul(pg[:, 0:CAP], wt[:, dc, ii * 128:(ii + 1) * 128], xT[:, dc, :],
                                                                          start=(dc == 0), stop=(dc == DC - 1)), [wr, r_xT], [rg])
                        for dc in range(DC):
                            M(lambda e, pu=pu, wt=wt, ii=ii, dc=dc: e.matmul(pu[:, 0:CAP], wt[:, dc, 256 + ii * 128:256 + (ii + 1) * 128], xT[:, dc, :],
                                                                          start=(dc == 0), stop=(dc == DC - 1)), [wr, r_xT], [ru])
                        V(lambda e, pg=pg, i=i, pi=pi: e.tensor_scalar(out=gg[:], in0=pg[:, 0:CAP], scalar1=bgu[pi][:, i:i + 1], scalar2=7.0,
                                                                      op0=ALU.add, op1=ALU.min), [rg, rbgu[pi]], [r_gg])
                        A(lambda e: e.activation(out=sg[:], in_=gg[:], func=AF.Sigmoid, scale=1.702), [r_gg], [r_sg])
                        V(lambda e, pu=pu, i=i, pi=pi: e.tensor_scalar(out=uu[:], in0=pu[:, 0:CAP], scalar1=bgu[pi][:, FC + i:FC + i + 1], scalar2=7.0,
                                                                      op0=ALU.add, op1=ALU.min), [ru, rbgu[pi]], [r_uu])
                        V(lambda e: e.tensor_scalar(out=uu[:], in0=uu[:], scalar1=-7.0, scalar2=1.0, op0=ALU.max, op1=ALU.add),
                          [r_uu], [r_uu])
                        V(lambda e: e.tensor_tensor(out=gg[:], in0=gg[:], in1=sg[:], op=ALU.mult), [r_gg, r_sg], [r_gg])
                        V(lambda e, i=i: e.tensor_tensor(out=hidT[:, i, :], in0=gg[:], in1=uu[:], op=ALU.mult), [r_gg, r_uu], [r_hid])
                else:
                    db = k
                    bd, rbd = cur_bd
                    for sbk in range(CAP // 128):
                        pb, rb = next_bank()
                        for i in range(FC):
                            M(lambda e, pb=pb, wt=wt, i=i, sbk=sbk: e.matmul(pb[:, :], hidT[:, i, sbk * 128:(sbk + 1) * 128], wt[:, i, :], start=(i == 0), stop=False),
                              [wr, r_hid], [rb])
                        M(lambda e, pb=pb, bd=bd: e.matmul(pb[:, :], ones_b[0:1, :], bd[0:1, :], start=False, stop=True),
                          [r_onesb, rbd], [rb])
                        V(lambda e, pb=pb, sbk=sbk, db=db: e.tensor_tensor(out=Ys[:, sbk, db * 512:(db + 1) * 512], in0=pb[:], in1=gt2_bc[:, db * 512:(db + 1) * 512],
                                                                          op=ALU.mult), [rb, r_gt2], [r_Ys])
                    for c in range(NOWN):
                        pb, rb = next_bank()
                        for sbk in range(CAP // 128):
                            M(lambda e, pb=pb, sbk=sbk, c=c, db=db: e.matmul(pb[:, :], STs[:, sbk, c * 128:(c + 1) * 128], Ys[:, sbk, db * 512:(db + 1) * 512],
                                                                            start=(sbk == 0), stop=(sbk == CAP // 128 - 1)), [r_ST, r_Ys], [rb])
                        V(lambda e, pb=pb, c=c, ex_i=ex_i, db=db: e.scalar_tensor_tensor(out=acc[:, c, db * 512:(db + 1) * 512], in0=pb[:], scalar=Gd[:, c, ex_i:ex_i + 1],
                                                                                       in1=acc[:, c, db * 512:(db + 1) * 512], op0=ALU.mult, op1=ALU.add),
                          [rb, r_Gd, r_acc[c]], [r_acc[c]])
                _compact(CONSTS + [r_u2T, r_Gd, r_gt2, r_posm, r_iota])
                cur = nxt

        r_out = Res("out")
        for c in range(NOWN):
            LD(out_d[c * 128:(c + 1) * 128, :], acc[:, c, :], [r_acc[c]], [r_out])
        P.finish()
    return nc


def _consts(cfg, h):
    NT = cfg.NCTX + cfg.NOTH + cfg.NOWN
    TA = NT * 128
    ident = np.eye(128, dtype=np.float32)
    jj, tt = np.meshgrid(np.arange(128), np.arange(128), indexing="ij")
    triU = (jj <= tt).astype(np.float32)
    triL = (jj >= tt).astype(np.float32)
    rot = np.zeros((128, 128), np.float32)
    for i in range(64):
        rot[2 * i + 1, 2 * i] = -1.0
        rot[2 * i, 2 * i + 1] = 1.0
    nctx = cfg.NCTX * 128
    seq = cfg.SEQ
    pos = np.arange(seq)
    if h == 0:
        pos = pos[::-1]
    rows = (pos // cfg.GRID_W).astype(np.float32)
    cols = (pos % cfg.GRID_W).astype(np.float32)
    freqs = np.exp(-math.log(10000.0) * np.arange(32, dtype=np.float32) / 32).astype(np.float32)
    ang = np.concatenate([rows[:, None] * freqs, cols[:, None] * freqs], axis=-1).astype(np.float32)
    cos = np.repeat(np.cos(ang), 2, axis=1).T
    sin = np.repeat(np.sin(ang), 2, axis=1).T
    cosT = np.concatenate([np.ones((128, nctx), np.float32), cos.astype(np.float32)], axis=1)
    sinT = np.concatenate([np.zeros((128, nctx), np.float32), sin.astype(np.float32)], axis=1)
    assert cosT.shape[1] == TA
    triSU = (jj < tt).astype(np.float32)
    iota = np.ascontiguousarray(np.broadcast_to(np.arange(512, dtype=np.float32)[None, :], (128, 512)))
    return dict(c_ident=ident, c_triU=triU, c_triL=triL, c_rot=rot, c_triSU=triSU, c_iota=iota,
                c_cos=np.ascontiguousarray(cosT), c_sin=np.ascontiguousarray(sinT))


def make_in_maps(cfg, inp):
    f = lambda a: np.ascontiguousarray(np.asarray(a, dtype=np.float32))
    x, c, ctx, c_ctx = f(inp["x"]), f(inp["c"]), f(inp["ctx"]), f(inp["c_ctx"])
    B = x.shape[0]
    shared = dict(
        w_mod=f(inp["w_mod"][0]), b_mod=f(inp["b_mod"][0])[None, :], g1=f(inp["g_norm1"][0])[None, :],
        g2=f(inp["g_norm2"][0])[None, :], w_in=f(inp["w_in"][0]), b_in=f(inp["b_in"][0])[None, :],
        g_q=f(inp["g_q"][0])[:, None], g_k=f(inp["g_k"][0])[:, None], g_mh=f(inp["g_mh"][0])[None, :],
        w_br_m=f(inp["w_br_m"][0]), w_br_a=f(inp["w_br_a"][0]), w_out=f(inp["w_out"][0]),
        w_router=f(inp["w_router"][0]), b_router=f(inp["b_router"][0])[None, :],
        w_gu=f(inp["w_gu"][0]), b_gu=f(inp["b_gu"][0]), w_dn=f(inp["w_dn"][0]), b_dn=f(inp["b_dn"][0]),
    )
    wgt = shared["w_in"][:, OFF_GT:OFF_GT + 16]
    bgt = shared["b_in"][0, OFF_GT:OFF_GT + 16]
    perm = {1: list(range(0, 4)) + list(range(8, 12)) + list(range(4, 8)) + list(range(12, 16)),
            0: list(range(8, 12)) + list(range(0, 4)) + list(range(12, 16)) + list(range(4, 8))}
    half = cfg.SEQ // 2
    maps = []
    for core in range(2 * B):
        b, h = core // 2, core % 2
        if h == 1:
            xfm = np.concatenate([ctx[b], x[b]], axis=0)
        else:
            xfm = np.concatenate([ctx[b, ::-1], x[b, ::-1]], axis=0)
        m = dict(shared)
        m["xf"] = np.ascontiguousarray(xfm)
        m["cc"] = np.ascontiguousarray(np.stack([c[b], c_ctx], axis=0))
        m["wg16"] = np.ascontiguousarray(wgt[:, perm[h]])
        m["bg16"] = np.ascontiguousarray(bgt[perm[h]])[None, :]
        m.update(_consts(cfg, h))
        maps.append(m)
    return maps


def assemble(cfg, results, B):
    half = cfg.SEQ // 2
    out = np.zeros((B, cfg.SEQ, cfg.D), np.float32)
    for core in range(2 * B):
        b, h = core // 2, core % 2
        o = np.asarray(results[core]["out"], dtype=np.float32)
        if h == 1:
            out[b, half:] = o
        else:
            out[b, :half] = o[::-1]
    return out


def kernel(**inputs):
    cfg = Cfg()
    nc = build(cfg)
    maps = make_in_maps(cfg, inputs)
    res = run_bass_kernel_spmd(nc, maps, core_ids=list(range(8)))
    return assemble(cfg, res.results, 4)
```

```python
import contextlib
import math
import numpy as np
import concourse.bass as bass
import concourse.mybir as mybir
from concourse.bass_utils import run_bass_kernel_spmd

F32 = mybir.dt.float32
BF16 = mybir.dt.bfloat16
ALU = mybir.AluOpType
AF = mybir.ActivationFunctionType

COMPUTE = ("pe", "dve", "act", "pool")
NDMA = {"sp": 8, "pool": 4}


class Res:
    __slots__ = ("name", "w", "rd")

    def __init__(self, name=""):
        self.name = name
        self.w = None
        self.rd = []


class Prog:
    def __init__(self, nc, es):
        self.nc = nc
        self.es = es
        self.ops = {e: [] for e in ("pe", "dve", "act", "pool", "sp")}
        self.cnt = {e: 0 for e in COMPUTE}
        self.sem = {e: es.enter_context(nc.semaphore("s_" + e)) for e in COMPUTE}
        self.known = {e: {} for e in ("pe", "dve", "act", "pool", "sp")}
        self.snaps = {e: [dict()] for e in COMPUTE}
        self.dsem = {}
        self.duse = {}
        self.drot = {}
        for q, n in NDMA.items():
            for i in range(n):
                self.dsem[(q, i)] = es.enter_context(nc.semaphore(f"d_{q}{i}"))
                self.duse[(q, i)] = 0
            self.drot[q] = 0

    def semh(self, key):
        return self.sem[key] if key in self.sem else self.dsem[key]

    def _need(self, eng, ev, waits):
        if ev is None:
            return
        key, val = ev
        if self.known[eng].get(key, 0) >= val:
            return
        if waits.get(key, 0) < val:
            waits[key] = val

    def _learn(self, eng, key, val):
        kn = self.known[eng]
        if kn.get(key, 0) < val:
            kn[key] = val
        if key in self.snaps:
            for k2, v2 in self.snaps[key][val].items():
                if kn.get(k2, 0) < v2:
                    kn[k2] = v2

    def _deps(self, eng, reads, writes):
        waits = {}
        for r in reads:
            self._need(eng, r.w, waits)
        for r in writes:
            self._need(eng, r.w, waits)
            for ev in r.rd:
                self._need(eng, ev, waits)
        return waits

    def op(self, eng, fn, reads=(), writes=()):
        waits = self._deps(eng, reads, writes)
        if eng in waits:
            own_raw = 0
            for r in reads:
                if r.w is not None and r.w[0] == eng:
                    own_raw = max(own_raw, r.w[1])
            if eng != "pe" and own_raw > self.known[eng].get(eng, 0):
                waits[eng] = own_raw
            else:
                del waits[eng]
        for k, v in waits.items():
            self._learn(eng, k, v)
        self.cnt[eng] += 1
        n = self.cnt[eng]
        self.ops[eng].append((list(waits.items()), fn, (eng, 1)))
        self.snaps[eng].append(dict(self.known[eng]))
        ev = (eng, n)
        for r in reads:
            if len(r.rd) > 48:
                _compact([r])
            r.rd.append(ev)
        for r in writes:
            r.w = ev
            r.rd = []
        return ev

    def dma(self, q, fn, reads=(), writes=()):
        waits = self._deps(q, reads, writes)
        i = self.drot[q]
        self.drot[q] = (i + 1) % NDMA[q]
        key = (q, i)
        prev = self.duse[key]
        if prev > 0 and self.known[q].get(key, 0) < prev:
            if waits.get(key, 0) < prev:
                waits[key] = prev
        for k, v in waits.items():
            self._learn(q, k, v)
        self.duse[key] = prev + 16
        self.ops[q].append((list(waits.items()), fn, (key, 16)))
        ev = (key, prev + 16)
        for r in reads:
            r.rd.append(ev)
        for r in writes:
            r.w = ev
            r.rd = []
        return ev

    def barrier(self):
        targets = {e: self.cnt[e] for e in COMPUTE if self.cnt[e] > 0}
        targets.update({k: v for k, v in self.duse.items() if v > 0})
        for e in ("pe", "dve", "act", "pool", "sp"):
            waits = {k: v for k, v in targets.items() if k != e and self.known[e].get(k, 0) < v}
            for k, v in waits.items():
                self._learn(e, k, v)
            if waits:
                self.ops[e].append((list(waits.items()), None, None))

    def finish(self):
        nc = self.nc
        waits = {}
        for key, v in self.duse.items():
            if v > 0:
                waits[key] = v
        for e in COMPUTE:
            if self.cnt[e] > 0:
                waits[e] = self.cnt[e]
        self.ops["sp"].append((list(waits.items()), None, None))
        hmap = {"pe": "tensor", "dve": "vector", "act": "scalar", "pool": "gpsimd", "sp": "sync"}
        with nc.Block() as block:
            for e, attr in hmap.items():
                ops = self.ops[e]
                if not ops:
                    continue

                def section(engh, ops=ops):
                    for waits, fn, inc in ops:
                        for k, v in waits:
                            engh.wait_ge(self.semh(k), v)
                        if fn is None:
                            continue
                        inst = fn(engh)
                        if inc is not None:
                            inst.then_inc(self.semh(inc[0]), inc[1])

                getattr(block, attr)(section)


def _compact(res_list):
    for r in res_list:
        d = {}
        for k, v in r.rd:
            if d.get(k, 0) < v:
                d[k] = v
        r.rd = list(d.items())


class Cfg:
    D = 2048
    NCTX = 2
    NOTH = 8
    NOWN = 8
    NE = 32
    DFF = 2048
    SEQ = 2048
    GRID_W = 64
    TOPK = 4
    DEBUG = False


HD = 128
MH = 4
MQK = 256
MV = 512
EPS = 1e-6
F_IN = 13328
OFF_MQ, OFF_MK, OFF_MV, OFF_OG, OFF_GT, OFF_AQ, OFF_AK, OFF_AV, OFF_MG = (
    0, 1024, 2048, 4096, 6144, 6160, 8208, 8720, 9232)


def build(cfg):
    D = cfg.D
    DC = D // 128
    NT = cfg.NCTX + cfg.NOTH + cfg.NOWN
    NOWN = cfg.NOWN
    C0 = cfg.NCTX + cfg.NOTH
    T = NOWN * 128
    TA = NT * 128
    TB = min(512, T)
    NB = T // TB
    NE = cfg.NE
    DFF = cfg.DFF
    FC = DFF // 128

    nc = bass.Bass("TRN2", target_bir_lowering=False)

    def din(name, shape):
        return nc.dram_tensor(name, list(shape), F32, kind="ExternalInput").ap()

    xf = din("xf", [TA, D])
    cc = din("cc", [2, D])
    w_mod = din("w_mod", [D, 6 * D])
    b_mod = din("b_mod", [1, 6 * D])
    g1 = din("g1", [1, D])
    g2 = din("g2", [1, D])
    w_in = din("w_in", [D, F_IN])
    b_in = din("b_in", [1, F_IN])
    wg16 = din("wg16", [D, 16])
    bg16 = din("bg16", [1, 16])
    g_q = din("g_q", [128, 1])
    g_k = din("g_k", [128, 1])
    g_mh = din("g_mh", [1, MH * MV])
    w_br_m = din("w_br_m", [D, D])
    w_br_a = din("w_br_a", [D, D])
    w_out = din("w_out", [D, D])
    w_router = din("w_router", [D, NE])
    b_router = din("b_router", [1, NE])
    w_gu = din("w_gu", [NE, D, 2 * DFF])
    b_gu = din("b_gu", [NE, 2 * DFF])
    w_dn = din("w_dn", [NE, DFF, D])
    b_dn = din("b_dn", [NE, D])
    c_ident = din("c_ident", [128, 128])
    c_triU = din("c_triU", [128, 128])
    c_triL = din("c_triL", [128, 128])
    c_rot = din("c_rot", [128, 128])
    c_triSU = din("c_triSU", [128, 128])
    c_iota = din("c_iota", [128, 512])
    c_cos = din("c_cos", [128, TA])
    c_sin = din("c_sin", [128, TA])
    out_d = nc.dram_tensor("out", [T, D], F32, kind="ExternalOutput").ap()

    def scratch(name, shape, dt=BF16):
        if cfg.DEBUG:
            return nc.dram_tensor(name, list(shape), dt, kind="ExternalOutput").ap()
        return nc.dram_tensor(name, list(shape), dt).ap()

    def dbg_out(name, shape, dt):
        return nc.dram_tensor(name, list(shape), dt, kind="ExternalOutput").ap()

    mod_d = scratch("mod_d", [2, 6 * D], F32)
    MQT = scratch("MQT", [8, 128, T])
    MKT = scratch("MKT", [8, 128, T])
    AQT = scratch("AQT", [16, 128, T])
    GMT = scratch("GMT", [32, 128, T])
    MKd = scratch("MKd", [NT, 128, MH * MQK])
    MVd = scratch("MVd", [NT, 128, MH * MV])
    OGd = scratch("OGd", [NOWN, 128, MH * MV])
    AVd = scratch("AVd", [NT, 128, 4 * HD])

    with contextlib.ExitStack() as es:
        P = Prog(nc, es)

        KB = 1024
        ARENA = 171 * KB
        arena = es.enter_context(nc.sbuf_tensor("arena", [128, ARENA // 4], F32))

        class Bump:
            def __init__(self, off, limit):
                self.off = off
                self.limit = limit

        def sb(stack, name, shape, dt):
            if isinstance(stack, Bump):
                esz = 4 if dt == F32 else 2
                n = int(np.prod(shape[1:]))
                nbytes = (n * esz + 31) // 32 * 32
                assert stack.off + nbytes <= stack.limit, (name, stack.off, nbytes, stack.limit)
                a = arena[0:shape[0], stack.off // 4:(stack.off + nbytes) // 4]
                stack.off += nbytes
                v = a if dt == F32 else a.bitcast(BF16)
                v = v[:, 0:n]
                if len(shape) == 3:
                    v = v.rearrange("p (a b) -> p a b", a=shape[1])
                return v
            return stack.enter_context(nc.sbuf_tensor(name, list(shape), dt))

        def at(off, shape, dt):
            return sb(Bump(off, ARENA), "x", shape, dt)

        def V(fn, r=(), w=()):
            return P.op("dve", fn, r, w)

        def A(fn, r=(), w=()):
            return P.op("act", fn, r, w)

        def G(fn, r=(), w=()):
            return P.op("pool", fn, r, w)

        def M(fn, r=(), w=()):
            return P.op("pe", fn, r, w)

        def LD(out, in_, r=(), w=(), slow=False):
            if slow:
                return P.dma("sp", lambda e: e.dma_start(out=out, in_=in_, allow_slow_non_contiguous=True), r, w)
            return P.dma("sp", lambda e: e.dma_start(out=out, in_=in_), r, w)

        def LDC(out, in_, r=(), w=(), slow=False):
            if slow:
                return P.dma("pool", lambda e: e.dma_start(out=out, in_=in_, allow_slow_non_contiguous=True), r, w)
            return P.dma("pool", lambda e: e.dma_start(out=out, in_=in_), r, w)

        pbank = [es.enter_context(nc.psum_tensor(f"pb{i}", [128, 512], F32)) for i in range(7)]
        rbank = [Res(f"pb{i}") for i in range(7)]
        ptr = es.enter_context(nc.psum_tensor("ptr", [128, 1024], BF16))
        r_ptr = Res("ptr")

        ident_f = sb(es, "ident_f", [128, 128], F32); r_identf = Res()
        ident_b = sb(es, "ident_b", [128, 128], BF16); r_identb = Res()
        triU = sb(es, "triU", [128, 128], F32); r_triU = Res()
        triL = sb(es, "triL", [128, 128], F32); r_triL = Res()
        ones_f = sb(es, "ones_f", [128, 128], F32); r_onesf = Res()
        ones_b = sb(es, "ones_b", [128, 128], BF16); r_onesb = Res()
        rot_b = sb(es, "rot_b", [128, 128], BF16); r_rot = Res()
        epsb = sb(es, "epsb", [128, 1], F32); r_eps = Res()
        CONSTS = [r_identf, r_identb, r_triU, r_triL, r_onesf, r_onesb, r_rot, r_eps]
        LD(ident_f[:], c_ident, w=[r_identf])
        LDC(ident_b[:], c_ident, w=[r_identb])
        LD(triU[:], c_triU, w=[r_triU])
        LD(triL[:], c_triL, w=[r_triL])
        LDC(rot_b[:], c_rot, w=[r_rot])
        G(lambda e: e.memset(ones_f[:], 1.0), w=[r_onesf])
        G(lambda e: e.memset(ones_b[:], 1.0), w=[r_onesb])
        G(lambda e: e.memset(epsb[:], EPS), w=[r_eps])

        r_gt1, r_gt2, r_A2, r_sh2 = Res(), Res(), Res(), Res()

        NR = 2
        wring = [sb(es, f"wring{i}", [128, DC, 512], BF16) for i in range(NR)]
        rring = [Res(f"wring{i}") for i in range(NR)]
        ring_i = [0]

        def load_w(src2d, ncols):
            i = ring_i[0]
            ring_i[0] = (i + 1) % NR
            t, r = wring[i], rring[i]
            half = DC // 2
            v = src2d.rearrange("(dc p) n -> p dc n", p=128)
            LDC(t[:, 0:half, 0:ncols], v[:, 0:half, :], w=[r])
            P.dma("pool", lambda e: e.dma_start(out=t[:, half:DC, 0:ncols], in_=v[:, half:DC, :]), [r], [r])
            return t, r

        class WStream:
            def __init__(self, specs):
                self.specs = specs
                self.k = 0
                self.nxt = None

            def get(self):
                cur = self.nxt if self.nxt is not None else load_w(*self.specs[self.k])
                self.k += 1
                self.nxt = load_w(*self.specs[self.k]) if self.k < len(self.specs) else None
                return cur

        bank_i = [0]

        def next_bank():
            i = bank_i[0]
            bank_i[0] = (i + 1) % 7
            return pbank[i], rbank[i]

        NS = 4
        stg = []
        rstg = [Res(f"stg{i}") for i in range(NS)]
        stg_i = [0]

        def next_stg():
            i = stg_i[0]
            stg_i[0] = (i + 1) % NS
            return stg[i], rstg[i]

        r_mod = Res("mod_d")
        r_MQT, r_MKT, r_AQT, r_GMT = Res(), Res(), Res(), Res()
        r_MKd, r_MVd, r_OGd, r_AVd = Res(), Res(), Res(), Res()

        with contextlib.ExitStack() as s0:
            s0 = Bump(90 * KB, ARENA)
            cT_f = sb(s0, "cT_f", [128, 2, DC], F32); r_cTf = Res()
            cT_b = sb(s0, "cT_b", [128, DC, 2], BF16); r_cTb = Res()
            bm2 = sb(s0, "bm2", [2, 512], F32); r_bm2 = Res()
            mrow = sb(s0, "mrow", [2, 512], F32); r_mrow = Res()
            for r_ in range(2):
                P.dma("sp", lambda e, r_=r_: e.dma_start(out=cT_f[:, r_, :], in_=cc[r_, :].rearrange("(dc p) -> p dc", p=128),
                                                         allow_slow_non_contiguous=True), [r_cTf], [r_cTf])
            for r_ in range(2):
                A(lambda e, r_=r_: e.activation(out=cT_b[:, :, r_], in_=cT_f[:, r_, :], func=AF.Silu), [r_cTf, r_cTb], [r_cTb])
            ws0 = WStream([(w_mod[:, blk * 512:(blk + 1) * 512], 512) for blk in range(6 * D // 512)])
            for blk in range(6 * D // 512):
                wt, wr = ws0.get()
                pb, rb = next_bank()
                for dc in range(DC):
                    M(lambda e, dc=dc, pb=pb, wt=wt: e.matmul(pb[0:2, :], cT_b[:, dc, :], wt[:, dc, :],
                                                                start=(dc == 0), stop=(dc == DC - 1)),
                      [r_cTb, wr], [rb])
                LD(bm2[0:1, :], b_mod[0:1, blk * 512:(blk + 1) * 512], w=[r_bm2])
                P.dma("sp", lambda e, blk=blk: e.dma_start(out=bm2[1:2, :], in_=b_mod[0:1, blk * 512:(blk + 1) * 512]),
                      [r_bm2], [r_bm2])
                V(lambda e, pb=pb: e.tensor_tensor(out=mrow[:], in0=pb[0:2, :], in1=bm2[:], op=ALU.add),
                  [rb, r_bm2], [r_mrow])
                LD(mod_d[:, blk * 512:(blk + 1) * 512], mrow[:], [r_mrow], [r_mod])

        def bc_row(ap_row, n):
            return ap_row.broadcast_to([128, n])

        P.barrier()

        uT = at(0, [128, DC, TA], BF16)
        r_uT = [Res(f"uT{c}") for c in range(NT)]
        with contextlib.ExitStack() as s1:
            s1 = Bump(72 * KB, ARENA)
            A1x = sb(s1, "A1x", [128, D], F32); r_A1x = Res()
            S1x = sb(s1, "S1x", [128, D], F32); r_S1x = Res()
            A1c = sb(s1, "A1c", [128, D], F32); r_A1c = Res()
            S1c = sb(s1, "S1c", [128, D], F32); r_S1c = Res()
            g1b = sb(s1, "g1b", [128, D], F32); r_g1b = Res()
            LD(g1b[:], bc_row(g1[0:1, :], D), w=[r_g1b])
            LD(A1x[:], bc_row(mod_d[0:1, D:2 * D], D), [r_mod], [r_A1x])
            LD(A1c[:], bc_row(mod_d[1:2, D:2 * D], D), [r_mod], [r_A1c])
            LD(S1x[:], bc_row(mod_d[0:1, 0:D], D), [r_mod], [r_S1x])
            LD(S1c[:], bc_row(mod_d[1:2, 0:D], D), [r_mod], [r_S1c])
            V(lambda e: e.scalar_tensor_tensor(out=A1x[:], in0=A1x[:], scalar=1.0, in1=g1b[:], op0=ALU.add, op1=ALU.mult),
              [r_A1x, r_g1b], [r_A1x])
            V(lambda e: e.scalar_tensor_tensor(out=A1c[:], in0=A1c[:], scalar=1.0, in1=g1b[:], op0=ALU.add, op1=ALU.mult),
              [r_A1c, r_g1b], [r_A1c])
            xt = [sb(s1, f"xt{i}", [128, D], F32) for i in range(2)]
            rxt = [Res(), Res()]
            xn = [sb(s1, f"xn{i}", [128, D], BF16) for i in range(2)]
            rxn = [Res(), Res()]
            ss = sb(s1, "ss", [128, 2], F32); r_ss = [Res(), Res()]
            for c in range(NT):
                i = c % 2
                Aw, rA, Sw, rS = (A1c, r_A1c, S1c, r_S1c) if c < cfg.NCTX else (A1x, r_A1x, S1x, r_S1x)
                LD(xt[i][:], xf[c * 128:(c + 1) * 128, :], w=[rxt[i]])
                G(lambda e, i=i: e.memset(ss[:, i:i + 1], 0.0), w=[r_ss[i]])
                A(lambda e, i=i: e.activation(out=xn[i][:], in_=xt[i][:], func=AF.Square, accum_out=ss[:, i:i + 1]),
                  [rxt[i], r_ss[i]], [rxn[i], r_ss[i]])
                A(lambda e, i=i: e.activation(out=ss[:, i:i + 1], in_=ss[:, i:i + 1], func=AF.Sqrt, bias=epsb[:], scale=1.0 / D),
                  [r_ss[i], r_eps], [r_ss[i]])
                V(lambda e, i=i: e.reciprocal(out=ss[:, i:i + 1], in_=ss[:, i:i + 1]), [r_ss[i]], [r_ss[i]])
                V(lambda e, i=i, Aw=Aw: e.scalar_tensor_tensor(out=xt[i][:], in0=xt[i][:], scalar=ss[:, i:i + 1], in1=Aw[:],
                                                              op0=ALU.mult, op1=ALU.mult),
                  [rxt[i], r_ss[i], rA], [rxt[i]])
                G(lambda e, i=i, Sw=Sw: e.tensor_tensor(out=xn[i][:], in0=xt[i][:], in1=Sw[:], op=ALU.add),
                  [rxt[i], rS], [rxn[i]])
                for half in range(DC // 8):
                    for j in range(8):
                        dc = half * 8 + j
                        M(lambda e, i=i, dc=dc, j=j: e.transpose(ptr[:, j * 128:(j + 1) * 128], xn[i][:, dc * 128:(dc + 1) * 128], ident_b[:]),
                          [rxn[i], r_identb], [r_ptr])
                    if half % 2 == 0:
                        V(lambda e, c=c, half=half: e.tensor_copy(
                            out=uT[:, half * 8:(half + 1) * 8, c * 128:(c + 1) * 128],
                            in_=ptr[:].rearrange("p (j t) -> p j t", j=8)), [r_ptr], [r_uT[c]])
                    else:
                        A(lambda e, c=c, half=half: e.activation(
                            out=uT[:, half * 8:(half + 1) * 8, c * 128:(c + 1) * 128],
                            in_=ptr[:].rearrange("p (j t) -> p j t", j=8), func=AF.Copy), [r_ptr], [r_uT[c]])
                _compact(CONSTS)

        own_blocks = [(C0 * 128 + b * TB, TB) for b in range(NB)]
        all_blocks = []
        o = 0
        while o < TA:
            n = min(512, TA - o)
            all_blocks.append((o, n))
            o += n

        def uT_res(t0, n):
            return r_uT[t0 // 128:(t0 + n + 127) // 128]

        if cfg.DEBUG:
            P.barrier()
            LD(dbg_out("dbg_uT", [128, DC, TA], BF16), uT[:], r_uT, [Res()])
        P.barrier()
        gates = at(160 * KB, [128, NT, 16], F32); r_gates = Res()
        AKT = at(72 * KB, [128, 4, TA], BF16); r_AKT = Res()

        with contextlib.ExitStack() as s2:
            s2 = Bump(90 * KB, 160 * KB)
            stg.extend(sb(s2, f"stg{i}", [128, 512], BF16) for i in range(NS))
            bT = sb(s2, "bT", [128, 72], F32); r_bT = Res()
            for (off, n, col) in ((OFF_MQ, 8, 0), (OFF_MK, 8, 8), (OFF_AQ, 16, 16), (OFF_AK, 4, 32), (OFF_MG, 32, 36)):
                P.dma("sp", lambda e, off=off, n=n, col=col: e.dma_start(
                    out=bT[:, col:col + n], in_=b_in[0, off:off + n * 128].rearrange("(j p) -> p j", p=128),
                    allow_slow_non_contiguous=True), [r_bT], [r_bT])
            brows = [sb(s2, f"brow{i}", [1, 512], BF16) for i in range(2)]
            rbrows = [Res(), Res()]
            brow_i = [0]
            bg_bc = sb(s2, "bg_bc", [128, 16], F32); r_bg = Res()
            LD(bg_bc[:], bc_row(bg16[0:1, :], 16), w=[r_bg])
            wg_b = sb(s2, "wg_b", [128, DC, 16], BF16); r_wg = Res()
            LDC(wg_b[:], wg16.rearrange("(dc p) n -> p dc n", p=128), w=[r_wg])
            gq_s = sb(s2, "gq_s", [128, 1], F32); r_gq = Res()
            gk_s = sb(s2, "gk_s", [128, 1], F32); r_gk = Res()
            LD(gq_s[:], g_q, w=[r_gq])
            LD(gk_s[:], g_k, w=[r_gk])
            cos_s = sb(s2, "cos_s", [128, TA], BF16); r_cos = Res()
            sin_s = sb(s2, "sin_s", [128, TA], BF16); r_sin = Res()
            LDC(cos_s[:], c_cos, w=[r_cos])
            LDC(sin_s[:], c_sin, w=[r_sin])
            sq = sb(s2, "sq", [128, 512], BF16); r_sq = Res()
            rstd = sb(s2, "rstd", [128, 512], F32); r_rstd = Res()
            xnb = sb(s2, "xnb", [128, 512], BF16); r_xnb = Res()
            t1 = sb(s2, "t1", [128, 512], F32); r_t1 = Res()
            t2 = sb(s2, "t2", [128, 512], F32); r_t2 = Res()

            specs2 = []
            for (off_, nch_) in ((OFF_MQ, 8), (OFF_MK, 8), (OFF_AQ, 16), (OFF_AK, 4), (OFF_MG, 32)):
                for s_ in range(0, nch_, 4):
                    nc_ = min(4, nch_ - s_) * 128
                    specs2.append((w_in[:, off_ + s_ * 128: off_ + s_ * 128 + nc_], nc_))
            for (off_, ncl_) in ((OFF_MK, MH * MQK), (OFF_MV, MH * MV), (OFF_OG, MH * MV), (OFF_AV, 4 * HD)):
                for s_ in range(0, ncl_, 512):
                    specs2.append((w_in[:, off_ + s_: off_ + s_ + 512], 512))
            ws2 = WStream(specs2)

            def fm_group(off, nchunks, blocks, evac):
                for s in range(0, nchunks, 4):
                    ncol = min(4, nchunks - s) * 128
                    wt, wr = ws2.get()
                    for jj in range(ncol // 128):
                        j = s + jj
                        for bi, (t0, n) in enumerate(blocks):
                            pb, rb = next_bank()
                            for dc in range(DC):
                                M(lambda e, dc=dc, pb=pb, wt=wt, jj=jj, t0=t0, n=n: e.matmul(
                                    pb[:, 0:n], wt[:, dc, jj * 128:(jj + 1) * 128], uT[:, dc, t0:t0 + n],
                                    start=(dc == 0), stop=(dc == DC - 1)),
                                  [wr] + uT_res(t0, n), [rb])
                            evac(j, bi, t0, n, pb, rb)
                    _compact(CONSTS + r_uT)

            def simple_evac(dst, rdst, bcol, scale):
                def f(j, bi, t0, n, pb, rb):
                    st, rs = next_stg()
                    V(lambda e: e.tensor_scalar(out=st[:, 0:n], in0=pb[:, 0:n], scalar1=bT[:, bcol + j:bcol + j + 1],
                                                scalar2=scale, op0=ALU.add, op1=ALU.mult), [rb, r_bT], [rs])
                    tl = t0 - C0 * 128
                    LD(dst[j, :, tl:tl + n], st[:, 0:n], [rs], [rdst])
                return f

            def sig_evac(dst, rdst, bcol):
                def f(j, bi, t0, n, pb, rb):
                    st, rs = next_stg()
                    A(lambda e: e.activation(out=st[:, 0:n], in_=pb[:, 0:n], func=AF.Sigmoid,
                                             bias=bT[:, bcol + j:bcol + j + 1], scale=1.0), [rb, r_bT], [rs])
                    tl = t0 - C0 * 128
                    LD(dst[j, :, tl:tl + n], st[:, 0:n], [rs], [rdst])
                return f

            def qknorm_evac(is_q):
                bcol = 16 if is_q else 32
                gs, rg = (gq_s, r_gq) if is_q else (gk_s, r_gk)

                def f(j, bi, t0, n, pb, rb):
                    V(lambda e: e.tensor_scalar(out=t1[:, 0:n], in0=pb[:, 0:n], scalar1=bT[:, bcol + j:bcol + j + 1],
                                                scalar2=None, op0=ALU.add), [rb, r_bT], [r_t1])
                    A(lambda e: e.activation(out=sq[:, 0:n], in_=t1[:, 0:n], func=AF.Square), [r_t1], [r_sq])
                    p2, r2 = next_bank()
                    M(lambda e: e.matmul(p2[:, 0:n], ones_b[:], sq[:, 0:n], start=True, stop=True), [r_onesb, r_sq], [r2])
                    A(lambda e: e.activation(out=rstd[:, 0:n], in_=p2[:, 0:n], func=AF.Sqrt, bias=epsb[:], scale=1.0 / HD),
                      [r2, r_eps], [r_rstd])
                    V(lambda e: e.reciprocal(out=rstd[:, 0:n], in_=rstd[:, 0:n]), [r_rstd], [r_rstd])
                    V(lambda e: e.scalar_tensor_tensor(out=xnb[:, 0:n], in0=t1[:, 0:n], scalar=gs[:, 0:1], in1=rstd[:, 0:n],
                                                       op0=ALU.mult, op1=ALU.mult), [r_t1, rg, r_rstd], [r_xnb])
                    p3, r3 = next_bank()
                    M(lambda e: e.matmul(p3[:, 0:n], rot_b[:], xnb[:, 0:n], start=True, stop=True), [r_rot, r_xnb], [r3])
                    V(lambda e: e.tensor_tensor(out=t2[:, 0:n], in0=p3[:, 0:n], in1=sin_s[:, t0:t0 + n], op=ALU.mult),
                      [r3, r_sin], [r_t2])
                    G(lambda e: e.tensor_tensor(out=t1[:, 0:n], in0=xnb[:, 0:n], in1=cos_s[:, t0:t0 + n], op=ALU.mult),
                      [r_xnb, r_cos], [r_t1])
                    if is_q:
                        st, rs = next_stg()
                        G(lambda e: e.tensor_tensor(out=st[:, 0:n], in0=t1[:, 0:n], in1=t2[:, 0:n], op=ALU.add),
                          [r_t1, r_t2], [rs])
                        tl = t0 - C0 * 128
                        LD(AQT[j, :, tl:tl + n], st[:, 0:n], [rs], [r_AQT])
                    else:
                        G(lambda e: e.tensor_tensor(out=AKT[:, j, t0:t0 + n], in0=t1[:, 0:n], in1=t2[:, 0:n], op=ALU.add),
                          [r_t1, r_t2], [r_AKT])
                return f

            fm_group(OFF_MQ, 8, own_blocks, simple_evac(MQT, r_MQT, 0, 1.0 / 16.0))
            fm_group(OFF_MK, 8, own_blocks, simple_evac(MKT, r_MKT, 8, 1.0))
            fm_group(OFF_AQ, 16, own_blocks, qknorm_evac(True))
            fm_group(OFF_AK, 4, all_blocks, qknorm_evac(False))
            fm_group(OFF_MG, 32, own_blocks, sig_evac(GMT, r_GMT, 36))

            def tm_group(off, ncols, chunks, dst, rdst, sig, own_only):
                for s in range(0, ncols, 512):
                    wt, wr = ws2.get()
                    bi_ = brow_i[0]
                    brow_i[0] = 1 - bi_
                    brow, r_brow = brows[bi_], rbrows[bi_]
                    LDC(brow[:], b_in[0:1, off + s: off + s + 512], w=[r_brow])
                    for c in chunks:
                        pb, rb = next_bank()
                        for dc in range(DC):
                            M(lambda e, dc=dc, pb=pb, wt=wt, c=c: e.matmul(
                                pb[:, :], uT[:, dc, c * 128:(c + 1) * 128], wt[:, dc, :], start=(dc == 0), stop=False),
                              [wr, r_uT[c]], [rb])
                        M(lambda e, pb=pb, brow=brow: e.matmul(pb[:, :], ones_b[0:1, :], brow[0:1, :],
                                                               start=False, stop=True), [r_onesb, r_brow], [rb])
                        st, rs = next_stg()
                        if sig:
                            A(lambda e, pb=pb, st=st: e.activation(out=st[:], in_=pb[:], func=AF.Sigmoid), [rb], [rs])
                        else:
                            V(lambda e, pb=pb, st=st: e.tensor_copy(out=st[:], in_=pb[:]), [rb], [rs])
                        cl = c - C0 if own_only else c
                        LD(dst[cl, :, s:s + 512], st[:], [rs], [rdst])
                    _compact(CONSTS + r_uT)

            allc = list(range(NT))
            ownc = list(range(C0, NT))
            tm_group(OFF_MK, MH * MQK, allc, MKd, r_MKd, False, False)
            tm_group(OFF_MV, MH * MV, allc, MVd, r_MVd, False, False)
            tm_group(OFF_OG, MH * MV, ownc, OGd, r_OGd, True, True)
            tm_group(OFF_AV, 4 * HD, allc, AVd, r_AVd, False, False)
            for c in allc:
                pb, rb = next_bank()
                for dc in range(DC):
                    M(lambda e, dc=dc, pb=pb, c=c: e.matmul(pb[:, 0:16], uT[:, dc, c * 128:(c + 1) * 128], wg_b[:, dc, :],
                                                            start=(dc == 0), stop=(dc == DC - 1)), [r_wg, r_uT[c]], [rb])
                V(lambda e, pb=pb, c=c: e.tensor_tensor(out=gates[:, c, :], in0=pb[:, 0:16], in1=bg_bc[:], op=ALU.add),
                  [rb, r_bg], [r_gates])
        _compact(CONSTS + r_uT)

        if cfg.DEBUG:
            P.barrier()
            LD(dbg_out("dbg_gates", [128, NT, 16], F32), gates[:], [r_gates], [Res()])
            LD(dbg_out("dbg_AKT", [128, 4, TA], BF16), AKT[:], [r_AKT], [Res()])
        P.barrier()
        MOT = at(0, [128, 16, T], BF16); r_MOT = Res()
        AOT = at(32 * KB, [128, 16, T], BF16); r_AOT = Res()

        with contextlib.ExitStack() as s3:
            s3 = Bump(90 * KB, 160 * KB)
            s3b = Bump(32 * KB, 72 * KB)
            lf = sb(s3, "lf", [128, NT, 8], F32); r_lf = Res()
            A(lambda e: e.activation(out=lf[:], in_=gates[:, :, 8:16], func=AF.Exp, scale=-1.0), [r_gates], [r_lf])
            A(lambda e: e.activation(out=lf[:], in_=lf[:], func=AF.Ln, bias=1.0, scale=1.0), [r_lf], [r_lf])
            V(lambda e: e.tensor_scalar(out=lf[:], in0=lf[:], scalar1=-1.0, scalar2=None, op0=ALU.mult), [r_lf], [r_lf])
            rr = sb(s3, "rr", [128, NT, 8], F32); r_rr = Res()
            einv = sb(s3, "einv", [128, NT, 8], F32); r_einv = Res()
            etot = sb(s3, "etot", [128, NT, 8], F32); r_etot = Res()
            for c in range(NT):
                pb, rb = next_bank()
                M(lambda e, pb=pb, c=c: e.matmul(pb[:, 0:4], triU[:], lf[:, c, 0:4], start=True, stop=True), [r_triU, r_lf], [rb])
                M(lambda e, pb=pb, c=c: e.matmul(pb[:, 4:8], triL[:], lf[:, c, 4:8], start=True, stop=True), [r_triL, r_lf], [rb])
                M(lambda e, pb=pb, c=c: e.matmul(pb[:, 8:16], ones_f[:], lf[:, c, 0:8], start=True, stop=True), [r_onesf, r_lf], [rb])
                V(lambda e, pb=pb, c=c: e.tensor_tensor(out=rr[:, c, :], in0=gates[:, c, 0:8], in1=pb[:, 0:8], op=ALU.subtract),
                  [rb, r_gates], [r_rr])
                A(lambda e, pb=pb, c=c: e.activation(out=einv[:, c, :], in_=pb[:, 0:8], func=AF.Exp, scale=-1.0), [rb], [r_einv])
                A(lambda e, pb=pb, c=c: e.activation(out=etot[:, c, :], in_=pb[:, 8:16], func=AF.Exp), [rb], [r_etot])
            A(lambda e: e.activation(out=rr[:], in_=rr[:], func=AF.Exp), [r_rr], [r_rr])
            _compact(CONSTS)

            gmh_bc = sb(s3, "gmh_bc", [128, MV], F32); r_gmh = Res()
            qT = sb(s3, "qT", [128, 2, T], BF16); r_qT = Res()
            kT = sb(s3, "kT", [128, 2, T], BF16); r_kT = Res()
            ktm = sb(s3, "ktm", [128, NT, MQK], BF16); r_ktm = Res()
            vtm = sb(s3b, "vtm", [128, NT, MV], BF16); r_vtm = Res()
            ogt = sb(s3, "ogt", [128, NOWN, MV], BF16); r_ogt = Res()
            hA = sb(s3b, "hA", [128, NOWN, MV], F32); r_hA = Res()
            Cst = sb(s3, "Cst", [128, 2, MV + 1], F32); r_Cst = Res()
            Cb = sb(s3, "Cb", [128, 2, MV + 1], BF16); r_Cb = Res()
            kr = sb(s3, "kr", [128, MQK], BF16); r_kr = Res()
            PT = sb(s3, "PT", [128, 128], BF16); r_PT = Res()
            dtmp = sb(s3, "dtmp", [128, 2, MV + 1], F32); r_dtmp = Res()
            den = sb(s3, "den", [128, 2], F32); r_den = Res()
            hs = sb(s3, "hs", [128, MV], F32); r_hs = Res()
            hjunk = sb(s3, "hjunk", [128, MV], BF16); r_hjunk = Res()
            hss = sb(s3, "hss", [128, 1], F32); r_hss = Res()
            mo = sb(s3, "mo", [128, MV], BF16); r_mo = Res()

            for h in range(MH):
                for j in range(2):
                    LD(qT[:, j, :], MQT[2 * h + j], [r_MQT], [r_qT])
                    LD(kT[:, j, :], MKT[2 * h + j], [r_MKT], [r_kT])
                LD(ktm[:], MKd[:, :, h * MQK:(h + 1) * MQK].rearrange("c p n -> p c n"), [r_MKd], [r_ktm])
                LD(vtm[:], MVd[:, :, h * MV:(h + 1) * MV].rearrange("c p n -> p c n"), [r_MVd], [r_vtm])
                LD(ogt[:], OGd[:, :, h * MV:(h + 1) * MV].rearrange("c p n -> p c n"), [r_OGd], [r_ogt])
                LD(gmh_bc[:], bc_row(g_mh[0:1, h * MV:(h + 1) * MV], MV), w=[r_gmh])
                for dirn in range(2):
                    gi = h + 4 * dirn
                    mask, rmask = (triU, r_triU) if dirn == 0 else (triL, r_triL)
                    if dirn == 0:
                        order = list(range(NT))
                    else:
                        order = [1, 0] if cfg.NCTX == 2 else list(range(cfg.NCTX - 1, -1, -1))
                        order = order + list(range(NT - 1, C0 - 1, -1))
                    G(lambda e: e.memset(Cst[:], 0.0), w=[r_Cst])
                    G(lambda e: e.memset(Cb[:], 0.0), w=[r_Cb])
                    for idx, c in enumerate(order):
                        own = c >= C0
                        last = idx == len(order) - 1
                        co = c - C0
                        if own:
                            pS, rS = next_bank()
                            for j in range(2):
                                M(lambda e, j=j, pS=pS, co=co: e.matmul(pS[:, 0:128], kT[:, j, co * 128:(co + 1) * 128],
                                                                         qT[:, j, co * 128:(co + 1) * 128], start=(j == 0), stop=(j == 1)),
                                  [r_kT, r_qT], [rS])
                            V(lambda e, pS=pS, c=c, gi=gi, mask=mask: e.scalar_tensor_tensor(
                                out=PT[:], in0=pS[:, 0:128], scalar=rr[:, c, gi:gi + 1], in1=mask[:], op0=ALU.mult, op1=ALU.mult),
                              [rS, r_rr, rmask], [r_PT])
                            pN, rN = next_bank()
                            pD, rD = next_bank()
                            M(lambda e, pN=pN, c=c: e.matmul(pN[:, :], PT[:], vtm[:, c, :], start=True, stop=False), [r_PT, r_vtm], [rN])
                            for j in range(2):
                                M(lambda e, pN=pN, j=j, co=co: e.matmul(pN[:, :], qT[:, j, co * 128:(co + 1) * 128], Cb[:, j, 0:MV],
                                                                         start=False, stop=(j == 1)), [r_qT, r_Cb], [rN])
                            M(lambda e, pD=pD: e.matmul(pD[:, 0:1], PT[:], ones_b[:, 0:1], start=True, stop=False), [r_PT, r_onesb], [rD])
                            for j in range(2):
                                M(lambda e, pD=pD, j=j, co=co: e.matmul(pD[:, 0:1], qT[:, j, co * 128:(co + 1) * 128], Cb[:, j, MV:MV + 1],
                                                                         start=False, stop=(j == 1)), [r_qT, r_Cb], [rD])
                            A(lambda e, pD=pD: e.activation(out=den[:, 0:1], in_=pD[:, 0:1], func=AF.Abs), [rD], [r_den])
                            V(lambda e, c=c, gi=gi: e.tensor_tensor(out=den[:, 0:1], in0=den[:, 0:1], in1=einv[:, c, gi:gi + 1], op=ALU.max),
                              [r_den, r_einv], [r_den])
                            V(lambda e: e.reciprocal(out=den[:, 1:2], in_=den[:, 0:1]), [r_den], [r_den])
                            if dirn == 0:
                                V(lambda e, pN=pN, co=co: e.tensor_scalar(out=hA[:, co, :], in0=pN[:, :], scalar1=den[:, 1:2], scalar2=None,
                                                                           op0=ALU.mult), [rN, r_den], [r_hA])
                            else:
                                V(lambda e, pN=pN, co=co: e.scalar_tensor_tensor(out=hs[:], in0=pN[:, :], scalar=den[:, 1:2], in1=hA[:, co, :],
                                                                                  op0=ALU.mult, op1=ALU.add), [rN, r_den, r_hA], [r_hs])
                                G(lambda e: e.memset(hss[:], 0.0), w=[r_hss])
                                A(lambda e: e.activation(out=hjunk[:], in_=hs[:], func=AF.Square, accum_out=hss[:]), [r_hs, r_hss], [r_hjunk, r_hss])
                                A(lambda e: e.activation(out=hss[:], in_=hss[:], func=AF.Sqrt, bias=epsb[:], scale=1.0 / MV), [r_hss, r_eps], [r_hss])
                                V(lambda e: e.reciprocal(out=hss[:], in_=hss[:]), [r_hss], [r_hss])
                                V(lambda e, h=h: e.scalar_tensor_tensor(out=hs[:], in0=hs[:], scalar=hss[:, 0:1], in1=gmh_bc[:],
                                                                       op0=ALU.mult, op1=ALU.mult), [r_hs, r_hss, r_gmh], [r_hs])
                                G(lambda e, co=co: e.tensor_tensor(out=mo[:], in0=hs[:], in1=ogt[:, co, :], op=ALU.mult), [r_hs, r_ogt], [r_mo])
                                for j in range(4):
                                    M(lambda e, j=j: e.transpose(ptr[:, j * 128:(j + 1) * 128], mo[:, j * 128:(j + 1) * 128], ident_b[:]),
                                      [r_mo, r_identb], [r_ptr])
                                V(lambda e, h=h, co=co: e.tensor_copy(out=MOT[:, 4 * h:4 * h + 4, co * 128:(co + 1) * 128],
                                                                     in_=ptr[:, 0:512].rearrange("p (j t) -> p j t", j=4)), [r_ptr], [r_MOT])
                        if not last:
                            V(lambda e, c=c, gi=gi: e.tensor_scalar(out=kr[:], in0=ktm[:, c, :], scalar1=rr[:, c, gi:gi + 1], scalar2=None,
                                                                    op0=ALU.mult), [r_ktm, r_rr], [r_kr])
                            for j in range(2):
                                pC, rC = next_bank()
                                pn, rn = next_bank()
                                M(lambda e, pC=pC, j=j, c=c: e.matmul(pC[:, :], kr[:, j * 128:(j + 1) * 128], vtm[:, c, :], start=True, stop=True),
                                  [r_kr, r_vtm], [rC])
                                M(lambda e, pn=pn, j=j: e.matmul(pn[:, 0:1], kr[:, j * 128:(j + 1) * 128], ones_b[:, 0:1], start=True, stop=True),
                                  [r_kr, r_onesb], [rn])
                                V(lambda e, pC=pC, j=j: e.tensor_tensor(out=dtmp[:, j, 0:MV], in0=pC[:, :], in1=Cst[:, j, 0:MV], op=ALU.add),
                                  [rC, r_Cst], [r_dtmp])
                                V(lambda e, pn=pn, j=j: e.tensor_tensor(out=dtmp[:, j, MV:MV + 1], in0=pn[:, 0:1], in1=Cst[:, j, MV:MV + 1], op=ALU.add),
                                  [rn, r_Cst], [r_dtmp])
                            V(lambda e, c=c, gi=gi: e.tensor_scalar(out=Cst[:], in0=dtmp[:], scalar1=etot[:, c, gi:gi + 1], scalar2=None, op0=ALU.mult),
                              [r_dtmp, r_etot], [r_Cst])
                            A(lambda e: e.activation(out=Cb[:], in_=Cst[:], func=AF.Copy), [r_Cst], [r_Cb])
                    _compact(CONSTS + [r_rr, r_einv, r_etot, r_vtm, r_ktm, r_qT, r_kT])

        P.barrier()
        with contextlib.ExitStack() as s4:
            s4 = Bump(90 * KB, ARENA)
            vat = sb(s4, "vat", [128, NT, HD], BF16); r_vat = Res()
            qh = sb(s4, "qh", [128, T], BF16); r_qh = Res()
            PTa = [sb(s4, f"PTa{i}", [128, 512], BF16) for i in range(2)]
            rPTa = [Res(), Res()]
            rsum = sb(s4, "rsum", [128, 512], F32); r_rsum = Res()
            sc = 1.0 / math.sqrt(HD)
            for kh in range(4):
                LD(vat[:], AVd[:, :, kh * HD:(kh + 1) * HD].rearrange("c p n -> p c n"), [r_AVd], [r_vat])
                for g in range(4):
                    head = kh * 4 + g
                    LD(qh[:], AQT[head], [r_AQT], [r_qh])
                    for b in range(NB):
                        oz = 2 * ((head * NB + b) % 2)
                        pO, rO = pbank[oz], rbank[oz]
                        pZ, rZ = pbank[oz + 1], rbank[oz + 1]
                        def qk(c, b=b, kh=kh):
                            si = 4 + (c % 3)
                            pS, rS = pbank[si], rbank[si]
                            M(lambda e, pS=pS, c=c, b=b, kh=kh: e.matmul(pS[:, 0:TB], AKT[:, kh, c * 128:(c + 1) * 128], qh[:, b * TB:(b + 1) * TB],
                                                                        start=True, stop=True), [r_AKT, r_qh], [rS])
                        qk(0)
                        if NT > 1:
                            qk(1)
                        for c in range(NT):
                            if c + 2 < NT:
                                qk(c + 2)
                            si = 4 + (c % 3)
                            pS, rS = pbank[si], rbank[si]
                            i = c % 2
                            A(lambda e, pS=pS, i=i: e.activation(out=PTa[i][:, 0:TB], in_=pS[:, 0:TB], func=AF.Exp, scale=sc), [rS], [rPTa[i]])
                            M(lambda e, pO=pO, c=c, i=i: e.matmul(pO[:, 0:TB], vat[:, c, :], PTa[i][:, 0:TB], start=(c == 0), stop=(c == NT - 1)),
                              [r_vat, rPTa[i]], [rO])
                            M(lambda e, pZ=pZ, c=c, i=i: e.matmul(pZ[:, 0:TB], ones_b[:], PTa[i][:, 0:TB], start=(c == 0), stop=(c == NT - 1)),
                              [r_onesb, rPTa[i]], [rZ])
                        V(lambda e, pZ=pZ: e.reciprocal(out=rsum[:, 0:TB], in_=pZ[:, 0:TB]), [rZ], [r_rsum])
                        V(lambda e, pO=pO, head=head, b=b: e.tensor_tensor(out=AOT[:, head, b * TB:(b + 1) * TB], in0=pO[:, 0:TB], in1=rsum[:, 0:TB],
                                                                          op=ALU.mult), [rO, r_rsum], [r_AOT])
                    _compact(CONSTS + [r_AKT, r_vat])

        P.barrier()

        acc = at(0, [128, NOWN, D], F32); r_acc = [Res(f"acc{c}") for c in range(NOWN)]
        u2tm = at(64 * KB, [128, NOWN, D], BF16); r_u2T = Res()
        Gd = at(96 * KB, [128, NOWN, NE], F32); r_Gd = Res()
        posm = at(97 * KB, [128, NOWN, NE], F32); r_posm = Res()
        gt1_bc = at(64 * KB, [128, D], F32)
        gt2_bc = at(98 * KB, [128, D], F32)

        with contextlib.ExitStack() as s5:
            s5 = Bump(122 * KB, ARENA)
            zT = at(90 * KB, [128, 16, T], BF16); r_zT = Res()
            gmt = sb(s5, "gmt", [128, 2, T], BF16); r_gmt = Res()
            za = sb(s5, "za", [128, 512], F32); r_za = Res()
            zb = sb(s5, "zb", [128, 512], F32); r_zb = Res()
            for s in range(4):
                wm, rwm = load_w(w_br_m[:, s * 512:(s + 1) * 512], 512)
                wa, rwa = load_w(w_br_a[:, s * 512:(s + 1) * 512], 512)
                for jj in range(4):
                    j = s * 4 + jj
                    LD(gmt[:, 0, :], GMT[j], [r_GMT], [r_gmt])
                    LD(gmt[:, 1, :], GMT[16 + j], [r_GMT], [r_gmt])
                    for b in range(NB):
                        pm, rm = next_bank()
                        pa, ra = next_bank()
                        for k in range(16):
                            M(lambda e, pm=pm, wm=wm, jj=jj, k=k, b=b: e.matmul(pm[:, 0:TB], wm[:, k, jj * 128:(jj + 1) * 128], MOT[:, k, b * TB:(b + 1) * TB],
                                                                               start=(k == 0), stop=(k == 15)), [rwm, r_MOT], [rm])
                        for k in range(16):
                            M(lambda e, pa=pa, wa=wa, jj=jj, k=k, b=b: e.matmul(pa[:, 0:TB], wa[:, k, jj * 128:(jj + 1) * 128], AOT[:, k, b * TB:(b + 1) * TB],
                                                                               start=(k == 0), stop=(k == 15)), [rwa, r_AOT], [ra])
                        V(lambda e, pm=pm, b=b: e.tensor_tensor(out=za[:, 0:TB], in0=pm[:, 0:TB], in1=gmt[:, 0, b * TB:(b + 1) * TB], op=ALU.mult),
                          [rm, r_gmt], [r_za])
                        V(lambda e, pa=pa, b=b: e.tensor_tensor(out=zb[:, 0:TB], in0=pa[:, 0:TB], in1=gmt[:, 1, b * TB:(b + 1) * TB], op=ALU.mult),
                          [ra, r_gmt], [r_zb])
                        G(lambda e, j=j, b=b: e.tensor_tensor(out=zT[:, j, b * TB:(b + 1) * TB], in0=za[:, 0:TB], in1=zb[:, 0:TB], op=ALU.add),
                          [r_za, r_zb], [r_zT])
                _compact(CONSTS + [r_MOT, r_AOT])
            if cfg.DEBUG:
                P.barrier()
                LD(dbg_out("dbg_MOT", [128, 16, T], BF16), MOT[:], [r_MOT], [Res()])
                LD(dbg_out("dbg_AOT", [128, 16, T], BF16), AOT[:], [r_AOT], [Res()])
                LD(dbg_out("dbg_zT", [128, 16, T], BF16), zT[:], [r_zT], [Res()])
            P.barrier()
            for c in range(NOWN):
                LD(acc[:, c, :], xf[(C0 + c) * 128:(C0 + c + 1) * 128, :], w=[r_acc[c]])
            LD(gt1_bc[:], bc_row(mod_d[0:1, 2 * D:3 * D], D), [r_mod], [r_gt1])
            for db in range(4):
                wo, rwo = load_w(w_out[:, db * 512:(db + 1) * 512], 512)
                for c in range(NOWN):
                    pb, rb = next_bank()
                    for k in range(16):
                        M(lambda e, pb=pb, wo=wo, k=k, c=c: e.matmul(pb[:, :], zT[:, k, c * 128:(c + 1) * 128], wo[:, k, :], start=(k == 0), stop=(k == 15)),
                          [rwo, r_zT], [rb])
                    V(lambda e, pb=pb, db=db: e.tensor_tensor(out=za[:], in0=pb[:], in1=gt1_bc[:, db * 512:(db + 1) * 512], op=ALU.mult),
                      [rb, r_gt1], [r_za])
                    G(lambda e, c=c, db=db: e.tensor_tensor(out=acc[:, c, db * 512:(db + 1) * 512], in0=acc[:, c, db * 512:(db + 1) * 512], in1=za[:], op=ALU.add),
                      [r_za, r_acc[c]], [r_acc[c]])
                _compact(CONSTS + [r_zT, r_gt1])
        P.barrier()

        if cfg.DEBUG:
            LD(dbg_out("dbg_hx", [128, NOWN, D], F32), acc[:], r_acc, [Res()])
            P.barrier()
        with contextlib.ExitStack() as s5b:
            s5b = Bump(98 * KB, ARENA)
            Mk_f = sb(s5b, "Mk_f", [128, NOWN, NE], F32); r_Mkf = Res()
            Mk_b = sb(s5b, "Mk_b", [128, NOWN, NE], BF16); r_Mkb = Res()
            triSU_b = sb(s5b, "triSU_b", [128, 128], BF16); r_triSU = Res()
            LDC(triSU_b[:], c_triSU, w=[r_triSU])
            A2_bc = sb(s5b, "A2_bc", [128, D], F32)
            sh2_bc = sb(s5b, "sh2_bc", [128, D], F32)
            g2b = sb(s5b, "g2b", [128, D], F32); r_g2b = Res()
            LD(g2b[:], bc_row(g2[0:1, :], D), w=[r_g2b])
            LD(sh2_bc[:], bc_row(mod_d[0:1, 3 * D:4 * D], D), [r_mod], [r_sh2])
            LD(A2_bc[:], bc_row(mod_d[0:1, 4 * D:5 * D], D), [r_mod], [r_A2])
            V(lambda e: e.scalar_tensor_tensor(out=A2_bc[:], in0=A2_bc[:], scalar=1.0, in1=g2b[:], op0=ALU.add, op1=ALU.mult),
              [r_A2, r_g2b], [r_A2])
            u2f = sb(s5b, "u2f", [128, D], F32); r_u2f = Res()
            u2b = sb(s5b, "u2b", [128, D], BF16); r_u2b = Res()
            u2Tf = sb(s5b, "u2Tf", [128, 4, 128], F32); r_u2Tf = Res()
            wr_f = sb(s5b, "wr_f", [128, DC, NE], F32); r_wr = Res()
            br_bc = sb(s5b, "br_bc", [128, NE], F32); r_br = Res()
            lg = sb(s5b, "lg", [128, NE], F32); r_lg = Res()
            mx8 = sb(s5b, "mx8", [128, 8], F32); r_mx8 = Res()
            nmx = sb(s5b, "nmx", [128, 1], F32); r_nmx = Res()
            ex = sb(s5b, "ex", [128, NE], F32); r_ex = Res()
            msk = sb(s5b, "msk", [128, NE], F32); r_msk = Res()
            esum = sb(s5b, "esum", [128, 1], F32); r_esum = Res()
            ss2 = sb(s5b, "ss2", [128, 1], F32); r_ss2 = Res()
            LD(wr_f[:], w_router.rearrange("(dc p) n -> p dc n", p=128), w=[r_wr])
            LD(br_bc[:], bc_row(b_router[0:1, :], NE), w=[r_br])
            for c in range(NOWN):
                G(lambda e: e.memset(ss2[:], 0.0), w=[r_ss2])
                A(lambda e, c=c: e.activation(out=u2b[:], in_=acc[:, c, :], func=AF.Square, accum_out=ss2[:]), [r_acc[c], r_ss2], [r_u2b, r_ss2])
                A(lambda e: e.activation(out=ss2[:], in_=ss2[:], func=AF.Sqrt, bias=epsb[:], scale=1.0 / D), [r_ss2, r_eps], [r_ss2])
                V(lambda e: e.reciprocal(out=ss2[:], in_=ss2[:]), [r_ss2], [r_ss2])
                V(lambda e, c=c: e.scalar_tensor_tensor(out=u2f[:], in0=acc[:, c, :], scalar=ss2[:, 0:1], in1=A2_bc[:], op0=ALU.mult, op1=ALU.mult),
                  [r_acc[c], r_ss2, r_A2], [r_u2f])
                V(lambda e: e.tensor_tensor(out=u2f[:], in0=u2f[:], in1=sh2_bc[:], op=ALU.add), [r_u2f, r_sh2], [r_u2f])
                G(lambda e, c=c: e.tensor_copy(out=u2tm[:, c, :], in_=u2f[:]), [r_u2f], [r_u2T])
                pl, rl = next_bank()
                for q4 in range(DC // 4):
                    pb, rb = next_bank()
                    if pb is pl:
                        pb, rb = next_bank()
                    for j in range(4):
                        dc = q4 * 4 + j
                        M(lambda e, pb=pb, dc=dc, j=j: e.transpose(pb[:, j * 128:(j + 1) * 128], u2f[:, dc * 128:(dc + 1) * 128], ident_f[:]),
                          [r_u2f, r_identf], [rb])
                    A(lambda e, pb=pb: e.activation(out=u2Tf[:], in_=pb[:].rearrange("p (j t) -> p j t", j=4), func=AF.Copy),
                      [rb], [r_u2Tf])
                    for j in range(4):
                        dc = q4 * 4 + j
                        M(lambda e, pl=pl, dc=dc, j=j: e.matmul(pl[:, 0:NE], u2Tf[:, j, :], wr_f[:, dc, :], start=(dc == 0), stop=(dc == DC - 1)),
                          [r_u2Tf, r_wr], [rl])
                V(lambda e, pl=pl: e.tensor_tensor(out=lg[:], in0=pl[:, 0:NE], in1=br_bc[:], op=ALU.add), [rl, r_br], [r_lg])
                V(lambda e: e.max(out=mx8[:], in_=lg[:]), [r_lg], [r_mx8])
                V(lambda e: e.tensor_scalar(out=nmx[:], in0=mx8[:, 0:1], scalar1=-1.0, scalar2=None, op0=ALU.mult), [r_mx8], [r_nmx])
                A(lambda e: e.activation(out=ex[:], in_=lg[:], func=AF.Exp, bias=nmx[:], scale=1.0), [r_lg, r_nmx], [r_ex])
                V(lambda e: e.tensor_scalar(out=msk[:], in0=lg[:], scalar1=mx8[:, cfg.TOPK - 1:cfg.TOPK], scalar2=None, op0=ALU.is_ge),
                  [r_lg, r_mx8], [r_msk])
                V(lambda e: e.tensor_tensor(out=ex[:], in0=ex[:], in1=msk[:], op=ALU.mult), [r_ex, r_msk], [r_ex])
                G(lambda e, c=c: e.tensor_copy(out=Mk_f[:, c, :], in_=msk[:]), [r_msk], [r_Mkf])
                G(lambda e, c=c: e.tensor_copy(out=Mk_b[:, c, :], in_=msk[:]), [r_msk], [r_Mkb])
                V(lambda e: e.tensor_reduce(out=esum[:], in_=ex[:], axis=mybir.AxisListType.X, op=ALU.add), [r_ex], [r_esum])
                V(lambda e: e.reciprocal(out=esum[:], in_=esum[:]), [r_esum], [r_esum])
                V(lambda e, c=c: e.tensor_scalar(out=Gd[:, c, :], in0=ex[:], scalar1=esum[:, 0:1], scalar2=None, op0=ALU.mult),
                  [r_ex, r_esum], [r_Gd])
                _compact(CONSTS + [r_wr, r_br, r_A2, r_sh2])
            for c in range(NOWN):
                pp, rp = next_bank()
                for c2 in range(c):
                    M(lambda e, pp=pp, c2=c2: e.matmul(pp[:, 0:NE], ones_b[:], Mk_b[:, c2, :], start=(c2 == 0), stop=False),
                      [r_onesb, r_Mkb], [rp])
                M(lambda e, pp=pp, c=c: e.matmul(pp[:, 0:NE], triSU_b[:], Mk_b[:, c, :], start=(c == 0), stop=True),
                  [r_triSU, r_Mkb], [rp])
                V(lambda e, pp=pp, c=c: e.scalar_tensor_tensor(out=posm[:, c, :], in0=pp[:, 0:NE], scalar=1.0, in1=Mk_f[:, c, :],
                                                               op0=ALU.add, op1=ALU.mult), [rp, r_Mkf], [r_posm])
            V(lambda e: e.tensor_scalar(out=posm[:], in0=posm[:], scalar1=-1.0, scalar2=None, op0=ALU.add), [r_posm], [r_posm])

        if cfg.DEBUG:
            LD(dbg_out("dbg_Gd", [128, NOWN, NE], F32), Gd[:], [r_Gd], [Res()])
            LD(dbg_out("dbg_u2tm", [128, NOWN, D], BF16), u2tm[:], [r_u2T], [Res()])
            LD(dbg_out("dbg_posm", [128, NOWN, NE], F32), posm[:], [r_posm], [Res()])
        P.barrier()
        if True:
            CAP = 512
            s6 = Bump(106 * KB, ARENA)
            LD(gt2_bc[:], bc_row(mod_d[0:1, 5 * D:6 * D], D), [r_mod], [r_gt2])
            iota_f = sb(s6, "iota_f", [128, CAP], F32); r_iota = Res()
            LD(iota_f[:], c_iota, w=[r_iota])
            STs = sb(s6, "STs", [128, CAP // 128, T], BF16); r_ST = Res()
            xT = sb(s6, "xT", [128, DC, CAP], BF16); r_xT = Res()
            hid_off = s6.off
            hidT = sb(s6, "hidT", [128, FC, CAP], BF16); r_hid = Res()
            Ys = sb(s6, "Ys", [128, CAP // 128, D], BF16); r_Ys = Res()
            assert NOWN <= FC
            Ssel = at(hid_off, [128, NOWN, CAP], BF16); r_S = r_hid
            bgu = [sb(s6, f"bgu{i}", [128, 2 * FC], F32) for i in range(2)]
            rbgu = [Res(), Res()]
            bdn = [sb(s6, f"bdn{i}", [1, 512], BF16) for i in range(2)]
            rbdn = [Res(), Res()]
            bdn_i = [0]
            gg = sb(s6, "gg", [128, CAP], F32); r_gg = Res()
            sg = sb(s6, "sg", [128, CAP], BF16); r_sg = Res()
            uu = sb(s6, "uu", [128, CAP], BF16); r_uu = Res()
            jobs = []
            for ex_i in range(NE):
                for s in range(0, FC, 2):
                    jobs.append(("gu", ex_i, s))
                for db in range(4):
                    jobs.append(("dn", ex_i, db))

            def issue(job):
                kind, ex_i, k = job
                i0 = ring_i[0]
                ring_i[0] = (i0 + 1) % NR
                wt, wr = wring[i0], rring[i0]
                if kind == "gu":
                    vg = w_gu[ex_i][:, k * 128:(k + 2) * 128].rearrange("(dc p) n -> p dc n", p=128)
                    vu = w_gu[ex_i][:, DFF + k * 128:DFF + (k + 2) * 128].rearrange("(dc p) n -> p dc n", p=128)
                    LDC(wt[:, :, 0:256], vg, w=[wr])
                    P.dma("pool", lambda e: e.dma_start(out=wt[:, :, 256:512], in_=vu), [wr], [wr])
                else:
                    vd = w_dn[ex_i][:, k * 512:(k + 1) * 512].rearrange("(dc p) n -> p dc n", p=128)
                    LDC(wt[:, 0:FC, :], vd, w=[wr])
                    bi_ = bdn_i[0]
                    bdn_i[0] = 1 - bi_
                    LDC(bdn[bi_][:], b_dn[ex_i:ex_i + 1, k * 512:(k + 1) * 512], w=[rbdn[bi_]])
                    bd_q.append((bdn[bi_], rbdn[bi_]))
                return wt, wr

            bd_q = []

            def prologue(ex_i):
                pi = ex_i % 2
                P.dma("sp", lambda e: e.dma_start(out=bgu[pi][:], in_=b_gu[ex_i, :].rearrange("(j p) -> p j", p=128),
                                                  allow_slow_non_contiguous=True), [], [rbgu[pi]])
                for c in range(NOWN):
                    V(lambda e, c=c: e.tensor_scalar(out=Ssel[:, c, :], in0=iota_f[:], scalar1=posm[:, c, ex_i:ex_i + 1], scalar2=None,
                                                     op0=ALU.is_equal), [r_iota, r_posm], [r_S])
                for sbk in range(CAP // 128):
                    for c in range(NOWN):
                        M(lambda e, c=c, sbk=sbk: e.transpose(ptr[:, c * 128:(c + 1) * 128], Ssel[:, c, sbk * 128:(sbk + 1) * 128], ident_b[:]),
                          [r_S, r_identb], [r_ptr])
                    A(lambda e, sbk=sbk: e.activation(out=STs[:, sbk, :], in_=ptr[:, 0:T], func=AF.Copy), [r_ptr], [r_ST])
                for dc in range(DC):
                    pb, rb = next_bank()
                    for c in range(NOWN):
                        M(lambda e, pb=pb, dc=dc, c=c: e.matmul(pb[:, 0:CAP], u2tm[:, c, dc * 128:(dc + 1) * 128], Ssel[:, c, :],
                                                                start=(c == 0), stop=(c == NOWN - 1)), [r_u2T, r_S], [rb])
                    if dc % 2 == 0:
                        V(lambda e, pb=pb, dc=dc: e.tensor_copy(out=xT[:, dc, :], in_=pb[:, 0:CAP]), [rb], [r_xT])
                    else:
                        A(lambda e, pb=pb, dc=dc: e.activation(out=xT[:, dc, :], in_=pb[:, 0:CAP], func=AF.Copy), [rb], [r_xT])

            cur = issue(jobs[0])
            for ji, job in enumerate(jobs):
                nxt = issue(jobs[ji + 1]) if ji + 1 < len(jobs) else None
                kind, ex_i, k = job
                wt, wr = cur
                pi = ex_i % 2
                if kind == "dn":
                    cur_bd = bd_q.pop(0)
                if kind == "gu" and k == 0:
                    prologue(ex_i)
                if kind == "gu":
                    for ii in range(2):
                        i = k + ii
                        pg, rg = next_bank()
                        pu, ru = next_bank()
                        for dc in range(DC):
                            M(lambda e, pg=pg, wt=wt, ii=ii, dc=dc: e.matmul(pg[:, 0:CAP], wt[:, dc, ii * 128:(ii + 1) * 128], xT[:, dc, :],
                                                                          start=(dc == 0), stop=(dc == DC - 1)), [wr, r_xT], [rg])
                        for dc in range(DC):
                            M(lambda e, pu=pu, wt=wt, ii=ii, dc=dc: e.matmul(pu[:, 0:CAP], wt[:, dc, 256 + ii * 128:256 + (ii + 1) * 128], xT[:, dc, :],
                                                                          start=(dc == 0), stop=(dc == DC - 1)), [wr, r_xT], [ru])
                        V(lambda e, pg=pg, i=i, pi=pi: e.tensor_scalar(out=gg[:], in0=pg[:, 0:CAP], scalar1=bgu[pi][:, i:i + 1], scalar2=7.0,
                                                                      op0=ALU.add, op1=ALU.min), [rg, rbgu[pi]], [r_gg])
                        A(lambda e: e.activation(out=sg[:], in_=gg[:], func=AF.Sigmoid, scale=1.702), [r_gg], [r_sg])
                        V(lambda e, pu=pu, i=i, pi=pi: e.tensor_scalar(out=uu[:], in0=pu[:, 0:CAP], scalar1=bgu[pi][:, FC + i:FC + i + 1], scalar2=7.0,
                                                                      op0=ALU.add, op1=ALU.min), [ru, rbgu[pi]], [r_uu])
                        V(lambda e: e.tensor_scalar(out=uu[:], in0=uu[:], scalar1=-7.0, scalar2=1.0, op0=ALU.max, op1=ALU.add),
                          [r_uu], [r_uu])
                        V(lambda e: e.tensor_tensor(out=gg[:], in0=gg[:], in1=sg[:], op=ALU.mult), [r_gg, r_sg], [r_gg])
                        V(lambda e, i=i: e.tensor_tensor(out=hidT[:, i, :], in0=gg[:], in1=uu[:], op=ALU.mult), [r_gg, r_uu], [r_hid])
                else:
                    db = k
                    bd, rbd = cur_bd
                    for sbk in range(CAP // 128):
                        pb, rb = next_bank()
                        for i in range(FC):
                            M(lambda e, pb=pb, wt=wt, i=i, sbk=sbk: e.matmul(pb[:, :], hidT[:, i, sbk * 128:(sbk + 1) * 128], wt[:, i, :], start=(i == 0), stop=False),
                              [wr, r_hid], [rb])
                        M(lambda e, pb=pb, bd=bd: e.matmul(pb[:, :], ones_b[0:1, :], bd[0:1, :], start=False, stop=True),
                          [r_onesb, rbd], [rb])
                        V(lambda e, pb=pb, sbk=sbk, db=db: e.tensor_tensor(out=Ys[:, sbk, db * 512:(db + 1) * 512], in0=pb[:], in1=gt2_bc[:, db * 512:(db + 1) * 512],
                                                                          op=ALU.mult), [rb, r_gt2], [r_Ys])
                    for c in range(NOWN):
                        pb, rb = next_bank()
                        for sbk in range(CAP // 128):
                            M(lambda e, pb=pb, sbk=sbk, c=c, db=db: e.matmul(pb[:, :], STs[:, sbk, c * 128:(c + 1) * 128], Ys[:, sbk, db * 512:(db + 1) * 512],
                                                                            start=(sbk == 0), stop=(sbk == CAP // 128 - 1)), [r_ST, r_Ys], [rb])
                        V(lambda e, pb=pb, c=c, ex_i=ex_i, db=db: e.scalar_tensor_tensor(out=acc[:, c, db * 512:(db + 1) * 512], in0=pb[:], scalar=Gd[:, c, ex_i:ex_i + 1],
                                                                                       in1=acc[:, c, db * 512:(db + 1) * 512], op0=ALU.mult, op1=ALU.add),
                          [rb, r_Gd, r_acc[c]], [r_acc[c]])
                _compact(CONSTS + [r_u2T, r_Gd, r_gt2, r_posm, r_iota])
                cur = nxt

        r_out = Res("out")
        for c in range(NOWN):
            LD(out_d[c * 128:(c + 1) * 128, :], acc[:, c, :], [r_acc[c]], [r_out])
        P.finish()
    return nc


def _consts(cfg, h):
    NT = cfg.NCTX + cfg.NOTH + cfg.NOWN
    TA = NT * 128
    ident = np.eye(128, dtype=np.float32)
    jj, tt = np.meshgrid(np.arange(128), np.arange(128), indexing="ij")
    triU = (jj <= tt).astype(np.float32)
    triL = (jj >= tt).astype(np.float32)
    rot = np.zeros((128, 128), np.float32)
    for i in range(64):
        rot[2 * i + 1, 2 * i] = -1.0
        rot[2 * i, 2 * i + 1] = 1.0
    nctx = cfg.NCTX * 128
    seq = cfg.SEQ
    pos = np.arange(seq)
    if h == 0:
        pos = pos[::-1]
    rows = (pos // cfg.GRID_W).astype(np.float32)
    cols = (pos % cfg.GRID_W).astype(np.float32)
    freqs = np.exp(-math.log(10000.0) * np.arange(32, dtype=np.float32) / 32).astype(np.float32)
    ang = np.concatenate([rows[:, None] * freqs, cols[:, None] * freqs], axis=-1).astype(np.float32)
    cos = np.repeat(np.cos(ang), 2, axis=1).T
    sin = np.repeat(np.sin(ang), 2, axis=1).T
    cosT = np.concatenate([np.ones((128, nctx), np.float32), cos.astype(np.float32)], axis=1)
    sinT = np.concatenate([np.zeros((128, nctx), np.float32), sin.astype(np.float32)], axis=1)
    assert cosT.shape[1] == TA
    triSU = (jj < tt).astype(np.float32)
    iota = np.ascontiguousarray(np.broadcast_to(np.arange(512, dtype=np.float32)[None, :], (128, 512)))
    return dict(c_ident=ident, c_triU=triU, c_triL=triL, c_rot=rot, c_triSU=triSU, c_iota=iota,
                c_cos=np.ascontiguousarray(cosT), c_sin=np.ascontiguousarray(sinT))


def make_in_maps(cfg, inp):
    f = lambda a: np.ascontiguousarray(np.asarray(a, dtype=np.float32))
    x, c, ctx, c_ctx = f(inp["x"]), f(inp["c"]), f(inp["ctx"]), f(inp["c_ctx"])
    B = x.shape[0]
    shared = dict(
        w_mod=f(inp["w_mod"][0]), b_mod=f(inp["b_mod"][0])[None, :], g1=f(inp["g_norm1"][0])[None, :],
        g2=f(inp["g_norm2"][0])[None, :], w_in=f(inp["w_in"][0]), b_in=f(inp["b_in"][0])[None, :],
        g_q=f(inp["g_q"][0])[:, None], g_k=f(inp["g_k"][0])[:, None], g_mh=f(inp["g_mh"][0])[None, :],
        w_br_m=f(inp["w_br_m"][0]), w_br_a=f(inp["w_br_a"][0]), w_out=f(inp["w_out"][0]),
        w_router=f(inp["w_router"][0]), b_router=f(inp["b_router"][0])[None, :],
        w_gu=f(inp["w_gu"][0]), b_gu=f(inp["b_gu"][0]), w_dn=f(inp["w_dn"][0]), b_dn=f(inp["b_dn"][0]),
    )
    wgt = shared["w_in"][:, OFF_GT:OFF_GT + 16]
    bgt = shared["b_in"][0, OFF_GT:OFF_GT + 16]
    perm = {1: list(range(0, 4)) + list(range(8, 12)) + list(range(4, 8)) + list(range(12, 16)),
            0: list(range(8, 12)) + list(range(0, 4)) + list(range(12, 16)) + list(range(4, 8))}
    half = cfg.SEQ // 2
    maps = []
    for core in range(2 * B):
        b, h = core // 2, core % 2
        if h == 1:
            xfm = np.concatenate([ctx[b], x[b]], axis=0)
        else:
            xfm = np.concatenate([ctx[b, ::-1], x[b, ::-1]], axis=0)
        m = dict(shared)
        m["xf"] = np.ascontiguousarray(xfm)
        m["cc"] = np.ascontiguousarray(np.stack([c[b], c_ctx], axis=0))
        m["wg16"] = np.ascontiguousarray(wgt[:, perm[h]])
        m["bg16"] = np.ascontiguousarray(bgt[perm[h]])[None, :]
        m.update(_consts(cfg, h))
        maps.append(m)
    return maps


def assemble(cfg, results, B):
    half = cfg.SEQ // 2
    out = np.zeros((B, cfg.SEQ, cfg.D), np.float32)
    for core in range(2 * B):
        b, h = core // 2, core % 2
        o = np.asarray(results[core]["out"], dtype=np.float32)
        if h == 1:
            out[b, half:] = o
        else:
            out[b, :half] = o[::-1]
    return out


def kernel(**inputs):
    cfg = Cfg()
    nc = build(cfg)
    maps = make_in_maps(cfg, inputs)
    res = run_bass_kernel_spmd(nc, maps, core_ids=list(range(8)))
    return assemble(cfg, res.results, 4)
```

```python
import contextlib
import math
import numpy as np
import concourse.bass as bass
import concourse.mybir as mybir
from concourse.bass_utils import run_bass_kernel_spmd

F32 = mybir.dt.float32
BF16 = mybir.dt.bfloat16
ALU = mybir.AluOpType
AF = mybir.ActivationFunctionType

COMPUTE = ("pe", "dve", "act", "pool")
NDMA = {"sp": 8, "pool": 4}


class Res:
    __slots__ = ("name", "w", "rd")

    def __init__(self, name=""):
        self.name = name
        self.w = None
        self.rd = []


class Prog:
    def __init__(self, nc, es):
        self.nc = nc
        self.es = es
        self.ops = {e: [] for e in ("pe", "dve", "act", "pool", "sp")}
        self.cnt = {e: 0 for e in COMPUTE}
        self.sem = {e: es.enter_context(nc.semaphore("s_" + e)) for e in COMPUTE}
        self.known = {e: {} for e in ("pe", "dve", "act", "pool", "sp")}
        self.snaps = {e: [dict()] for e in COMPUTE}
        self.dsem = {}
        self.duse = {}
        self.drot = {}
        for q, n in NDMA.items():
            for i in range(n):
                self.dsem[(q, i)] = es.enter_context(nc.semaphore(f"d_{q}{i}"))
                self.duse[(q, i)] = 0
            self.drot[q] = 0

    def semh(self, key):
        return self.sem[key] if key in self.sem else self.dsem[key]

    def _need(self, eng, ev, waits):
        if ev is None:
            return
        key, val = ev
        if self.known[eng].get(key, 0) >= val:
            return
        if waits.get(key, 0) < val:
            waits[key] = val

    def _learn(self, eng, key, val):
        kn = self.known[eng]
        if kn.get(key, 0) < val:
            kn[key] = val
        if key in self.snaps:
            for k2, v2 in self.snaps[key][val].items():
                if kn.get(k2, 0) < v2:
                    kn[k2] = v2

    def _deps(self, eng, reads, writes):
        waits = {}
        for r in reads:
            self._need(eng, r.w, waits)
        for r in writes:
            self._need(eng, r.w, waits)
            for ev in r.rd:
                self._need(eng, ev, waits)
        return waits

    def op(self, eng, fn, reads=(), writes=()):
        waits = self._deps(eng, reads, writes)
        if eng in waits:
            own_raw = 0
            for r in reads:
                if r.w is not None and r.w[0] == eng:
                    own_raw = max(own_raw, r.w[1])
            if eng != "pe" and own_raw > self.known[eng].get(eng, 0):
                waits[eng] = own_raw
            else:
                del waits[eng]
        for k, v in waits.items():
            self._learn(eng, k, v)
        self.cnt[eng] += 1
        n = self.cnt[eng]
        self.ops[eng].append((list(waits.items()), fn, (eng, 1)))
        self.snaps[eng].append(dict(self.known[eng]))
        ev = (eng, n)
        for r in reads:
            if len(r.rd) > 48:
                _compact([r])
            r.rd.append(ev)
        for r in writes:
            r.w = ev
            r.rd = []
        return ev

    def dma(self, q, fn, reads=(), writes=()):
        waits = self._deps(q, reads, writes)
        i = self.drot[q]
        self.drot[q] = (i + 1) % NDMA[q]
        key = (q, i)
        prev = self.duse[key]
        if prev > 0 and self.known[q].get(key, 0) < prev:
            if waits.get(key, 0) < prev:
                waits[key] = prev
        for k, v in waits.items():
            self._learn(q, k, v)
        self.duse[key] = prev + 16
        self.ops[q].append((list(waits.items()), fn, (key, 16)))
        ev = (key, prev + 16)
        for r in reads:
            r.rd.append(ev)
        for r in writes:
            r.w = ev
            r.rd = []
        return ev

    def barrier(self):
        targets = {e: self.cnt[e] for e in COMPUTE if self.cnt[e] > 0}
        targets.update({k: v for k, v in self.duse.items() if v > 0})
        for e in ("pe", "dve", "act", "pool", "sp"):
            waits = {k: v for k, v in targets.items() if k != e and self.known[e].get(k, 0) < v}
            for k, v in waits.items():
                self._learn(e, k, v)
            if waits:
                self.ops[e].append((list(waits.items()), None, None))

    def finish(self):
        nc = self.nc
        waits = {}
        for key, v in self.duse.items():
            if v > 0:
                waits[key] = v
        for e in COMPUTE:
            if self.cnt[e] > 0:
                waits[e] = self.cnt[e]
        self.ops["sp"].append((list(waits.items()), None, None))
        hmap = {"pe": "tensor", "dve": "vector", "act": "scalar", "pool": "gpsimd", "sp": "sync"}
        with nc.Block() as block:
            for e, attr in hmap.items():
                ops = self.ops[e]
                if not ops:
                    continue

                def section(engh, ops=ops):
                    for waits, fn, inc in ops:
                        for k, v in waits:
                            engh.wait_ge(self.semh(k), v)
                        if fn is None:
                            continue
                        inst = fn(engh)
                        if inc is not None:
                            inst.then_inc(self.semh(inc[0]), inc[1])

                getattr(block, attr)(section)


def _compact(res_list):
    for r in res_list:
        d = {}
        for k, v in r.rd:
            if d.get(k, 0) < v:
                d[k] = v
        r.rd = list(d.items())


class Cfg:
    D = 2048
    NCTX = 2
    NOTH = 8
    NOWN = 8
    NE = 32
    DFF = 2048
    SEQ = 2048
    GRID_W = 64
    TOPK = 4
    DEBUG = False


HD = 128
MH = 4
MQK = 256
MV = 512
EPS = 1e-6
F_IN = 13328
OFF_MQ, OFF_MK, OFF_MV, OFF_OG, OFF_GT, OFF_AQ, OFF_AK, OFF_AV, OFF_MG = (
    0, 1024, 2048, 4096, 6144, 6160, 8208, 8720, 9232)


def build(cfg):
    D = cfg.D
    DC = D // 128
    NT = cfg.NCTX + cfg.NOTH + cfg.NOWN
    NOWN = cfg.NOWN
    C0 = cfg.NCTX + cfg.NOTH
    T = NOWN * 128
    TA = NT * 128
    TB = min(512, T)
    NB = T // TB
    NE = cfg.NE
    DFF = cfg.DFF
    FC = DFF // 128

    nc = bass.Bass("TRN2", target_bir_lowering=False)

    def din(name, shape):
        return nc.dram_tensor(name, list(shape), F32, kind="ExternalInput").ap()

    xf = din("xf", [TA, D])
    cc = din("cc", [2, D])
    w_mod = din("w_mod", [D, 6 * D])
    b_mod = din("b_mod", [1, 6 * D])
    g1 = din("g1", [1, D])
    g2 = din("g2", [1, D])
    w_in = din("w_in", [D, F_IN])
    b_in = din("b_in", [1, F_IN])
    wg16 = din("wg16", [D, 16])
    bg16 = din("bg16", [1, 16])
    g_q = din("g_q", [128, 1])
    g_k = din("g_k", [128, 1])
    g_mh = din("g_mh", [1, MH * MV])
    w_br_m = din("w_br_m", [D, D])
    w_br_a = din("w_br_a", [D, D])
    w_out = din("w_out", [D, D])
    w_router = din("w_router", [D, NE])
    b_router = din("b_router", [1, NE])
    w_gu = din("w_gu", [NE, D, 2 * DFF])
    b_gu = din("b_gu", [NE, 2 * DFF])
    w_dn = din("w_dn", [NE, DFF, D])
    b_dn = din("b_dn", [NE, D])
    c_ident = din("c_ident", [128, 128])
    c_triU = din("c_triU", [128, 128])
    c_triL = din("c_triL", [128, 128])
    c_rot = din("c_rot", [128, 128])
    c_triSU = din("c_triSU", [128, 128])
    c_iota = din("c_iota", [128, 512])
    c_cos = din("c_cos", [128, TA])
    c_sin = din("c_sin", [128, TA])
    out_d = nc.dram_tensor("out", [T, D], F32, kind="ExternalOutput").ap()

    def scratch(name, shape, dt=BF16):
        if cfg.DEBUG:
            return nc.dram_tensor(name, list(shape), dt, kind="ExternalOutput").ap()
        return nc.dram_tensor(name, list(shape), dt).ap()

    def dbg_out(name, shape, dt):
        return nc.dram_tensor(name, list(shape), dt, kind="ExternalOutput").ap()

    mod_d = scratch("mod_d", [2, 6 * D], F32)
    MQT = scratch("MQT", [8, 128, T])
    MKT = scratch("MKT", [8, 128, T])
    AQT = scratch("AQT", [16, 128, T])
    GMT = scratch("GMT", [32, 128, T])
    MKd = scratch("MKd", [NT, 128, MH * MQK])
    MVd = scratch("MVd", [NT, 128, MH * MV])
    OGd = scratch("OGd", [NOWN, 128, MH * MV])
    AVd = scratch("AVd", [NT, 128, 4 * HD])

    with contextlib.ExitStack() as es:
        P = Prog(nc, es)

        KB = 1024
        ARENA = 171 * KB
        arena = es.enter_context(nc.sbuf_tensor("arena", [128, ARENA // 4], F32))

        class Bump:
            def __init__(self, off, limit):
                self.off = off
                self.limit = limit

        def sb(stack, name, shape, dt):
            if isinstance(stack, Bump):
                esz = 4 if dt == F32 else 2
                n = int(np.prod(shape[1:]))
                nbytes = (n * esz + 31) // 32 * 32
                assert stack.off + nbytes <= stack.limit, (name, stack.off, nbytes, stack.limit)
                a = arena[0:shape[0], stack.off // 4:(stack.off + nbytes) // 4]
                stack.off += nbytes
                v = a if dt == F32 else a.bitcast(BF16)
                v = v[:, 0:n]
                if len(shape) == 3:
                    v = v.rearrange("p (a b) -> p a b", a=shape[1])
                return v
            return stack.enter_context(nc.sbuf_tensor(name, list(shape), dt))

        def at(off, shape, dt):
            return sb(Bump(off, ARENA), "x", shape, dt)

        def V(fn, r=(), w=()):
            return P.op("dve", fn, r, w)

        def A(fn, r=(), w=()):
            return P.op("act", fn, r, w)

        def G(fn, r=(), w=()):
            return P.op("pool", fn, r, w)

        def M(fn, r=(), w=()):
            return P.op("pe", fn, r, w)

        def LD(out, in_, r=(), w=(), slow=False):
            if slow:
                return P.dma("sp", lambda e: e.dma_start(out=out, in_=in_, allow_slow_non_contiguous=True), r, w)
            return P.dma("sp", lambda e: e.dma_start(out=out, in_=in_), r, w)

        def LDC(out, in_, r=(), w=(), slow=False):
            if slow:
                return P.dma("pool", lambda e: e.dma_start(out=out, in_=in_, allow_slow_non_contiguous=True), r, w)
            return P.dma("pool", lambda e: e.dma_start(out=out, in_=in_), r, w)

        pbank = [es.enter_context(nc.psum_tensor(f"pb{i}", [128, 512], F32)) for i in range(7)]
        rbank = [Res(f"pb{i}") for i in range(7)]
        ptr = es.enter_context(nc.psum_tensor("ptr", [128, 1024], BF16))
        r_ptr = Res("ptr")

        ident_f = sb(es, "ident_f", [128, 128], F32); r_identf = Res()
        ident_b = sb(es, "ident_b", [128, 128], BF16); r_identb = Res()
        triU = sb(es, "triU", [128, 128], F32); r_triU = Res()
        triL = sb(es, "triL", [128, 128], F32); r_triL = Res()
        ones_f = sb(es, "ones_f", [128, 128], F32); r_onesf = Res()
        ones_b = sb(es, "ones_b", [128, 128], BF16); r_onesb = Res()
        rot_b = sb(es, "rot_b", [128, 128], BF16); r_rot = Res()
        epsb = sb(es, "epsb", [128, 1], F32); r_eps = Res()
        CONSTS = [r_identf, r_identb, r_triU, r_triL, r_onesf, r_onesb, r_rot, r_eps]
        LD(ident_f[:], c_ident, w=[r_identf])
        LDC(ident_b[:], c_ident, w=[r_identb])
        LD(triU[:], c_triU, w=[r_triU])
        LD(triL[:], c_triL, w=[r_triL])
        LDC(rot_b[:], c_rot, w=[r_rot])
        G(lambda e: e.memset(ones_f[:], 1.0), w=[r_onesf])
        G(lambda e: e.memset(ones_b[:], 1.0), w=[r_onesb])
        G(lambda e: e.memset(epsb[:], EPS), w=[r_eps])

        r_gt1, r_gt2, r_A2, r_sh2 = Res(), Res(), Res(), Res()

        NR = 2
        wring = [sb(es, f"wring{i}", [128, DC, 512], BF16) for i in range(NR)]
        rring = [Res(f"wring{i}") for i in range(NR)]
        ring_i = [0]

        def load_w(src2d, ncols):
            i = ring_i[0]
            ring_i[0] = (i + 1) % NR
            t, r = wring[i], rring[i]
            half = DC // 2
            v = src2d.rearrange("(dc p) n -> p dc n", p=128)
            LDC(t[:, 0:half, 0:ncols], v[:, 0:half, :], w=[r])
            P.dma("pool", lambda e: e.dma_start(out=t[:, half:DC, 0:ncols], in_=v[:, half:DC, :]), [r], [r])
            return t, r

        class WStream:
            def __init__(self, specs):
                self.specs = specs
                self.k = 0
                self.nxt = None

            def get(self):
                cur = self.nxt if self.nxt is not None else load_w(*self.specs[self.k])
                self.k += 1
                self.nxt = load_w(*self.specs[self.k]) if self.k < len(self.specs) else None
                return cur

        bank_i = [0]

        def next_bank():
            i = bank_i[0]
            bank_i[0] = (i + 1) % 7
            return pbank[i], rbank[i]

        NS = 4
        stg = []
        rstg = [Res(f"stg{i}") for i in range(NS)]
        stg_i = [0]

        def next_stg():
            i = stg_i[0]
            stg_i[0] = (i + 1) % NS
            return stg[i], rstg[i]

        r_mod = Res("mod_d")
        r_MQT, r_MKT, r_AQT, r_GMT = Res(), Res(), Res(), Res()
        r_MKd, r_MVd, r_OGd, r_AVd = Res(), Res(), Res(), Res()

        with contextlib.ExitStack() as s0:
            s0 = Bump(90 * KB, ARENA)
            cT_f = sb(s0, "cT_f", [128, 2, DC], F32); r_cTf = Res()
            cT_b = sb(s0, "cT_b", [128, DC, 2], BF16); r_cTb = Res()
            bm2 = sb(s0, "bm2", [2, 512], F32); r_bm2 = Res()
            mrow = sb(s0, "mrow", [2, 512], F32); r_mrow = Res()
            for r_ in range(2):
                P.dma("sp", lambda e, r_=r_: e.dma_start(out=cT_f[:, r_, :], in_=cc[r_, :].rearrange("(dc p) -> p dc", p=128),
                                                         allow_slow_non_contiguous=True), [r_cTf], [r_cTf])
            for r_ in range(2):
                A(lambda e, r_=r_: e.activation(out=cT_b[:, :, r_], in_=cT_f[:, r_, :], func=AF.Silu), [r_cTf, r_cTb], [r_cTb])
            ws0 = WStream([(w_mod[:, blk * 512:(blk + 1) * 512], 512) for blk in range(6 * D // 512)])
            for blk in range(6 * D // 512):
                wt, wr = ws0.get()
                pb, rb = next_bank()
                for dc in range(DC):
                    M(lambda e, dc=dc, pb=pb, wt=wt: e.matmul(pb[0:2, :], cT_b[:, dc, :], wt[:, dc, :],
                                                                start=(dc == 0), stop=(dc == DC - 1)),
                      [r_cTb, wr], [rb])
                LD(bm2[0:1, :], b_mod[0:1, blk * 512:(blk + 1) * 512], w=[r_bm2])
                P.dma("sp", lambda e, blk=blk: e.dma_start(out=bm2[1:2, :], in_=b_mod[0:1, blk * 512:(blk + 1) * 512]),
                      [r_bm2], [r_bm2])
                V(lambda e, pb=pb: e.tensor_tensor(out=mrow[:], in0=pb[0:2, :], in1=bm2[:], op=ALU.add),
                  [rb, r_bm2], [r_mrow])
                LD(mod_d[:, blk * 512:(blk + 1) * 512], mrow[:], [r_mrow], [r_mod])

        def bc_row(ap_row, n):
            return ap_row.broadcast_to([128, n])

        P.barrier()

        uT = at(0, [128, DC, TA], BF16)
        r_uT = [Res(f"uT{c}") for c in range(NT)]
        with contextlib.ExitStack() as s1:
            s1 = Bump(72 * KB, ARENA)
            A1x = sb(s1, "A1x", [128, D], F32); r_A1x = Res()
            S1x = sb(s1, "S1x", [128, D], F32); r_S1x = Res()
            A1c = sb(s1, "A1c", [128, D], F32); r_A1c = Res()
            S1c = sb(s1, "S1c", [128, D], F32); r_S1c = Res()
            g1b = sb(s1, "g1b", [128, D], F32); r_g1b = Res()
            LD(g1b[:], bc_row(g1[0:1, :], D), w=[r_g1b])
            LD(A1x[:], bc_row(mod_d[0:1, D:2 * D], D), [r_mod], [r_A1x])
            LD(A1c[:], bc_row(mod_d[1:2, D:2 * D], D), [r_mod], [r_A1c])
            LD(S1x[:], bc_row(mod_d[0:1, 0:D], D), [r_mod], [r_S1x])
            LD(S1c[:], bc_row(mod_d[1:2, 0:D], D), [r_mod], [r_S1c])
            V(lambda e: e.scalar_tensor_tensor(out=A1x[:], in0=A1x[:], scalar=1.0, in1=g1b[:], op0=ALU.add, op1=ALU.mult),
              [r_A1x, r_g1b], [r_A1x])
            V(lambda e: e.scalar_tensor_tensor(out=A1c[:], in0=A1c[:], scalar=1.0, in1=g1b[:], op0=ALU.add, op1=ALU.mult),
              [r_A1c, r_g1b], [r_A1c])
            xt = [sb(s1, f"xt{i}", [128, D], F32) for i in range(2)]
            rxt = [Res(), Res()]
            xn = [sb(s1, f"xn{i}", [128, D], BF16) for i in range(2)]
            rxn = [Res(), Res()]
            ss = sb(s1, "ss", [128, 2], F32); r_ss = [Res(), Res()]
            for c in range(NT):
                i = c % 2
                Aw, rA, Sw, rS = (A1c, r_A1c, S1c, r_S1c) if c < cfg.NCTX else (A1x, r_A1x, S1x, r_S1x)
                LD(xt[i][:], xf[c * 128:(c + 1) * 128, :], w=[rxt[i]])
                G(lambda e, i=i: e.memset(ss[:, i:i + 1], 0.0), w=[r_ss[i]])
                A(lambda e, i=i: e.activation(out=xn[i][:], in_=xt[i][:], func=AF.Square, accum_out=ss[:, i:i + 1]),
                  [rxt[i], r_ss[i]], [rxn[i], r_ss[i]])
                A(lambda e, i=i: e.activation(out=ss[:, i:i + 1], in_=ss[:, i:i + 1], func=AF.Sqrt, bias=epsb[:], scale=1.0 / D),
                  [r_ss[i], r_eps], [r_ss[i]])
                V(lambda e, i=i: e.reciprocal(out=ss[:, i:i + 1], in_=ss[:, i:i + 1]), [r_ss[i]], [r_ss[i]])
                V(lambda e, i=i, Aw=Aw: e.scalar_tensor_tensor(out=xt[i][:], in0=xt[i][:], scalar=ss[:, i:i + 1], in1=Aw[:],
                                                              op0=ALU.mult, op1=ALU.mult),
                  [rxt[i], r_ss[i], rA], [rxt[i]])
                G(lambda e, i=i, Sw=Sw: e.tensor_tensor(out=xn[i][:], in0=xt[i][:], in1=Sw[:], op=ALU.add),
                  [rxt[i], rS], [rxn[i]])
                for half in range(DC // 8):
                    for j in range(8):
                        dc = half * 8 + j
                        M(lambda e, i=i, dc=dc, j=j: e.transpose(ptr[:, j * 128:(j + 1) * 128], xn[i][:, dc * 128:(dc + 1) * 128], ident_b[:]),
                          [rxn[i], r_identb], [r_ptr])
                    if half % 2 == 0:
                        V(lambda e, c=c, half=half: e.tensor_copy(
                            out=uT[:, half * 8:(half + 1) * 8, c * 128:(c + 1) * 128],
                            in_=ptr[:].rearrange("p (j t) -> p j t", j=8)), [r_ptr], [r_uT[c]])
                    else:
                        A(lambda e, c=c, half=half: e.activation(
                            out=uT[:, half * 8:(half + 1) * 8, c * 128:(c + 1) * 128],
                            in_=ptr[:].rearrange("p (j t) -> p j t", j=8), func=AF.Copy), [r_ptr], [r_uT[c]])
                _compact(CONSTS)

        own_blocks = [(C0 * 128 + b * TB, TB) for b in range(NB)]
        all_blocks = []
        o = 0
        while o < TA:
            n = min(512, TA - o)
            all_blocks.append((o, n))
            o += n

        def uT_res(t0, n):
            return r_uT[t0 // 128:(t0 + n + 127) // 128]

        if cfg.DEBUG:
            P.barrier()
            LD(dbg_out("dbg_uT", [128, DC, TA], BF16), uT[:], r_uT, [Res()])
        P.barrier()
        gates = at(160 * KB, [128, NT, 16], F32); r_gates = Res()
        AKT = at(72 * KB, [128, 4, TA], BF16); r_AKT = Res()

        with contextlib.ExitStack() as s2:
            s2 = Bump(90 * KB, 160 * KB)
            stg.extend(sb(s2, f"stg{i}", [128, 512], BF16) for i in range(NS))
            bT = sb(s2, "bT", [128, 72], F32); r_bT = Res()
            for (off, n, col) in ((OFF_MQ, 8, 0), (OFF_MK, 8, 8), (OFF_AQ, 16, 16), (OFF_AK, 4, 32), (OFF_MG, 32, 36)):
                P.dma("sp", lambda e, off=off, n=n, col=col: e.dma_start(
                    out=bT[:, col:col + n], in_=b_in[0, off:off + n * 128].rearrange("(j p) -> p j", p=128),
                    allow_slow_non_contiguous=True), [r_bT], [r_bT])
            brows = [sb(s2, f"brow{i}", [1, 512], BF16) for i in range(2)]
            rbrows = [Res(), Res()]
            brow_i = [0]
            bg_bc = sb(s2, "bg_bc", [128, 16], F32); r_bg = Res()
            LD(bg_bc[:], bc_row(bg16[0:1, :], 16), w=[r_bg])
            wg_b = sb(s2, "wg_b", [128, DC, 16], BF16); r_wg = Res()
            LDC(wg_b[:], wg16.rearrange("(dc p) n -> p dc n", p=128), w=[r_wg])
            gq_s = sb(s2, "gq_s", [128, 1], F32); r_gq = Res()
            gk_s = sb(s2, "gk_s", [128, 1], F32); r_gk = Res()
            LD(gq_s[:], g_q, w=[r_gq])
            LD(gk_s[:], g_k, w=[r_gk])
            cos_s = sb(s2, "cos_s", [128, TA], BF16); r_cos = Res()
            sin_s = sb(s2, "sin_s", [128, TA], BF16); r_sin = Res()
            LDC(cos_s[:], c_cos, w=[r_cos])
            LDC(sin_s[:], c_sin, w=[r_sin])
            sq = sb(s2, "sq", [128, 512], BF16); r_sq = Res()
            rstd = sb(s2, "rstd", [128, 512], F32); r_rstd = Res()
            xnb = sb(s2, "xnb", [128, 512], BF16); r_xnb = Res()
            t1 = sb(s2, "t1", [128, 512], F32); r_t1 = Res()
            t2 = sb(s2, "t2", [128, 512], F32); r_t2 = Res()

            specs2 = []
            for (off_, nch_) in ((OFF_MQ, 8), (OFF_MK, 8), (OFF_AQ, 16), (OFF_AK, 4), (OFF_MG, 32)):
                for s_ in range(0, nch_, 4):
                    nc_ = min(4, nch_ - s_) * 128
                    specs2.append((w_in[:, off_ + s_ * 128: off_ + s_ * 128 + nc_], nc_))
            for (off_, ncl_) in ((OFF_MK, MH * MQK), (OFF_MV, MH * MV), (OFF_OG, MH * MV), (OFF_AV, 4 * HD)):
                for s_ in range(0, ncl_, 512):
                    specs2.append((w_in[:, off_ + s_: off_ + s_ + 512], 512))
            ws2 = WStream(specs2)

            def fm_group(off, nchunks, blocks, evac):
                for s in range(0, nchunks, 4):
                    ncol = min(4, nchunks - s) * 128
                    wt, wr = ws2.get()
                    for jj in range(ncol // 128):
                        j = s + jj
                        for bi, (t0, n) in enumerate(blocks):
                            pb, rb = next_bank()
                            for dc in range(DC):
                                M(lambda e, dc=dc, pb=pb, wt=wt, jj=jj, t0=t0, n=n: e.matmul(
                                    pb[:, 0:n], wt[:, dc, jj * 128:(jj + 1) * 128], uT[:, dc, t0:t0 + n],
                                    start=(dc == 0), stop=(dc == DC - 1)),
                                  [wr] + uT_res(t0, n), [rb])
                            evac(j, bi, t0, n, pb, rb)
                    _compact(CONSTS + r_uT)

            def simple_evac(dst, rdst, bcol, scale):
                def f(j, bi, t0, n, pb, rb):
                    st, rs = next_stg()
                    V(lambda e: e.tensor_scalar(out=st[:, 0:n], in0=pb[:, 0:n], scalar1=bT[:, bcol + j:bcol + j + 1],
                                                scalar2=scale, op0=ALU.add, op1=ALU.mult), [rb, r_bT], [rs])
                    tl = t0 - C0 * 128
                    LD(dst[j, :, tl:tl + n], st[:, 0:n], [rs], [rdst])
                return f

            def sig_evac(dst, rdst, bcol):
                def f(j, bi, t0, n, pb, rb):
                    st, rs = next_stg()
                    A(lambda e: e.activation(out=st[:, 0:n], in_=pb[:, 0:n], func=AF.Sigmoid,
                                             bias=bT[:, bcol + j:bcol + j + 1], scale=1.0), [rb, r_bT], [rs])
                    tl = t0 - C0 * 128
                    LD(dst[j, :, tl:tl + n], st[:, 0:n], [rs], [rdst])
                return f

            def qknorm_evac(is_q):
                bcol = 16 if is_q else 32
                gs, rg = (gq_s, r_gq) if is_q else (gk_s, r_gk)

                def f(j, bi, t0, n, pb, rb):
                    V(lambda e: e.tensor_scalar(out=t1[:, 0:n], in0=pb[:, 0:n], scalar1=bT[:, bcol + j:bcol + j + 1],
                                                scalar2=None, op0=ALU.add), [rb, r_bT], [r_t1])
                    A(lambda e: e.activation(out=sq[:, 0:n], in_=t1[:, 0:n], func=AF.Square), [r_t1], [r_sq])
                    p2, r2 = next_bank()
                    M(lambda e: e.matmul(p2[:, 0:n], ones_b[:], sq[:, 0:n], start=True, stop=True), [r_onesb, r_sq], [r2])
                    A(lambda e: e.activation(out=rstd[:, 0:n], in_=p2[:, 0:n], func=AF.Sqrt, bias=epsb[:], scale=1.0 / HD),
                      [r2, r_eps], [r_rstd])
                    V(lambda e: e.reciprocal(out=rstd[:, 0:n], in_=rstd[:, 0:n]), [r_rstd], [r_rstd])
                    V(lambda e: e.scalar_tensor_tensor(out=xnb[:, 0:n], in0=t1[:, 0:n], scalar=gs[:, 0:1], in1=rstd[:, 0:n],
                                                       op0=ALU.mult, op1=ALU.mult), [r_t1, rg, r_rstd], [r_xnb])
                    p3, r3 = next_bank()
                    M(lambda e: e.matmul(p3[:, 0:n], rot_b[:], xnb[:, 0:n], start=True, stop=True), [r_rot, r_xnb], [r3])
                    V(lambda e: e.tensor_tensor(out=t2[:, 0:n], in0=p3[:, 0:n], in1=sin_s[:, t0:t0 + n], op=ALU.mult),
                      [r3, r_sin], [r_t2])
                    G(lambda e: e.tensor_tensor(out=t1[:, 0:n], in0=xnb[:, 0:n], in1=cos_s[:, t0:t0 + n], op=ALU.mult),
                      [r_xnb, r_cos], [r_t1])
                    if is_q:
                        st, rs = next_stg()
                        G(lambda e: e.tensor_tensor(out=st[:, 0:n], in0=t1[:, 0:n], in1=t2[:, 0:n], op=ALU.add),
                          [r_t1, r_t2], [rs])
                        tl = t0 - C0 * 128
                        LD(AQT[j, :, tl:tl + n], st[:, 0:n], [rs], [r_AQT])
                    else:
                        G(lambda e: e.tensor_tensor(out=AKT[:, j, t0:t0 + n], in0=t1[:, 0:n], in1=t2[:, 0:n], op=ALU.add),
                          [r_t1, r_t2], [r_AKT])
                return f

            fm_group(OFF_MQ, 8, own_blocks, simple_evac(MQT, r_MQT, 0, 1.0 / 16.0))
            fm_group(OFF_MK, 8, own_blocks, simple_evac(MKT, r_MKT, 8, 1.0))
            fm_group(OFF_AQ, 16, own_blocks, qknorm_evac(True))
            fm_group(OFF_AK, 4, all_blocks, qknorm_evac(False))
            fm_group(OFF_MG, 32, own_blocks, sig_evac(GMT, r_GMT, 36))

            def tm_group(off, ncols, chunks, dst, rdst, sig, own_only):
                for s in range(0, ncols, 512):
                    wt, wr = ws2.get()
                    bi_ = brow_i[0]
                    brow_i[0] = 1 - bi_
                    brow, r_brow = brows[bi_], rbrows[bi_]
                    LDC(brow[:], b_in[0:1, off + s: off + s + 512], w=[r_brow])
                    for c in chunks:
                        pb, rb = next_bank()
                        for dc in range(DC):
                            M(lambda e, dc=dc, pb=pb, wt=wt, c=c: e.matmul(
                                pb[:, :], uT[:, dc, c * 128:(c + 1) * 128], wt[:, dc, :], start=(dc == 0), stop=False),
                              [wr, r_uT[c]], [rb])
                        M(lambda e, pb=pb, brow=brow: e.matmul(pb[:, :], ones_b[0:1, :], brow[0:1, :],
                                                               start=False, stop=True), [r_onesb, r_brow], [rb])
                        st, rs = next_stg()
                        if sig:
                            A(lambda e, pb=pb, st=st: e.activation(out=st[:], in_=pb[:], func=AF.Sigmoid), [rb], [rs])
                        else:
                            V(lambda e, pb=pb, st=st: e.tensor_copy(out=st[:], in_=pb[:]), [rb], [rs])
                        cl = c - C0 if own_only else c
                        LD(dst[cl, :, s:s + 512], st[:], [rs], [rdst])
                    _compact(CONSTS + r_uT)

            allc = list(range(NT))
            ownc = list(range(C0, NT))
            tm_group(OFF_MK, MH * MQK, allc, MKd, r_MKd, False, False)
            tm_group(OFF_MV, MH * MV, allc, MVd, r_MVd, False, False)
            tm_group(OFF_OG, MH * MV, ownc, OGd, r_OGd, True, True)
            tm_group(OFF_AV, 4 * HD, allc, AVd, r_AVd, False, False)
            for c in allc:
                pb, rb = next_bank()
                for dc in range(DC):
                    M(lambda e, dc=dc, pb=pb, c=c: e.matmul(pb[:, 0:16], uT[:, dc, c * 128:(c + 1) * 128], wg_b[:, dc, :],
                                                            start=(dc == 0), stop=(dc == DC - 1)), [r_wg, r_uT[c]], [rb])
                V(lambda e, pb=pb, c=c: e.tensor_tensor(out=gates[:, c, :], in0=pb[:, 0:16], in1=bg_bc[:], op=ALU.add),
                  [rb, r_bg], [r_gates])
        _compact(CONSTS + r_uT)

        if cfg.DEBUG:
            P.barrier()
            LD(dbg_out("dbg_gates", [128, NT, 16], F32), gates[:], [r_gates], [Res()])
            LD(dbg_out("dbg_AKT", [128, 4, TA], BF16), AKT[:], [r_AKT], [Res()])
        P.barrier()
        MOT = at(0, [128, 16, T], BF16); r_MOT = Res()
        AOT = at(32 * KB, [128, 16, T], BF16); r_AOT = Res()

        with contextlib.ExitStack() as s3:
            s3 = Bump(90 * KB, 160 * KB)
            s3b = Bump(32 * KB, 72 * KB)
            lf = sb(s3, "lf", [128, NT, 8], F32); r_lf = Res()
            A(lambda e: e.activation(out=lf[:], in_=gates[:, :, 8:16], func=AF.Exp, scale=-1.0), [r_gates], [r_lf])
            A(lambda e: e.activation(out=lf[:], in_=lf[:], func=AF.Ln, bias=1.0, scale=1.0), [r_lf], [r_lf])
            V(lambda e: e.tensor_scalar(out=lf[:], in0=lf[:], scalar1=-1.0, scalar2=None, op0=ALU.mult), [r_lf], [r_lf])
            rr = sb(s3, "rr", [128, NT, 8], F32); r_rr = Res()
            einv = sb(s3, "einv", [128, NT, 8], F32); r_einv = Res()
            etot = sb(s3, "etot", [128, NT, 8], F32); r_etot = Res()
            for c in range(NT):
                pb, rb = next_bank()
                M(lambda e, pb=pb, c=c: e.matmul(pb[:, 0:4], triU[:], lf[:, c, 0:4], start=True, stop=True), [r_triU, r_lf], [rb])
                M(lambda e, pb=pb, c=c: e.matmul(pb[:, 4:8], triL[:], lf[:, c, 4:8], start=True, stop=True), [r_triL, r_lf], [rb])
                M(lambda e, pb=pb, c=c: e.matmul(pb[:, 8:16], ones_f[:], lf[:, c, 0:8], start=True, stop=True), [r_onesf, r_lf], [rb])
                V(lambda e, pb=pb, c=c: e.tensor_tensor(out=rr[:, c, :], in0=gates[:, c, 0:8], in1=pb[:, 0:8], op=ALU.subtract),
                  [rb, r_gates], [r_rr])
                A(lambda e, pb=pb, c=c: e.activation(out=einv[:, c, :], in_=pb[:, 0:8], func=AF.Exp, scale=-1.0), [rb], [r_einv])
                A(lambda e, pb=pb, c=c: e.activation(out=etot[:, c, :], in_=pb[:, 8:16], func=AF.Exp), [rb], [r_etot])
            A(lambda e: e.activation(out=rr[:], in_=rr[:], func=AF.Exp), [r_rr], [r_rr])
            _compact(CONSTS)

            gmh_bc = sb(s3, "gmh_bc", [128, MV], F32); r_gmh = Res()
            qT = sb(s3, "qT", [128, 2, T], BF16); r_qT = Res()
            kT = sb(s3, "kT", [128, 2, T], BF16); r_kT = Res()
            ktm = sb(s3, "ktm", [128, NT, MQK], BF16); r_ktm = Res()
            vtm = sb(s3b, "vtm", [128, NT, MV], BF16); r_vtm = Res()
            ogt = sb(s3, "ogt", [128, NOWN, MV], BF16); r_ogt = Res()
            hA = sb(s3b, "hA", [128, NOWN, MV], F32); r_hA = Res()
            Cst = sb(s3, "Cst", [128, 2, MV + 1], F32); r_Cst = Res()
            Cb = sb(s3, "Cb", [128, 2, MV + 1], BF16); r_Cb = Res()
            kr = sb(s3, "kr", [128, MQK], BF16); r_kr = Res()
            PT = sb(s3, "PT", [128, 128], BF16); r_PT = Res()
            dtmp = sb(s3, "dtmp", [128, 2, MV + 1], F32); r_dtmp = Res()
            den = sb(s3, "den", [128, 2], F32); r_den = Res()
            hs = sb(s3, "hs", [128, MV], F32); r_hs = Res()
            hjunk = sb(s3, "hjunk", [128, MV], BF16); r_hjunk = Res()
            hss = sb(s3, "hss", [128, 1], F32); r_hss = Res()
            mo = sb(s3, "mo", [128, MV], BF16); r_mo = Res()

            for h in range(MH):
                for j in range(2):
                    LD(qT[:, j, :], MQT[2 * h + j], [r_MQT], [r_qT])
                    LD(kT[:, j, :], MKT[2 * h + j], [r_MKT], [r_kT])
                LD(ktm[:], MKd[:, :, h * MQK:(h + 1) * MQK].rearrange("c p n -> p c n"), [r_MKd], [r_ktm])
                LD(vtm[:], MVd[:, :, h * MV:(h + 1) * MV].rearrange("c p n -> p c n"), [r_MVd], [r_vtm])
                LD(ogt[:], OGd[:, :, h * MV:(h + 1) * MV].rearrange("c p n -> p c n"), [r_OGd], [r_ogt])
                LD(gmh_bc[:], bc_row(g_mh[0:1, h * MV:(h + 1) * MV], MV), w=[r_gmh])
                for dirn in range(2):
                    gi = h + 4 * dirn
                    mask, rmask = (triU, r_triU) if dirn == 0 else (triL, r_triL)
                    if dirn == 0:
                        order = list(range(NT))
                    else:
                        order = [1, 0] if cfg.NCTX == 2 else list(range(cfg.NCTX - 1, -1, -1))
                        order = order + list(range(NT - 1, C0 - 1, -1))
                    G(lambda e: e.memset(Cst[:], 0.0), w=[r_Cst])
                    G(lambda e: e.memset(Cb[:], 0.0), w=[r_Cb])
                    for idx, c in enumerate(order):
                        own = c >= C0
                        last = idx == len(order) - 1
                        co = c - C0
                        if own:
                            pS, rS = next_bank()
                            for j in range(2):
                                M(lambda e, j=j, pS=pS, co=co: e.matmul(pS[:, 0:128], kT[:, j, co * 128:(co + 1) * 128],
                                                                         qT[:, j, co * 128:(co + 1) * 128], start=(j == 0), stop=(j == 1)),
                                  [r_kT, r_qT], [rS])
                            V(lambda e, pS=pS, c=c, gi=gi, mask=mask: e.scalar_tensor_tensor(
                                out=PT[:], in0=pS[:, 0:128], scalar=rr[:, c, gi:gi + 1], in1=mask[:], op0=ALU.mult, op1=ALU.mult),
                              [rS, r_rr, rmask], [r_PT])
                            pN, rN = next_bank()
                            pD, rD = next_bank()
                            M(lambda e, pN=pN, c=c: e.matmul(pN[:, :], PT[:], vtm[:, c, :], start=True, stop=False), [r_PT, r_vtm], [rN])
                            for j in range(2):
                                M(lambda e, pN=pN, j=j, co=co: e.matmul(pN[:, :], qT[:, j, co * 128:(co + 1) * 128], Cb[:, j, 0:MV],
                                                                         start=False, stop=(j == 1)), [r_qT, r_Cb], [rN])
                            M(lambda e, pD=pD: e.matmul(pD[:, 0:1], PT[:], ones_b[:, 0:1], start=True, stop=False), [r_PT, r_onesb], [rD])
                            for j in range(2):
                                M(lambda e, pD=pD, j=j, co=co: e.matmul(pD[:, 0:1], qT[:, j, co * 128:(co + 1) * 128], Cb[:, j, MV:MV + 1],
                                                                         start=False, stop=(j == 1)), [r_qT, r_Cb], [rD])
                            A(lambda e, pD=pD: e.activation(out=den[:, 0:1], in_=pD[:, 0:1], func=AF.Abs), [rD], [r_den])
                            V(lambda e, c=c, gi=gi: e.tensor_tensor(out=den[:, 0:1], in0=den[:, 0:1], in1=einv[:, c, gi:gi + 1], op=ALU.max),
                              [r_den, r_einv], [r_den])
                            V(lambda e: e.reciprocal(out=den[:, 1:2], in_=den[:, 0:1]), [r_den], [r_den])
                            if dirn == 0:
                                V(lambda e, pN=pN, co=co: e.tensor_scalar(out=hA[:, co, :], in0=pN[:, :], scalar1=den[:, 1:2], scalar2=None,
                                                                           op0=ALU.mult), [rN, r_den], [r_hA])
                            else:
                                V(lambda e, pN=pN, co=co: e.scalar_tensor_tensor(out=hs[:], in0=pN[:, :], scalar=den[:, 1:2], in1=hA[:, co, :],
                                                                                  op0=ALU.mult, op1=ALU.add), [rN, r_den, r_hA], [r_hs])
                                G(lambda e: e.memset(hss[:], 0.0), w=[r_hss])
                                A(lambda e: e.activation(out=hjunk[:], in_=hs[:], func=AF.Square, accum_out=hss[:]), [r_hs, r_hss], [r_hjunk, r_hss])
                                A(lambda e: e.activation(out=hss[:], in_=hss[:], func=AF.Sqrt, bias=epsb[:], scale=1.0 / MV), [r_hss, r_eps], [r_hss])
                                V(lambda e: e.reciprocal(out=hss[:], in_=hss[:]), [r_hss], [r_hss])
                                V(lambda e, h=h: e.scalar_tensor_tensor(out=hs[:], in0=hs[:], scalar=hss[:, 0:1], in1=gmh_bc[:],
                                                                       op0=ALU.mult, op1=ALU.mult), [r_hs, r_hss, r_gmh], [r_hs])
                                G(lambda e, co=co: e.tensor_tensor(out=mo[:], in0=hs[:], in1=ogt[:, co, :], op=ALU.mult), [r_hs, r_ogt], [r_mo])
                                for j in range(4):
                                    M(lambda e, j=j: e.transpose(ptr[:, j * 128:(j + 1) * 128], mo[:, j * 128:(j + 1) * 128], ident_b[:]),
                                      [r_mo, r_identb], [r_ptr])
                                V(lambda e, h=h, co=co: e.tensor_copy(out=MOT[:, 4 * h:4 * h + 4, co * 128:(co + 1) * 128],
                                                                     in_=ptr[:, 0:512].rearrange("p (j t) -> p j t", j=4)), [r_ptr], [r_MOT])
                        if not last:
                            V(lambda e, c=c, gi=gi: e.tensor_scalar(out=kr[:], in0=ktm[:, c, :], scalar1=rr[:, c, gi:gi + 1], scalar2=None,
                                                                    op0=ALU.mult), [r_ktm, r_rr], [r_kr])
                            for j in range(2):
                                pC, rC = next_bank()
                                pn, rn = next_bank()
                                M(lambda e, pC=pC, j=j, c=c: e.matmul(pC[:, :], kr[:, j * 128:(j + 1) * 128], vtm[:, c, :], start=True, stop=True),
                                  [r_kr, r_vtm], [rC])
                                M(lambda e, pn=pn, j=j: e.matmul(pn[:, 0:1], kr[:, j * 128:(j + 1) * 128], ones_b[:, 0:1], start=True, stop=True),
                                  [r_kr, r_onesb], [rn])
                                V(lambda e, pC=pC, j=j: e.tensor_tensor(out=dtmp[:, j, 0:MV], in0=pC[:, :], in1=Cst[:, j, 0:MV], op=ALU.add),
                                  [rC, r_Cst], [r_dtmp])
                                V(lambda e, pn=pn, j=j: e.tensor_tensor(out=dtmp[:, j, MV:MV + 1], in0=pn[:, 0:1], in1=Cst[:, j, MV:MV + 1], op=ALU.add),
                                  [rn, r_Cst], [r_dtmp])
                            V(lambda e, c=c, gi=gi: e.tensor_scalar(out=Cst[:], in0=dtmp[:], scalar1=etot[:, c, gi:gi + 1], scalar2=None, op0=ALU.mult),
                              [r_dtmp, r_etot], [r_Cst])
                            A(lambda e: e.activation(out=Cb[:], in_=Cst[:], func=AF.Copy), [r_Cst], [r_Cb])
                    _compact(CONSTS + [r_rr, r_einv, r_etot, r_vtm, r_ktm, r_qT, r_kT])

        P.barrier()
        with contextlib.ExitStack() as s4:
            s4 = Bump(90 * KB, ARENA)
            vat = sb(s4, "vat", [128, NT, HD], BF16); r_vat = Res()
            qh = sb(s4, "qh", [128, T], BF16); r_qh = Res()
            PTa = [sb(s4, f"PTa{i}", [128, 512], BF16) for i in range(2)]
            rPTa = [Res(), Res()]
            rsum = sb(s4, "rsum", [128, 512], F32); r_rsum = Res()
            sc = 1.0 / math.sqrt(HD)
            for kh in range(4):
                LD(vat[:], AVd[:, :, kh * HD:(kh + 1) * HD].rearrange("c p n -> p c n"), [r_AVd], [r_vat])
                for g in range(4):
                    head = kh * 4 + g
                    LD(qh[:], AQT[head], [r_AQT], [r_qh])
                    for b in range(NB):
                        oz = 2 * ((head * NB + b) % 2)
                        pO, rO = pbank[oz], rbank[oz]
                        pZ, rZ = pbank[oz + 1], rbank[oz + 1]
                        def qk(c, b=b, kh=kh):
                            si = 4 + (c % 3)
                            pS, rS = pbank[si], rbank[si]
                            M(lambda e, pS=pS, c=c, b=b, kh=kh: e.matmul(pS[:, 0:TB], AKT[:, kh, c * 128:(c + 1) * 128], qh[:, b * TB:(b + 1) * TB],
                                                                        start=True, stop=True), [r_AKT, r_qh], [rS])
                        qk(0)
                        if NT > 1:
                            qk(1)
                        for c in range(NT):
                            if c + 2 < NT:
                                qk(c + 2)
                            si = 4 + (c % 3)
                            pS, rS = pbank[si], rbank[si]
                            i = c % 2
                            A(lambda e, pS=pS, i=i: e.activation(out=PTa[i][:, 0:TB], in_=pS[:, 0:TB], func=AF.Exp, scale=sc), [rS], [rPTa[i]])
                            M(lambda e, pO=pO, c=c, i=i: e.matmul(pO[:, 0:TB], vat[:, c, :], PTa[i][:, 0:TB], start=(c == 0), stop=(c == NT - 1)),
                              [r_vat, rPTa[i]], [rO])
                            M(lambda e, pZ=pZ, c=c, i=i: e.matmul(pZ[:, 0:TB], ones_b[:], PTa[i][:, 0:TB], start=(c == 0), stop=(c == NT - 1)),
                              [r_onesb, rPTa[i]], [rZ])
                        V(lambda e, pZ=pZ: e.reciprocal(out=rsum[:, 0:TB], in_=pZ[:, 0:TB]), [rZ], [r_rsum])
                        V(lambda e, pO=pO, head=head, b=b: e.tensor_tensor(out=AOT[:, head, b * TB:(b + 1) * TB], in0=pO[:, 0:TB], in1=rsum[:, 0:TB],
                                                                          op=ALU.mult), [rO, r_rsum], [r_AOT])
                    _compact(CONSTS + [r_AKT, r_vat])

        P.barrier()

        acc = at(0, [128, NOWN, D], F32); r_acc = [Res(f"acc{c}") for c in range(NOWN)]
        u2tm = at(64 * KB, [128, NOWN, D], BF16); r_u2T = Res()
        Gd = at(96 * KB, [128, NOWN, NE], F32); r_Gd = Res()
        posm = at(97 * KB, [128, NOWN, NE], F32); r_posm = Res()
        gt1_bc = at(64 * KB, [128, D], F32)
        gt2_bc = at(98 * KB, [128, D], F32)

        with contextlib.ExitStack() as s5:
            s5 = Bump(122 * KB, ARENA)
            zT = at(90 * KB, [128, 16, T], BF16); r_zT = Res()
            gmt = sb(s5, "gmt", [128, 2, T], BF16); r_gmt = Res()
            za = sb(s5, "za", [128, 512], F32); r_za = Res()
            zb = sb(s5, "zb", [128, 512], F32); r_zb = Res()
            for s in range(4):
                wm, rwm = load_w(w_br_m[:, s * 512:(s + 1) * 512], 512)
                wa, rwa = load_w(w_br_a[:, s * 512:(s + 1) * 512], 512)
                for jj in range(4):
                    j = s * 4 + jj
                    LD(gmt[:, 0, :], GMT[j], [r_GMT], [r_gmt])
                    LD(gmt[:, 1, :], GMT[16 + j], [r_GMT], [r_gmt])
                    for b in range(NB):
                        pm, rm = next_bank()
                        pa, ra = next_bank()
                        for k in range(16):
                            M(lambda e, pm=pm, wm=wm, jj=jj, k=k, b=b: e.matmul(pm[:, 0:TB], wm[:, k, jj * 128:(jj + 1) * 128], MOT[:, k, b * TB:(b + 1) * TB],
                                                                               start=(k == 0), stop=(k == 15)), [rwm, r_MOT], [rm])
                        for k in range(16):
                            M(lambda e, pa=pa, wa=wa, jj=jj, k=k, b=b: e.matmul(pa[:, 0:TB], wa[:, k, jj * 128:(jj + 1) * 128], AOT[:, k, b * TB:(b + 1) * TB],
                                                                               start=(k == 0), stop=(k == 15)), [rwa, r_AOT], [ra])
                        V(lambda e, pm=pm, b=b: e.tensor_tensor(out=za[:, 0:TB], in0=pm[:, 0:TB], in1=gmt[:, 0, b * TB:(b + 1) * TB], op=ALU.mult),
                          [rm, r_gmt], [r_za])
                        V(lambda e, pa=pa, b=b: e.tensor_tensor(out=zb[:, 0:TB], in0=pa[:, 0:TB], in1=gmt[:, 1, b * TB:(b + 1) * TB], op=ALU.mult),
                          [ra, r_gmt], [r_zb])
                        G(lambda e, j=j, b=b: e.tensor_tensor(out=zT[:, j, b * TB:(b + 1) * TB], in0=za[:, 0:TB], in1=zb[:, 0:TB], op=ALU.add),
                          [r_za, r_zb], [r_zT])
                _compact(CONSTS + [r_MOT, r_AOT])
            if cfg.DEBUG:
                P.barrier()
                LD(dbg_out("dbg_MOT", [128, 16, T], BF16), MOT[:], [r_MOT], [Res()])
                LD(dbg_out("dbg_AOT", [128, 16, T], BF16), AOT[:], [r_AOT], [Res()])
                LD(dbg_out("dbg_zT", [128, 16, T], BF16), zT[:], [r_zT], [Res()])
            P.barrier()
            for c in range(NOWN):
                LD(acc[:, c, :], xf[(C0 + c) * 128:(C0 + c + 1) * 128, :], w=[r_acc[c]])
            LD(gt1_bc[:], bc_row(mod_d[0:1, 2 * D:3 * D], D), [r_mod], [r_gt1])
            for db in range(4):
                wo, rwo = load_w(w_out[:, db * 512:(db + 1) * 512], 512)
                for c in range(NOWN):
                    pb, rb = next_bank()
                    for k in range(16):
                        M(lambda e, pb=pb, wo=wo, k=k, c=c: e.matmul(pb[:, :], zT[:, k, c * 128:(c + 1) * 128], wo[:, k, :], start=(k == 0), stop=(k == 15)),
                          [rwo, r_zT], [rb])
                    V(lambda e, pb=pb, db=db: e.tensor_tensor(out=za[:], in0=pb[:], in1=gt1_bc[:, db * 512:(db + 1) * 512], op=ALU.mult),
                      [rb, r_gt1], [r_za])
                    G(lambda e, c=c, db=db: e.tensor_tensor(out=acc[:, c, db * 512:(db + 1) * 512], in0=acc[:, c, db * 512:(db + 1) * 512], in1=za[:], op=ALU.add),
                      [r_za, r_acc[c]], [r_acc[c]])
                _compact(CONSTS + [r_zT, r_gt1])
        P.barrier()

        if cfg.DEBUG:
            LD(dbg_out("dbg_hx", [128, NOWN, D], F32), acc[:], r_acc, [Res()])
            P.barrier()
        with contextlib.ExitStack() as s5b:
            s5b = Bump(98 * KB, ARENA)
            Mk_f = sb(s5b, "Mk_f", [128, NOWN, NE], F32); r_Mkf = Res()
            Mk_b = sb(s5b, "Mk_b", [128, NOWN, NE], BF16); r_Mkb = Res()
            triSU_b = sb(s5b, "triSU_b", [128, 128], BF16); r_triSU = Res()
            LDC(triSU_b[:], c_triSU, w=[r_triSU])
            A2_bc = sb(s5b, "A2_bc", [128, D], F32)
            sh2_bc = sb(s5b, "sh2_bc", [128, D], F32)
            g2b = sb(s5b, "g2b", [128, D], F32); r_g2b = Res()
            LD(g2b[:], bc_row(g2[0:1, :], D), w=[r_g2b])
            LD(sh2_bc[:], bc_row(mod_d[0:1, 3 * D:4 * D], D), [r_mod], [r_sh2])
            LD(A2_bc[:], bc_row(mod_d[0:1, 4 * D:5 * D], D), [r_mod], [r_A2])
            V(lambda e: e.scalar_tensor_tensor(out=A2_bc[:], in0=A2_bc[:], scalar=1.0, in1=g2b[:], op0=ALU.add, op1=ALU.mult),
              [r_A2, r_g2b], [r_A2])
            u2f = sb(s5b, "u2f", [128, D], F32); r_u2f = Res()
            u2b = sb(s5b, "u2b", [128, D], BF16); r_u2b = Res()
            u2Tf = sb(s5b, "u2Tf", [128, 4, 128], F32); r_u2Tf = Res()
            wr_f = sb(s5b, "wr_f", [128, DC, NE], F32); r_wr = Res()
            br_bc = sb(s5b, "br_bc", [128, NE], F32); r_br = Res()
            lg = sb(s5b, "lg", [128, NE], F32); r_lg = Res()
            mx8 = sb(s5b, "mx8", [128, 8], F32); r_mx8 = Res()
            nmx = sb(s5b, "nmx", [128, 1], F32); r_nmx = Res()
            ex = sb(s5b, "ex", [128, NE], F32); r_ex = Res()
            msk = sb(s5b, "msk", [128, NE], F32); r_msk = Res()
            esum = sb(s5b, "esum", [128, 1], F32); r_esum = Res()
            ss2 = sb(s5b, "ss2", [128, 1], F32); r_ss2 = Res()
            LD(wr_f[:], w_router.rearrange("(dc p) n -> p dc n", p=128), w=[r_wr])
            LD(br_bc[:], bc_row(b_router[0:1, :], NE), w=[r_br])
            for c in range(NOWN):
                G(lambda e: e.memset(ss2[:], 0.0), w=[r_ss2])
                A(lambda e, c=c: e.activation(out=u2b[:], in_=acc[:, c, :], func=AF.Square, accum_out=ss2[:]), [r_acc[c], r_ss2], [r_u2b, r_ss2])
                A(lambda e: e.activation(out=ss2[:], in_=ss2[:], func=AF.Sqrt, bias=epsb[:], scale=1.0 / D), [r_ss2, r_eps], [r_ss2])
                V(lambda e: e.reciprocal(out=ss2[:], in_=ss2[:]), [r_ss2], [r_ss2])
                V(lambda e, c=c: e.scalar_tensor_tensor(out=u2f[:], in0=acc[:, c, :], scalar=ss2[:, 0:1], in1=A2_bc[:], op0=ALU.mult, op1=ALU.mult),
                  [r_acc[c], r_ss2, r_A2], [r_u2f])
                V(lambda e: e.tensor_tensor(out=u2f[:], in0=u2f[:], in1=sh2_bc[:], op=ALU.add), [r_u2f, r_sh2], [r_u2f])
                G(lambda e, c=c: e.tensor_copy(out=u2tm[:, c, :], in_=u2f[:]), [r_u2f], [r_u2T])
                pl, rl = next_bank()
                for q4 in range(DC // 4):
                    pb, rb = next_bank()
                    if pb is pl:
                        pb, rb = next_bank()
                    for j in range(4):
                        dc = q4 * 4 + j
                        M(lambda e, pb=pb, dc=dc, j=j: e.transpose(pb[:, j * 128:(j + 1) * 128], u2f[:, dc * 128:(dc + 1) * 128], ident_f[:]),
                          [r_u2f, r_identf], [rb])
                    A(lambda e, pb=pb: e.activation(out=u2Tf[:], in_=pb[:].rearrange("p (j t) -> p j t", j=4), func=AF.Copy),
                      [rb], [r_u2Tf])
                    for j in range(4):
                        dc = q4 * 4 + j
                        M(lambda e, pl=pl, dc=dc, j=j: e.matmul(pl[:, 0:NE], u2Tf[:, j, :], wr_f[:, dc, :], start=(dc == 0), stop=(dc == DC - 1)),
                          [r_u2Tf, r_wr], [rl])
                V(lambda e, pl=pl: e.tensor_tensor(out=lg[:], in0=pl[:, 0:NE], in1=br_bc[:], op=ALU.add), [rl, r_br], [r_lg])
                V(lambda e: e.max(out=mx8[:], in_=lg[:]), [r_lg], [r_mx8])
                V(lambda e: e.tensor_scalar(out=nmx[:], in0=mx8[:, 0:1], scalar1=-1.0, scalar2=None, op0=ALU.mult), [r_mx8], [r_nmx])
                A(lambda e: e.activation(out=ex[:], in_=lg[:], func=AF.Exp, bias=nmx[:], scale=1.0), [r_lg, r_nmx], [r_ex])
                V(lambda e: e.tensor_scalar(out=msk[:], in0=lg[:], scalar1=mx8[:, cfg.TOPK - 1:cfg.TOPK], scalar2=None, op0=ALU.is_ge),
                  [r_lg, r_mx8], [r_msk])
                V(lambda e: e.tensor_tensor(out=ex[:], in0=ex[:], in1=msk[:], op=ALU.mult), [r_ex, r_msk], [r_ex])
                G(lambda e, c=c: e.tensor_copy(out=Mk_f[:, c, :], in_=msk[:]), [r_msk], [r_Mkf])
                G(lambda e, c=c: e.tensor_copy(out=Mk_b[:, c, :], in_=msk[:]), [r_msk], [r_Mkb])
                V(lambda e: e.tensor_reduce(out=esum[:], in_=ex[:], axis=mybir.AxisListType.X, op=ALU.add), [r_ex], [r_esum])
                V(lambda e: e.reciprocal(out=esum[:], in_=esum[:]), [r_esum], [r_esum])
                V(lambda e, c=c: e.tensor_scalar(out=Gd[:, c, :], in0=ex[:], scalar1=esum[:, 0:1], scalar2=None, op0=ALU.mult),
                  [r_ex, r_esum], [r_Gd])
                _compact(CONSTS + [r_wr, r_br, r_A2, r_sh2])
            for c in range(NOWN):
                pp, rp = next_bank()
                for c2 in range(c):
                    M(lambda e, pp=pp, c2=c2: e.matmul(pp[:, 0:NE], ones_b[:], Mk_b[:, c2, :], start=(c2 == 0), stop=False),
                      [r_onesb, r_Mkb], [rp])
                M(lambda e, pp=pp, c=c: e.matmul(pp[:, 0:NE], triSU_b[:], Mk_b[:, c, :], start=(c == 0), stop=True),
                  [r_triSU, r_Mkb], [rp])
                V(lambda e, pp=pp, c=c: e.scalar_tensor_tensor(out=posm[:, c, :], in0=pp[:, 0:NE], scalar=1.0, in1=Mk_f[:, c, :],
                                                               op0=ALU.add, op1=ALU.mult), [rp, r_Mkf], [r_posm])
            V(lambda e: e.tensor_scalar(out=posm[:], in0=posm[:], scalar1=-1.0, scalar2=None, op0=ALU.add), [r_posm], [r_posm])

        if cfg.DEBUG:
            LD(dbg_out("dbg_Gd", [128, NOWN, NE], F32), Gd[:], [r_Gd], [Res()])
            LD(dbg_out("dbg_u2tm", [128, NOWN, D], BF16), u2tm[:], [r_u2T], [Res()])
            LD(dbg_out("dbg_posm", [128, NOWN, NE], F32), posm[:], [r_posm], [Res()])
        P.barrier()
        if True:
            CAP = 384
            s6 = Bump(106 * KB, ARENA)
            LD(gt2_bc[:], bc_row(mod_d[0:1, 5 * D:6 * D], D), [r_mod], [r_gt2])
            iota_f = sb(s6, "iota_f", [128, CAP], F32); r_iota = Res()
            LD(iota_f[:], c_iota[:, 0:CAP], w=[r_iota])
            STs = sb(s6, "STs", [128, CAP // 128, T], BF16); r_ST = Res()
            xT = sb(s6, "xT", [128, DC, CAP], BF16); r_xT = Res()
            hid_off = s6.off
            hidT = sb(s6, "hidT", [128, FC, CAP], BF16); r_hid = Res()
            Ys = sb(s6, "Ys", [128, CAP // 128, D], BF16); r_Ys = Res()
            assert NOWN <= FC
            Ssel = at(hid_off, [128, NOWN, CAP], BF16); r_S = r_hid
            bgu = [sb(s6, f"bgu{i}", [128, 2 * FC], F32) for i in range(2)]
            rbgu = [Res(), Res()]
            bdn = [sb(s6, f"bdn{i}", [1, 512], BF16) for i in range(2)]
            rbdn = [Res(), Res()]
            bdn_i = [0]
            gg = sb(s6, "gg", [128, CAP], F32); r_gg = Res()
            sg = sb(s6, "sg", [128, CAP], BF16); r_sg = Res()
            uu = sb(s6, "uu", [128, CAP], BF16); r_uu = Res()
            jobs = []
            for ex_i in range(NE):
                for s in range(0, FC, 2):
                    jobs.append(("gu", ex_i, s))
                for db in range(4):
                    jobs.append(("dn", ex_i, db))

            def issue(job):
                kind, ex_i, k = job
                i0 = ring_i[0]
                ring_i[0] = (i0 + 1) % NR
                wt, wr = wring[i0], rring[i0]
                if kind == "gu":
                    vg = w_gu[ex_i][:, k * 128:(k + 2) * 128].rearrange("(dc p) n -> p dc n", p=128)
                    vu = w_gu[ex_i][:, DFF + k * 128:DFF + (k + 2) * 128].rearrange("(dc p) n -> p dc n", p=128)
                    LDC(wt[:, :, 0:256], vg, w=[wr])
                    P.dma("pool", lambda e: e.dma_start(out=wt[:, :, 256:512], in_=vu), [wr], [wr])
                else:
                    vd = w_dn[ex_i][:, k * 512:(k + 1) * 512].rearrange("(dc p) n -> p dc n", p=128)
                    LDC(wt[:, 0:FC, :], vd, w=[wr])
                    bi_ = bdn_i[0]
                    bdn_i[0] = 1 - bi_
                    LDC(bdn[bi_][:], b_dn[ex_i:ex_i + 1, k * 512:(k + 1) * 512], w=[rbdn[bi_]])
                    bd_q.append((bdn[bi_], rbdn[bi_]))
                return wt, wr

            bd_q = []

            def prologue(ex_i):
                pi = ex_i % 2
                P.dma("sp", lambda e: e.dma_start(out=bgu[pi][:], in_=b_gu[ex_i, :].rearrange("(j p) -> p j", p=128),
                                                  allow_slow_non_contiguous=True), [], [rbgu[pi]])
                for c in range(NOWN):
                    V(lambda e, c=c: e.tensor_scalar(out=Ssel[:, c, :], in0=iota_f[:], scalar1=posm[:, c, ex_i:ex_i + 1], scalar2=None,
                                                     op0=ALU.is_equal), [r_iota, r_posm], [r_S])
                for sbk in range(CAP // 128):
                    for c in range(NOWN):
                        M(lambda e, c=c, sbk=sbk: e.transpose(ptr[:, c * 128:(c + 1) * 128], Ssel[:, c, sbk * 128:(sbk + 1) * 128], ident_b[:]),
                          [r_S, r_identb], [r_ptr])
                    A(lambda e, sbk=sbk: e.activation(out=STs[:, sbk, :], in_=ptr[:, 0:T], func=AF.Copy), [r_ptr], [r_ST])
                for dc in range(DC):
                    pb, rb = next_bank()
                    for c in range(NOWN):
                        M(lambda e, pb=pb, dc=dc, c=c: e.matmul(pb[:, 0:CAP], u2tm[:, c, dc * 128:(dc + 1) * 128], Ssel[:, c, :],
                                                                start=(c == 0), stop=(c == NOWN - 1)), [r_u2T, r_S], [rb])
                    if dc % 2 == 0:
                        V(lambda e, pb=pb, dc=dc: e.tensor_copy(out=xT[:, dc, :], in_=pb[:, 0:CAP]), [rb], [r_xT])
                    else:
                        A(lambda e, pb=pb, dc=dc: e.activation(out=xT[:, dc, :], in_=pb[:, 0:CAP], func=AF.Copy), [rb], [r_xT])

            cur = issue(jobs[0])
            for ji, job in enumerate(jobs):
                nxt = issue(jobs[ji + 1]) if ji + 1 < len(jobs) else None
                kind, ex_i, k = job
                wt, wr = cur
                pi = ex_i % 2
                if kind == "dn":
                    cur_bd = bd_q.pop(0)
                if kind == "gu" and k == 0:
                    prologue(ex_i)
                if kind == "gu":
                    for ii in range(2):
                        i = k + ii
                        pg, rg = next_bank()
                        pu, ru = next_bank()
                        for dc in range(DC):
                            M(lambda e, pg=pg, wt=wt, ii=ii, dc=dc: e.matmul(pg[:, 0:CAP], wt[:, dc, ii * 128:(ii + 1) * 128], xT[:, dc, :],
                                                                          start=(dc == 0), stop=(dc == DC - 1)), [wr, r_xT], [rg])
                        for dc in range(DC):
                            M(lambda e, pu=pu, wt=wt, ii=ii, dc=dc: e.matmul(pu[:, 0:CAP], wt[:, dc, 256 + ii * 128:256 + (ii + 1) * 128], xT[:, dc, :],
                                                                          start=(dc == 0), stop=(dc == DC - 1)), [wr, r_xT], [ru])
                        V(lambda e, pg=pg, i=i, pi=pi: e.tensor_scalar(out=gg[:], in0=pg[:, 0:CAP], scalar1=bgu[pi][:, i:i + 1], scalar2=7.0,
                                                                      op0=ALU.add, op1=ALU.min), [rg, rbgu[pi]], [r_gg])
                        A(lambda e: e.activation(out=sg[:], in_=gg[:], func=AF.Sigmoid, scale=1.702), [r_gg], [r_sg])
                        V(lambda e, pu=pu, i=i, pi=pi: e.tensor_scalar(out=uu[:], in0=pu[:, 0:CAP], scalar1=bgu[pi][:, FC + i:FC + i + 1], scalar2=7.0,
                                                                      op0=ALU.add, op1=ALU.min), [ru, rbgu[pi]], [r_uu])
                        V(lambda e: e.tensor_scalar(out=uu[:], in0=uu[:], scalar1=-7.0, scalar2=1.0, op0=ALU.max, op1=ALU.add),
                          [r_uu], [r_uu])
                        V(lambda e: e.tensor_tensor(out=gg[:], in0=gg[:], in1=sg[:], op=ALU.mult), [r_gg, r_sg], [r_gg])
                        V(lambda e, i=i: e.tensor_tensor(out=hidT[:, i, :], in0=gg[:], in1=uu[:], op=ALU.mult), [r_gg, r_uu], [r_hid])
                else:
                    db = k
                    bd, rbd = cur_bd
                    for sbk in range(CAP // 128):
                        pb, rb = next_bank()
                        for i in range(FC):
                            M(lambda e, pb=pb, wt=wt, i=i, sbk=sbk: e.matmul(pb[:, :], hidT[:, i, sbk * 128:(sbk + 1) * 128], wt[:, i, :], start=(i == 0), stop=False),
                              [wr, r_hid], [rb])
                        M(lambda e, pb=pb, bd=bd: e.matmul(pb[:, :], ones_b[0:1, :], bd[0:1, :], start=False, stop=True),
                          [r_onesb, rbd], [rb])
                        V(lambda e, pb=pb, sbk=sbk, db=db: e.tensor_tensor(out=Ys[:, sbk, db * 512:(db + 1) * 512], in0=pb[:], in1=gt2_bc[:, db * 512:(db + 1) * 512],
                                                                          op=ALU.mult), [rb, r_gt2], [r_Ys])
                    for c in range(NOWN):
                        pb, rb = next_bank()
                        for sbk in range(CAP // 128):
                            M(lambda e, pb=pb, sbk=sbk, c=c, db=db: e.matmul(pb[:, :], STs[:, sbk, c * 128:(c + 1) * 128], Ys[:, sbk, db * 512:(db + 1) * 512],
                                                                            start=(sbk == 0), stop=(sbk == CAP // 128 - 1)), [r_ST, r_Ys], [rb])
                        V(lambda e, pb=pb, c=c, ex_i=ex_i, db=db: e.scalar_tensor_tensor(out=acc[:, c, db * 512:(db + 1) * 512], in0=pb[:], scalar=Gd[:, c, ex_i:ex_i + 1],
                                                                                       in1=acc[:, c, db * 512:(db + 1) * 512], op0=ALU.mult, op1=ALU.add),
                          [rb, r_Gd, r_acc[c]], [r_acc[c]])
                _compact(CONSTS + [r_u2T, r_Gd, r_gt2, r_posm, r_iota])
                cur = nxt

        r_out = Res("out")
        for c in range(NOWN):
            LD(out_d[c * 128:(c + 1) * 128, :], acc[:, c, :], [r_acc[c]], [r_out])
        P.finish()
    return nc


def _consts(cfg, h):
    NT = cfg.NCTX + cfg.NOTH + cfg.NOWN
    TA = NT * 128
    ident = np.eye(128, dtype=np.float32)
    jj, tt = np.meshgrid(np.arange(128), np.arange(128), indexing="ij")
    triU = (jj <= tt).astype(np.float32)
    triL = (jj >= tt).astype(np.float32)
    rot = np.zeros((128, 128), np.float32)
    for i in range(64):
        rot[2 * i + 1, 2 * i] = -1.0
        rot[2 * i, 2 * i + 1] = 1.0
    nctx = cfg.NCTX * 128
    seq = cfg.SEQ
    pos = np.arange(seq)
    if h == 0:
        pos = pos[::-1]
    rows = (pos // cfg.GRID_W).astype(np.float32)
    cols = (pos % cfg.GRID_W).astype(np.float32)
    freqs = np.exp(-math.log(10000.0) * np.arange(32, dtype=np.float32) / 32).astype(np.float32)
    ang = np.concatenate([rows[:, None] * freqs, cols[:, None] * freqs], axis=-1).astype(np.float32)
    cos = np.repeat(np.cos(ang), 2, axis=1).T
    sin = np.repeat(np.sin(ang), 2, axis=1).T
    cosT = np.concatenate([np.ones((128, nctx), np.float32), cos.astype(np.float32)], axis=1)
    sinT = np.concatenate([np.zeros((128, nctx), np.float32), sin.astype(np.float32)], axis=1)
    assert cosT.shape[1] == TA
    triSU = (jj < tt).astype(np.float32)
    iota = np.ascontiguousarray(np.broadcast_to(np.arange(512, dtype=np.float32)[None, :], (128, 512)))
    return dict(c_ident=ident, c_triU=triU, c_triL=triL, c_rot=rot, c_triSU=triSU, c_iota=iota,
                c_cos=np.ascontiguousarray(cosT), c_sin=np.ascontiguousarray(sinT))


def make_in_maps(cfg, inp):
    f = lambda a: np.ascontiguousarray(np.asarray(a, dtype=np.float32))
    x, c, ctx, c_ctx = f(inp["x"]), f(inp["c"]), f(inp["ctx"]), f(inp["c_ctx"])
    B = x.shape[0]
    shared = dict(
        w_mod=f(inp["w_mod"][0]), b_mod=f(inp["b_mod"][0])[None, :], g1=f(inp["g_norm1"][0])[None, :],
        g2=f(inp["g_norm2"][0])[None, :], w_in=f(inp["w_in"][0]), b_in=f(inp["b_in"][0])[None, :],
        g_q=f(inp["g_q"][0])[:, None], g_k=f(inp["g_k"][0])[:, None], g_mh=f(inp["g_mh"][0])[None, :],
        w_br_m=f(inp["w_br_m"][0]), w_br_a=f(inp["w_br_a"][0]), w_out=f(inp["w_out"][0]),
        w_router=f(inp["w_router"][0]), b_router=f(inp["b_router"][0])[None, :],
        w_gu=f(inp["w_gu"][0]), b_gu=f(inp["b_gu"][0]), w_dn=f(inp["w_dn"][0]), b_dn=f(inp["b_dn"][0]),
    )
    wgt = shared["w_in"][:, OFF_GT:OFF_GT + 16]
    bgt = shared["b_in"][0, OFF_GT:OFF_GT + 16]
    perm = {1: list(range(0, 4)) + list(range(8, 12)) + list(range(4, 8)) + list(range(12, 16)),
            0: list(range(8, 12)) + list(range(0, 4)) + list(range(12, 16)) + list(range(4, 8))}
    half = cfg.SEQ // 2
    maps = []
    for core in range(2 * B):
        b, h = core // 2, core % 2
        if h == 1:
            xfm = np.concatenate([ctx[b], x[b]], axis=0)
        else:
            xfm = np.concatenate([ctx[b, ::-1], x[b, ::-1]], axis=0)
        m = dict(shared)
        m["xf"] = np.ascontiguousarray(xfm)
        m["cc"] = np.ascontiguousarray(np.stack([c[b], c_ctx], axis=0))
        m["wg16"] = np.ascontiguousarray(wgt[:, perm[h]])
        m["bg16"] = np.ascontiguousarray(bgt[perm[h]])[None, :]
        m.update(_consts(cfg, h))
        maps.append(m)
    return maps


def assemble(cfg, results, B):
    half = cfg.SEQ // 2
    out = np.zeros((B, cfg.SEQ, cfg.D), np.float32)
    for core in range(2 * B):
        b, h = core // 2, core % 2
        o = np.asarray(results[core]["out"], dtype=np.float32)
        if h == 1:
            out[b, half:] = o
        else:
            out[b, :half] = o[::-1]
    return out


def kernel(**inputs):
    cfg = Cfg()
    nc = build(cfg)
    maps = make_in_maps(cfg, inputs)
    res = run_bass_kernel_spmd(nc, maps, core_ids=list(range(8)))
    return assemble(cfg, res.results, 4)
```
